# Optimizing a Trainium2 kernel written in Bass

```python
import math
import jax, jax.numpy as jnp
from jax import lax
import numpy as np

D_MODEL = 2048
BATCH = 4
SEQ = 4096
DEPTH = 1

GRID_W = 64
CTX_LEN = 256
SSD_HEADS = 32
SSD_HEAD_DIM = 64
SSD_INNER = SSD_HEADS * SSD_HEAD_DIM
SSD_GROUPS = 4
SSD_HPG = SSD_HEADS // SSD_GROUPS
SSD_STATE = 128
SSD_CONV = 3
CHUNK = 128
SSD_XBC = SSD_INNER + 2 * SSD_GROUPS * SSD_STATE
SC_DIM = 2048
SC_CONV = 3
N_EXPERTS = 16
EXPERT_FF = 2048
CAPACITY_FACTOR = 2
EPS = 1e-6
PROJ_OFFSETS = (
    SSD_INNER,
    SSD_INNER + SSD_XBC,
    SSD_INNER + SSD_XBC + 2 * SSD_HEADS,
    SSD_INNER + SSD_XBC + 2 * SSD_HEADS + SC_DIM,
    SSD_INNER + SSD_XBC + 2 * SSD_HEADS + 2 * SC_DIM,
    SSD_INNER + SSD_XBC + 2 * SSD_HEADS + 3 * SC_DIM,
)
PROJ_COLS = SSD_INNER + SSD_XBC + 2 * SSD_HEADS + 3 * SC_DIM + 2 * D_MODEL

kernel_name = "hybrid_ssd_shortconv_ecmoe_dit_block"


def rmsnorm(x, w):
    x32 = x.astype(jnp.float32)
    y = x32 * lax.rsqrt(jnp.mean(x32 * x32, axis=-1, keepdims=True) + EPS)
    return (y * w.astype(jnp.float32)).astype(x.dtype)


def rev(a):
    return jnp.flip(a, axis=1)


def dwconv(x, w, b=None):
    k_w = w.shape[0]
    pad = k_w // 2
    length = x.shape[1]
    xp = jnp.pad(x, ((0, 0), (pad, pad), (0, 0)))
    y = sum(xp[:, k:k + length] * w[k] for k in range(k_w))
    return y if b is None else y + b


def seq_conv(x, w, b, grid):
    if grid:
        bsz, length, ch = x.shape
        rows = length // GRID_W
        return dwconv(x.reshape(bsz * rows, GRID_W, ch), w, b).reshape(bsz, length, ch)
    return dwconv(x, w, b)


def ssd_prep(xbc, dt_raw, p, grid):
    bsz, length, _ = xbc.shape
    xbc = jax.nn.silu(seq_conv(xbc, p['ssd_conv_w'], p['ssd_conv_b'], grid))
    xs, bm, cm = jnp.split(xbc, [SSD_INNER, SSD_INNER + SSD_GROUPS * SSD_STATE], axis=-1)
    xs = xs.reshape(bsz, length, SSD_GROUPS, SSD_HPG, SSD_HEAD_DIM)
    bm = bm.reshape(bsz, length, SSD_GROUPS, SSD_STATE)
    cm = cm.reshape(bsz, length, SSD_GROUPS, SSD_STATE)
    dt = jax.nn.softplus(dt_raw.astype(jnp.float32).reshape(bsz, length, 2, SSD_HEADS)
                         + p['dt_bias'].astype(jnp.float32))
    dt = dt.reshape(bsz, length, 2, SSD_GROUPS, SSD_HPG)
    a = -jnp.exp(p['a_log'].astype(jnp.float32)).reshape(2, SSD_GROUPS, SSD_HPG)
    return xs, bm, cm, dt, a


def ssd_chunk_states(xs, dt, a, bm, h0):
    bsz, length = xs.shape[:2]
    nc = length // CHUNK
    xc = xs.reshape(bsz, nc, CHUNK, SSD_GROUPS, SSD_HPG, SSD_HEAD_DIM)
    dtc = dt.reshape(bsz, nc, CHUNK, SSD_GROUPS, SSD_HPG)
    bc = bm.reshape(bsz, nc, CHUNK, SSD_GROUPS, SSD_STATE)
    acs = jnp.cumsum(dtc * a, axis=2)
    decay_to_end = jnp.exp(acs[:, :, -1:] - acs)
    s = jnp.einsum('bcjgr,bcjgn,bcjgrp->bcgrpn', decay_to_end * dtc, bc, xc)
    chunk_decay = jnp.exp(acs[:, :, -1])

    def step(h, inp):
        dec, s_c = inp
        return dec[..., None, None] * h + s_c, h

    h_final, h_enter = lax.scan(step, h0,
                                (jnp.moveaxis(chunk_decay, 1, 0), jnp.moveaxis(s, 1, 0)))
    return acs, jnp.moveaxis(h_enter, 0, 1), h_final


def ssd_scan(xs, dt, a, bm, cm, h0):
    bsz, length = xs.shape[:2]
    nc = length // CHUNK
    acs, h_enter, h_final = ssd_chunk_states(xs, dt, a, bm, h0)
    xc = xs.reshape(bsz, nc, CHUNK, SSD_GROUPS, SSD_HPG, SSD_HEAD_DIM)
    dtc = dt.reshape(bsz, nc, CHUNK, SSD_GROUPS, SSD_HPG)
    bc = bm.reshape(bsz, nc, CHUNK, SSD_GROUPS, SSD_STATE)
    cc = cm.reshape(bsz, nc, CHUNK, SSD_GROUPS, SSD_STATE)
    cb = jnp.einsum('bcign,bcjgn->bcgij', cc, bc)
    at = jnp.moveaxis(acs, 2, -1)
    seg = at[..., :, None] - at[..., None, :]
    mask = jnp.tril(jnp.ones((CHUNK, CHUNK), dtype=bool))
    lmat = jnp.exp(jnp.where(mask, seg, -jnp.inf))
    m = cb[:, :, :, None] * lmat * jnp.moveaxis(dtc, 2, -1)[..., None, :]
    y_diag = jnp.einsum('bcgrij,bcjgrp->bcigrp', m, xc)
    y_off = jnp.einsum('bcign,bcgrpn->bcigrp', cc, h_enter) * jnp.exp(acs)[..., None]
    y = (y_diag + y_off).reshape(bsz, length, SSD_GROUPS, SSD_HPG, SSD_HEAD_DIM)
    return y, h_final


def token_mixer(h, states, p, grid):
    bsz, length, _ = h.shape
    proj = h @ p['w_in']
    z, xbc, dt_raw, sc_b, sc_c, sc_v, g_raw = jnp.split(proj, PROJ_OFFSETS, axis=-1)
    xs, bm, cm, dt, a = ssd_prep(xbc, dt_raw, p, grid)
    h0_f, h0_b = states
    y_f, hf_f = ssd_scan(xs, dt[:, :, 0], a[0], bm, cm, h0_f)
    y_b, hf_b = ssd_scan(rev(xs), rev(dt[:, :, 1]), a[1], rev(bm), rev(cm), h0_b)
    y = y_f + rev(y_b) + p['ssd_d'].reshape(SSD_GROUPS, SSD_HPG)[..., None] * xs
    y = y.reshape(bsz, length, SSD_INNER) * jax.nn.silu(z)
    y = rmsnorm(y.reshape(bsz, length, SSD_GROUPS, -1),
                p['ssd_norm_w'].reshape(SSD_GROUPS, -1)).reshape(bsz, length, SSD_INNER)
    ssd_out = y.astype(h.dtype) @ p['w_ssd_out']
    sc_y = sc_b * seq_conv(sc_c * sc_v, p['sc_conv_w'], None, grid)
    sc_out = sc_y @ p['w_sc_out']
    g_ssd, g_sc = jnp.split(jax.nn.sigmoid(g_raw + p['b_gate']), 2, axis=-1)
    merged = g_ssd * ssd_out + g_sc * sc_out
    return (merged @ p['w_o']).astype(h.dtype), (hf_f, hf_b)


def ctx_final_states(hc, p):
    cols = hc @ p['w_in'][:, SSD_INNER:SSD_INNER + SSD_XBC + 2 * SSD_HEADS]
    xbc, dt_raw = jnp.split(cols, [SSD_XBC], axis=-1)
    xs, bm, _, dt, a = ssd_prep(xbc, dt_raw, p, False)
    h0 = jnp.zeros((hc.shape[0], SSD_GROUPS, SSD_HPG, SSD_HEAD_DIM, SSD_STATE), jnp.float32)
    hf_f = ssd_chunk_states(xs, dt[:, :, 0], a[0], bm, h0)[2]
    hf_b = ssd_chunk_states(rev(xs), rev(dt[:, :, 1]), a[1], rev(bm), h0)[2]
    return (hf_f, hf_b)


def expert_choice_ffn(h, p):
    bsz, n, d = h.shape
    cap = CAPACITY_FACTOR * n // N_EXPERTS
    aff = jax.nn.softmax((h @ p['w_router']).astype(jnp.float32), axis=-1)
    vals, idx = lax.top_k(jnp.swapaxes(aff, 1, 2), cap)
    xg = jax.vmap(lambda hb, ib: hb[ib])(h, idx)
    g = jnp.einsum('becd,edf->becf', xg, p['w_e1'])
    u = jnp.einsum('becd,edf->becf', xg, p['w_e3'])
    out = jnp.einsum('becf,efd->becd', jax.nn.silu(g) * u, p['w_e2'])
    out = out * vals[..., None].astype(out.dtype)
    y = jax.vmap(lambda ib, ob: jnp.zeros((n, d), ob.dtype).at[ib.reshape(-1)].add(ob.reshape(-1, d)))(idx, out)
    return y.astype(h.dtype)


def modulate(h, shift, scale):
    return h * (1 + scale) + shift


def layer(x, ctx, c, c_ctx, p, last):
    mod_x = (jax.nn.silu(c) @ p['w_mod'] + p['b_mod'])[:, None, :]
    mod_c = jax.nn.silu(c_ctx) @ p['w_mod'] + p['b_mod']
    shx, scx, gx, shfx, scfx, gfx = jnp.split(mod_x, 6, axis=-1)
    shc, scc, gc, shfc, scfc, gfc = jnp.split(mod_c, 6, axis=-1)
    hc = modulate(rmsnorm(ctx, p['norm_pre_mix']), shc, scc)
    if last:
        states = ctx_final_states(hc, p)
    else:
        h0 = jnp.zeros((ctx.shape[0], SSD_GROUPS, SSD_HPG, SSD_HEAD_DIM, SSD_STATE), jnp.float32)
        out_c, states = token_mixer(hc, (h0, h0), p, False)
        ctx = ctx + gc * rmsnorm(out_c, p['norm_post_mix'])
        hc2 = modulate(rmsnorm(ctx, p['norm_pre_ffn']), shfc, scfc)
        ctx = ctx + gfc * rmsnorm(expert_choice_ffn(hc2, p), p['norm_post_ffn'])
    hx = modulate(rmsnorm(x, p['norm_pre_mix']), shx, scx)
    out_x, _ = token_mixer(hx, states, p, True)
    x = x + gx * rmsnorm(out_x, p['norm_post_mix'])
    hx2 = modulate(rmsnorm(x, p['norm_pre_ffn']), shfx, scfx)
    x = x + gfx * rmsnorm(expert_choice_ffn(hx2, p), p['norm_post_ffn'])
    return x, ctx


def setup_inputs(seed: int = 0) -> dict:
    key = jax.random.key(seed)
    ks = jax.random.split(key, 32)
    f32 = jnp.float32

    def nrm(k, shape, scale):
        return jax.random.normal(k, shape, f32) * scale

    def gain(k, shape):
        return 1.0 + 0.05 * jax.random.normal(k, shape, f32)

    u = jax.random.uniform(ks[10], (DEPTH, 2, SSD_HEADS), f32)
    dt0 = jnp.exp(u * (math.log(0.1) - math.log(0.001)) + math.log(0.001))
    dt_bias = dt0 + jnp.log(-jnp.expm1(-dt0))
    a_log = jnp.log(jax.random.uniform(ks[11], (DEPTH, 2, SSD_HEADS), f32, minval=1.0, maxval=16.0))
    return {
        'x': nrm(ks[0], (BATCH, SEQ, D_MODEL), 1.0),
        'c': nrm(ks[1], (BATCH, D_MODEL), 1.0),
        'ctx': nrm(ks[2], (BATCH, CTX_LEN, D_MODEL), 1.0),
        'c_ctx': nrm(ks[3], (D_MODEL,), 1.0),
        'w_mod': nrm(ks[4], (DEPTH, D_MODEL, 6 * D_MODEL), 0.3 * D_MODEL ** -0.5),
        'b_mod': nrm(ks[5], (DEPTH, 6 * D_MODEL), 0.02),
        'norm_pre_mix': gain(ks[6], (DEPTH, D_MODEL)),
        'norm_post_mix': gain(ks[7], (DEPTH, D_MODEL)),
        'w_in': nrm(ks[8], (DEPTH, D_MODEL, PROJ_COLS), D_MODEL ** -0.5),
        'ssd_conv_w': nrm(ks[9], (DEPTH, SSD_CONV, SSD_XBC), SSD_CONV ** -0.5),
        'ssd_conv_b': nrm(ks[12], (DEPTH, SSD_XBC), 0.02),
        'dt_bias': dt_bias,
        'a_log': a_log,
        'ssd_d': gain(ks[13], (DEPTH, SSD_HEADS)),
        'ssd_norm_w': gain(ks[14], (DEPTH, SSD_INNER)),
        'w_ssd_out': nrm(ks[15], (DEPTH, SSD_INNER, D_MODEL), SSD_INNER ** -0.5),
        'sc_conv_w': nrm(ks[16], (DEPTH, SC_CONV, SC_DIM), SC_CONV ** -0.5),
        'w_sc_out': nrm(ks[17], (DEPTH, SC_DIM, D_MODEL), SC_DIM ** -0.5),
        'b_gate': nrm(ks[18], (DEPTH, 2 * D_MODEL), 0.02),
        'w_o': nrm(ks[19], (DEPTH, D_MODEL, D_MODEL), D_MODEL ** -0.5),
        'norm_pre_ffn': gain(ks[20], (DEPTH, D_MODEL)),
        'norm_post_ffn': gain(ks[21], (DEPTH, D_MODEL)),
        'w_router': nrm(ks[22], (DEPTH, D_MODEL, N_EXPERTS), D_MODEL ** -0.5),
        'w_e1': nrm(ks[23], (DEPTH, N_EXPERTS, D_MODEL, EXPERT_FF), D_MODEL ** -0.5),
        'w_e3': nrm(ks[24], (DEPTH, N_EXPERTS, D_MODEL, EXPERT_FF), D_MODEL ** -0.5),
        'w_e2': nrm(ks[25], (DEPTH, N_EXPERTS, EXPERT_FF, D_MODEL), EXPERT_FF ** -0.5),
    }


def reference(x, c, ctx, c_ctx, w_mod, b_mod, norm_pre_mix, norm_post_mix, w_in, ssd_conv_w,
              ssd_conv_b, dt_bias, a_log, ssd_d, ssd_norm_w, w_ssd_out, sc_conv_w, w_sc_out,
              b_gate, w_o, norm_pre_ffn, norm_post_ffn, w_router, w_e1, w_e3, w_e2):
    for l in range(DEPTH):
        p = {
            'w_mod': w_mod[l], 'b_mod': b_mod[l],
            'norm_pre_mix': norm_pre_mix[l], 'norm_post_mix': norm_post_mix[l],
            'w_in': w_in[l], 'ssd_conv_w': ssd_conv_w[l], 'ssd_conv_b': ssd_conv_b[l],
            'dt_bias': dt_bias[l], 'a_log': a_log[l], 'ssd_d': ssd_d[l],
            'ssd_norm_w': ssd_norm_w[l], 'w_ssd_out': w_ssd_out[l],
            'sc_conv_w': sc_conv_w[l], 'w_sc_out': w_sc_out[l],
            'b_gate': b_gate[l], 'w_o': w_o[l],
            'norm_pre_ffn': norm_pre_ffn[l], 'norm_post_ffn': norm_post_ffn[l],
            'w_router': w_router[l], 'w_e1': w_e1[l], 'w_e3': w_e3[l], 'w_e2': w_e2[l],
        }
        x, ctx = layer(x, ctx, c, c_ctx, p, l == DEPTH - 1)
    return x
```

```python
import contextlib
import numpy as np
import ml_dtypes
import concourse.bass as bass
import concourse.mybir as mybir
from concourse.bass_utils import run_bass_kernel_spmd

F32 = mybir.dt.float32
BF16 = mybir.dt.bfloat16
I32 = mybir.dt.int32
AF = mybir.ActivationFunctionType
ALU = mybir.AluOpType

D = 2048
L = 4096
NCH = L // 128
CTXL = 256
PROJ = 15424
C_Z, C_XBC, C_DT, C_SCB, C_SCC, C_SCV, C_GS, C_GC = 0, 2048, 5120, 5184, 7232, 9280, 11328, 13376
NE = 16
CAP = 512
EPS = 1e-6
TS = 256
NBIS = 34

PK_ID, PK_SU, PK_SL, PK_TLE, PK_TGE, PK_ONE = 0, 128, 256, 384, 512, 640
PK_C, PK_BMOD, PK_NPM, PK_SNW, PK_CW, PK_CB, PK_SCW, PK_BG = 768, 800, 832, 848, 864, 936, 960, 1008
PK_DTB, PK_ALOG, PK_SSD_D, PK_WR, PK_IOTA, PK_PIDX = 1040, 1104, 1168, 1200, 1456, 1968
PK_SELM, PK_GOFF, PK_OSRC, NPK = 1984, 2008, 2104, 2168


class T:
    __slots__ = ("name", "ap", "w", "r", "dsem", "dcnt", "root")

    def __init__(self, name, ap, root=None):
        self.name = name
        self.ap = ap
        self.w = None
        self.r = []
        self.dsem = None
        self.dcnt = 0
        self.root = root if root is not None else self


class K:
    def __init__(self, nc):
        self.nc = nc
        self.engs = {"pe": nc.tensor, "act": nc.scalar, "dve": nc.vector, "pool": nc.gpsimd, "sp": nc.sync}
        self.sems = {}
        self.cnt = {}
        for k in ("pe", "act", "dve", "pool"):
            self.sems[k] = nc.alloc_semaphore("s_" + k)
            self.cnt[k] = 0
        self.waited = {}
        self.nsem = 0
        self.dsems = []

    def _deps(self, reads, writes):
        deps = {}

        def add(d):
            if d is None:
                return
            k, v = d
            if deps.get(k, 0) < v:
                deps[k] = v
        for t in reads:
            add(t.root.w)
        for t in writes:
            add(t.root.w)
            for r in t.root.r:
                add(r)
        return deps

    def _wait(self, ek, deps):
        eng = self.engs[ek]
        for k, v in deps.items():
            if ek == "pe" and k == "pe":
                continue
            if self.waited.get((ek, k), 0) >= v:
                continue
            eng.wait_ge(self.sems[k], v)
            self.waited[(ek, k)] = v

    def _mark(self, me, reads, writes):
        for t in reads:
            t.root.r.append(me)
        for t in writes:
            t.root.w = me
            t.root.r = []

    def op(self, ek, fn, reads=(), writes=()):
        self._wait(ek, self._deps(reads, writes))
        ins = fn(self.engs[ek])
        self.cnt[ek] += 1
        ins.then_inc(self.sems[ek], 1)
        self._mark((ek, self.cnt[ek]), reads, writes)
        return ins

    def _dsem(self, semt):
        semt = semt.root
        if semt.dsem is None:
            semt.dsem = "d%d_%s" % (self.nsem, semt.name)
            self.sems[semt.dsem] = self.nc.alloc_semaphore(semt.dsem)
            self.dsems.append(semt)
            self.nsem += 1
        return semt

    def dma(self, qk, out_ap, in_ap, semt, reads=(), writes=(), fn=None, **kw):
        semt = self._dsem(semt)
        self._wait(qk, self._deps(reads, writes))
        if fn is None:
            ins = self.engs[qk].dma_start(out=out_ap, in_=in_ap, **kw)
        else:
            ins = fn(self.engs[qk])
        semt.dcnt += 1
        ins.then_inc(self.sems[semt.dsem], 16)
        me = (semt.dsem, 16 * semt.dcnt)
        self._mark(me, reads, writes)
        return me

    def barrier(self):
        tot = {k: self.cnt[k] for k in ("pe", "act", "dve", "pool")}
        for t in self.dsems:
            tot[t.dsem] = 16 * t.dcnt
        for ek in ("pe", "act", "dve", "pool", "sp"):
            eng = self.engs[ek]
            for k, v in tot.items():
                if v == 0 or self.waited.get((ek, k), 0) >= v:
                    continue
                eng.wait_ge(self.sems[k], v)
                self.waited[(ek, k)] = v


def build_program(stage=99, dbg=False, one_core=False):
    nc = bass.Bass("TRN2", target_bir_lowering=False)
    k = K(nc)

    def din(name, shape, dt=F32):
        return nc.dram_tensor(name, list(shape), dt, kind="ExternalInput").ap()

    x_d = din("x", [L, D])
    ctx_d = din("ctx", [CTXL, D])
    w_in_d = din("w_in", [D, PROJ])
    w_mod_d = din("w_mod", [D, 6 * D])
    w_ssd_d = din("w_ssd_out", [D, D])
    w_sc_d = din("w_sc_out", [D, D])
    w_o_d = din("w_o", [D, D])
    we_shape = [NE, D, D] if stage >= 5 else [2, 8, 8]
    w_e1_d = din("w_e1", we_shape)
    w_e3_d = din("w_e3", we_shape)
    w_e2_d = din("w_e2", we_shape)
    pk_d = din("pk", [128, NPK])
    reps_d = din("reps", [128, 7 * D])
    idb_d = din("idb", [128, 128], BF16)
    out_d = nc.dram_tensor("out", [L, D], F32, kind="ExternalOutput").ap()
    hb_s = nc.dram_tensor("hb_s", [NCH, 128, D], BF16, kind="Internal").ap()
    x1_s = nc.dram_tensor("x1_s", [L, D], F32, kind="Internal").ap()
    hx2_s = nc.dram_tensor("hx2_s", [L, D], BF16, kind="Internal").ap()
    y_s = nc.dram_tensor("y_s", [L, D], F32, kind="Internal").ap()
    g2_s = nc.dram_tensor("g2_s", [128, D], F32, kind="Internal").ap()
    win16 = nc.dram_tensor("win16", [D, PROJ], BF16, kind="Internal").ap()
    wssd16 = nc.dram_tensor("wssd16", [D, D], BF16, kind="Internal").ap()
    wsc16 = nc.dram_tensor("wsc16", [D, D], BF16, kind="Internal").ap()
    wo16 = nc.dram_tensor("wo16", [D, D], BF16, kind="Internal").ap()
    dbg_d = {}

    def dbg_out(name, shape, dt=F32):
        dbg_d[name] = nc.dram_tensor(name, list(shape), dt, kind="ExternalOutput").ap()
        return dbg_d[name]

    es_all = contextlib.ExitStack()
    with es_all:
        def mk_alloc(es):
            def S(name, shape, dt=F32):
                return T(name, es.enter_context(nc.sbuf_tensor("sb_" + name, list(shape), dt)))
            return S
        SP = mk_alloc(es_all)
        psum = es_all.enter_context(nc.psum_tensor("psum", [128, 4096], F32))
        PB = [T("pb%d" % i, psum[:, i * 512:(i + 1) * 512]) for i in range(8)]

        def pbf(i, n=1):
            return psum[:, i * 512:(i + n) * 512]

        def pbh(i, n=1):
            return psum[:, i * 512:(i + n) * 512].bitcast(BF16)

        pk = SP("pk", [128, NPK])
        idb = SP("idb", [128, 128], BF16)
        G1 = SP("G1", [128, D])
        A2 = SP("A2", [128, D])
        B2 = SP("B2", [128, D])
        modfm = SP("modfm", [128, 64])
        arep = SP("arep", [128, 64])
        wdt = SP("wdt", [128, 16, 64], BF16)
        affAll = SP("affAll", [128, NCH, NE])
        hf = SP("hf", [128, D])
        h16 = SP("h16", [128, D], BF16)

        def pkc(c0, n):
            return pk.ap[:, c0:c0 + n]
        ident = pkc(PK_ID, 128)
        SU, SL, TLE, TGE, ONES = pkc(PK_SU, 128), pkc(PK_SL, 128), pkc(PK_TLE, 128), pkc(PK_TGE, 128), pkc(PK_ONE, 128)

        k.dma("sp", pk.ap[:], pk_d[:, :], pk, writes=[pk])
        k.dma("sp", idb.ap[:], idb_d[:, :], idb, writes=[idb])
        k.dma("pool", wdt.ap[:], w_in_d[:, C_DT:C_DT + 64].rearrange("(kb p) c -> p kb c", p=128), wdt, writes=[wdt])
        t_wA, t_wB, t_wC = T("winA", win16), T("winB", win16), T("winC", win16)
        t_wssd, t_wsc, t_wo = T("wssd16", wssd16), T("wsc16", wsc16), T("wo16", wo16)

        def cast_region(dst, srcw, c0, c1, piece, rows_per, tt):
            for r0 in range(0, D, rows_per):
                k.dma("pool", dst[r0:r0 + rows_per, c0:c1].rearrange("r (a c) -> r a c", c=piece),
                      srcw[r0:r0 + rows_per, c0:c1].rearrange("r (a c) -> r a c", c=piece), tt, writes=[tt])
        cast_region(win16, w_in_d, C_XBC, C_DT, 1536, 512, t_wA)
        cast_region(win16, w_in_d, C_Z, C_XBC, 2048, 1024, t_wB)
        cast_region(win16, w_in_d, C_SCB, PROJ, 2048, 256, t_wC)
        cast_region(wssd16, w_ssd_d, 0, D, 2048, 1024, t_wssd)
        cast_region(wsc16, w_sc_d, 0, D, 2048, 1024, t_wsc)
        cast_region(wo16, w_o_d, 0, D, 2048, 1024, t_wo)

        with contextlib.ExitStack() as es0:
            S0 = mk_alloc(es0)
            csil = S0("csil", [128, 32])
            crep = S0("crep", [128, 16, 128])
            wm = [S0("wm%d" % i, [128, 16, 512]) for i in range(2)]
            brp = [S0("brp%d" % i, [128, 512]) for i in range(2)]
            nrp = [S0("nrp%d" % i, [128, 512]) for i in range(2)]
            tmp0 = S0("tmp0", [128, 512])
            G2 = S0("G2", [128, D])
            k.op("act", lambda e: e.activation(out=csil.ap[:], in_=pkc(PK_C, 32), func=AF.Silu), reads=[pk], writes=[csil])
            k.op("dve", lambda e: e.tensor_copy(out=crep.ap[:], in_=csil.ap[:, 0:32].rearrange("p (kb w) -> p kb w", w=2)[:, :, 0:1].to_broadcast([128, 16, 128])),
                 reads=[csil], writes=[crep])
            k.op("act", lambda e: e.activation(out=arep.ap[:], in_=pkc(PK_ALOG, 64), func=AF.Exp), reads=[pk], writes=[arep])
            k.op("dve", lambda e: e.tensor_scalar(out=arep.ap[:], in0=arep.ap[:], scalar1=-1.0, scalar2=None, op0=ALU.mult), reads=[arep], writes=[arep])
            ngrp = 24
            for g in range(ngrp):
                w_t = wm[g % 2]
                k.dma("sp", w_t.ap[:], w_mod_d[:, g * 512:(g + 1) * 512].rearrange("(kb p) c -> p kb c", p=128), w_t, writes=[w_t])
                if g < 8:
                    pb = PB[g % 2]
                    for j in range(4):
                        for kb in range(16):
                            k.op("pe", lambda e: e.matmul(out=pb.ap[:, 2 * j:2 * j + 2], lhsT=w_t.ap[:, kb, j * 128:(j + 1) * 128],
                                                          rhs=csil.ap[:, 2 * kb:2 * kb + 2], start=(kb == 0), stop=(kb == 15)),
                                 reads=[w_t, csil], writes=[pb])
                    db0 = (g % 4) * 4
                    boff = PK_BMOD + (0 if g < 4 else 16) + db0
                    for which in range(2):
                        dst0 = which * 32 + (16 if g < 4 else 0) + db0
                        k.op("dve", lambda e: e.tensor_tensor(out=modfm.ap[:, dst0:dst0 + 4],
                                                              in0=pb.ap[:, 0:8].rearrange("p (j w) -> p w j", w=2)[:, which, :],
                                                              in1=pk.ap[:, boff:boff + 4], op=ALU.add),
                             reads=[pb, pk], writes=[modfm])
                else:
                    gi = g - 8
                    ty, cg = gi // 4, gi % 4
                    b_t, n_t = brp[g % 2], nrp[g % 2]
                    k.dma("sp", b_t.ap[:], reps_d[:, 3 * D + gi * 512: 3 * D + (gi + 1) * 512], b_t, writes=[b_t])
                    if ty != 1:
                        nsel = {0: 0, 2: 1, 3: 2}[ty]
                        k.dma("sp", n_t.ap[:], reps_d[:, nsel * D + cg * 512: nsel * D + (cg + 1) * 512], n_t, writes=[n_t])
                    pb = PB[2 + g % 2]
                    for kb in range(16):
                        k.op("pe", lambda e: e.matmul(out=pb.ap[:, :], lhsT=crep.ap[:, kb, :], rhs=w_t.ap[:, kb, :], start=(kb == 0), stop=(kb == 15)),
                             reads=[w_t, crep], writes=[pb])
                    dst = {0: G1, 1: B2, 2: A2, 3: G2}[ty]
                    dsl = dst.ap[:, cg * 512:(cg + 1) * 512]
                    if ty == 1:
                        k.op("dve", lambda e: e.tensor_tensor(out=dsl, in0=pb.ap[:, :], in1=b_t.ap[:], op=ALU.add), reads=[pb, b_t], writes=[dst])
                    else:
                        k.op("dve", lambda e: e.tensor_tensor(out=tmp0.ap[:], in0=pb.ap[:, :], in1=b_t.ap[:], op=ALU.add), reads=[pb, b_t], writes=[tmp0])
                        if ty == 2:
                            k.op("dve", lambda e: e.scalar_tensor_tensor(out=dsl, in0=tmp0.ap[:], scalar=1.0, in1=n_t.ap[:], op0=ALU.add, op1=ALU.mult),
                                 reads=[tmp0, n_t], writes=[dst])
                        else:
                            k.op("dve", lambda e: e.tensor_tensor(out=dsl, in0=tmp0.ap[:], in1=n_t.ap[:], op=ALU.mult), reads=[tmp0, n_t], writes=[dst])
            for which in range(2):
                k.op("dve", lambda e: e.scalar_tensor_tensor(out=modfm.ap[:, which * 32:which * 32 + 16], in0=modfm.ap[:, which * 32:which * 32 + 16],
                                                             scalar=1.0, in1=pkc(PK_NPM, 16), op0=ALU.add, op1=ALU.mult),
                     reads=[modfm, pk], writes=[modfm])
            k.dma("sp", g2_s[:, :], G2.ap[:], G2, reads=[G2])
            k.barrier()
        if dbg and stage == 0:
            o1 = dbg_out("d_modfm", [128, 64])
            o2 = dbg_out("d_G1", [128, D])
            o3 = dbg_out("d_A2", [128, D])
            k.dma("sp", o1[:, :], modfm.ap[:], modfm, reads=[modfm])
            k.dma("sp", o2[:, :], G1.ap[:], G1, reads=[G1])
            k.dma("sp", o3[:, :], A2.ap[:], A2, reads=[A2])
        if stage == 0:
            k.barrier()
            return nc, dbg_d

        esM = contextlib.ExitStack()
        with esM:
            SM = mk_alloc(esM)
            NQ = TS // 128
            xc1 = SM("xc", [128, D])
            xc = [xc1, xc1]
            xn = [xc1, xc1]
            stat = SM("stat", [128, 16])
            hxT = SM("hxT", [128, 16, TS], BF16)
            wst = [SM("wst%d" % i, [128, 16, 256], BF16) for i in range(3)]
            xsT = SM("xsT", [128, 16, TS], BF16)
            BT = SM("BT", [128, 4, TS], BF16)
            CT = SM("CT", [128, 4, TS], BF16)
            ctmp = [SM("ctmp%d" % i, [128, TS]) for i in range(2)]
            zs = SM("zs", [128, NQ, D], BF16)
            cv = SM("cv", [128, 16, TS])
            scyT = SM("scyT", [128, 16, TS], BF16)
            gcT = SM("gcT", [128, 16, TS], BF16)
            ynT = SM("ynT", [128, 16, TS], BF16)
            xs_tok = [SM("xs_tok%d" % i, [128, D], BF16) for i in range(NQ)]
            B_tok = [SM("B_tok%d" % i, [128, 512], BF16) for i in range(NQ)]
            dtt = [SM("dtt%d" % i, [128, 64]) for i in range(NQ)]
            dta = [SM("dta%d" % i, [128, 64]) for i in range(NQ)]
            sm = SM("sm", [128, 128])
            wgt = SM("wgt", [128, 32])
            xdt_f = SM("xdt_f", [128, D], BF16)
            xdt_b = SM("xdt_b", [128, D], BF16)
            xD = SM("xD", [128, D], BF16)
            Rf = SM("Rf", [128, 1024])
            Rb = SM("Rb", [128, 1024])
            Lf = SM("Lf", [128, 1024], BF16)
            Lb = SM("Lb", [128, 1024], BF16)
            cbm = SM("cbm", [128, 256], BF16)
            yt1 = SM("yt1", [128, 512])
            yt2 = SM("yt2", [128, 512])
            yz = SM("yz", [128, D])
            yzn = SM("yzn", [128, D], BF16)
            hb1 = SM("hb16", [128, D], BF16)
            hb16 = [hb1, hb1]
            lg = SM("lg", [128, 32])
            mergedT = T("mergedT", xsT.ap, root=xsT)
            mtmp = T("mtmp", cv.ap, root=cv)
            outx = T("outx", yz.ap, root=yz)
            x1buf = T("x1buf", cv.ap[:, 0:8, :].rearrange("p a b -> p (a b)"), root=cv)
            xw = T("xw", xdt_f.ap, root=xdt_f)
            hx2 = T("hx2", xdt_b.ap, root=xdt_b)
            junk = T("junk", yzn.ap, root=yzn)
            Mf = T("Mf", Lf.ap, root=Lf)
            Mb = T("Mb", Lb.ap, root=Lb)

            wctr = [0]

            def wload(src_ap, treg):
                t = wst[wctr[0] % 3]
                wctr[0] += 1
                k.dma("sp", t.ap[:], src_ap.rearrange("(kb p) c -> p kb c", p=128), t, reads=[treg], writes=[t])
                return t

            pbctr = [0]

            def next_pb(lo=0, n=8):
                b = lo + pbctr[0] % n
                pbctr[0] += 1
                return b

            def gen_proj_fm(wsrc, col0, nblk, rhsT, tt, evac, treg=None, banks=None):
                for g0 in range(0, nblk, 2):
                    t = wload(wsrc[:, col0 + g0 * 128: col0 + g0 * 128 + 256], treg)
                    for j in range(2):
                        m = g0 + j
                        if banks is None:
                            b = next_pb()
                        else:
                            b = banks[pbctr[0] % len(banks)]
                            pbctr[0] += 1
                        for kb in range(16):
                            k.op("pe", lambda e: e.matmul(out=PB[b].ap[:, 0:tt], lhsT=t.ap[:, kb, j * 128:(j + 1) * 128], rhs=rhsT.ap[:, kb, 0:tt],
                                                          start=(kb == 0), stop=(kb == 15)), reads=[t, rhsT], writes=[PB[b]])
                        evac(m, PB[b])
                        yield

            def proj_fm(*a, **kw):
                for _ in gen_proj_fm(*a, **kw):
                    pass

            def pull(gen, n):
                for _ in range(n):
                    try:
                        next(gen)
                    except StopIteration:
                        return

            def prep_chunk(src_rows, q, a_off):
                xt, xnt = xc[q % 2], xn[q % 2]
                k.dma("sp", xt.ap[:], src_rows, xt, writes=[xt])
                k.op("act", lambda e: e.activation(out=junk.ap[:], in_=xt.ap[:], func=AF.Square, accum_out=stat.ap[:, 0:1]), reads=[xt], writes=[junk, stat])
                k.op("act", lambda e: e.activation(out=stat.ap[:, 1:2], in_=stat.ap[:, 0:1], func=AF.Sqrt, scale=1.0 / D, bias=EPS), reads=[stat], writes=[stat])
                k.op("dve", lambda e: e.reciprocal(out=stat.ap[:, 2:3], in_=stat.ap[:, 1:2]), reads=[stat], writes=[stat])
                k.op("act", lambda e: e.activation(out=xnt.ap[:], in_=xt.ap[:], func=AF.Copy, scale=stat.ap[:, 2:3]), reads=[xt, stat], writes=[xnt])
                b0 = 4 * (q % 2)
                for kb in range(16):
                    b = b0 + kb // 4
                    k.op("pe", lambda e: e.transpose(out=PB[b].ap[:, (kb % 4) * 128:(kb % 4 + 1) * 128], in_=xnt.ap[:, kb * 128:(kb + 1) * 128], identity=ident),
                         reads=[xnt, pk], writes=[PB[b]])
                for kb in range(16):
                    b = b0 + kb // 4
                    k.op("act", lambda e: e.activation(out=hxT.ap[:, kb, q * 128:(q + 1) * 128], in_=PB[b].ap[:, (kb % 4) * 128:(kb % 4 + 1) * 128], func=AF.Identity,
                                                       scale=modfm.ap[:, a_off + kb:a_off + kb + 1], bias=modfm.ap[:, a_off + 16 + kb:a_off + 17 + kb]),
                         reads=[PB[b], modfm], writes=[hxT])

            def conv_evac(dstT, blk0, tt, rl):
                nr = tt // rl

                def f(m, pb):
                    cb = blk0 + m
                    ct = ctmp[m % 2]
                    w0 = pk.ap[:, PK_CW + cb:PK_CW + cb + 1]
                    w1 = pk.ap[:, PK_CW + 24 + cb:PK_CW + 25 + cb]
                    w2 = pk.ap[:, PK_CW + 48 + cb:PK_CW + 49 + cb]
                    bb = pk.ap[:, PK_CB + cb:PK_CB + cb + 1]
                    k.op("act", lambda e: e.activation(out=ct.ap[:, 0:tt], in_=pb.ap[:, 0:tt], func=AF.Identity, scale=w1, bias=bb), reads=[pb, pk], writes=[ct])
                    pv = pb.ap[:, 0:tt].rearrange("p (r c) -> p r c", c=rl)
                    cvw = ct.ap[:, 0:tt].rearrange("p (r c) -> p r c", c=rl)
                    k.op("dve", lambda e: e.scalar_tensor_tensor(out=cvw[:, :, 1:rl], in0=pv[:, :, 0:rl - 1], scalar=w0, in1=cvw[:, :, 1:rl], op0=ALU.mult, op1=ALU.add),
                         reads=[pb, ct, pk], writes=[ct])
                    k.op("dve", lambda e: e.scalar_tensor_tensor(out=cvw[:, :, 0:rl - 1], in0=pv[:, :, 1:rl], scalar=w2, in1=cvw[:, :, 0:rl - 1], op0=ALU.mult, op1=ALU.add),
                         reads=[pb, ct, pk], writes=[ct])
                    if cb < 16:
                        dst, di = dstT[0], cb
                    elif cb < 20:
                        dst, di = dstT[1], cb - 16
                    else:
                        dst, di = dstT[2], cb - 20
                    k.op("act", lambda e: e.activation(out=dst.ap[:, di, 0:tt], in_=ct.ap[:, 0:tt], func=AF.Silu), reads=[ct], writes=[dst])
                return f

            def dt_chunk(q):
                pb = PB[0]
                for kb in range(16):
                    k.op("pe", lambda e: e.matmul(out=pb.ap[:, 0:64], lhsT=hxT.ap[:, kb, q * 128:(q + 1) * 128], rhs=wdt.ap[:, kb, :], start=(kb == 0), stop=(kb == 15)),
                         reads=[hxT, wdt], writes=[pb])
                k.op("dve", lambda e: e.tensor_tensor(out=dtt[q].ap[:], in0=pb.ap[:, 0:64], in1=pkc(PK_DTB, 64), op=ALU.add), reads=[pb, pk], writes=[dtt[q]])
                k.op("act", lambda e: e.activation(out=dtt[q].ap[:], in_=dtt[q].ap[:], func=AF.Exp), reads=[dtt[q]], writes=[dtt[q]])
                k.op("act", lambda e: e.activation(out=dtt[q].ap[:], in_=dtt[q].ap[:], func=AF.Ln, bias=1.0), reads=[dtt[q]], writes=[dtt[q]])
                k.op("dve", lambda e: e.tensor_tensor(out=dta[q].ap[:], in0=dtt[q].ap[:], in1=arep.ap[:], op=ALU.mult), reads=[dtt[q], arep], writes=[dta[q]])

            def to_tok(q, with_b=True):
                for half in range(2):
                    b = 4 + 2 * half
                    for i in range(8):
                        cbk = half * 8 + i
                        bb = b + i // 4
                        k.op("pe", lambda e: e.transpose(out=pbh(bb)[:, (i % 4) * 128:(i % 4 + 1) * 128], in_=xsT.ap[:, cbk, q * 128:(q + 1) * 128], identity=idb.ap[:]),
                             reads=[xsT, idb], writes=[PB[bb]])
                for half in range(2):
                    b = 4 + 2 * half
                    src = psum[:, b * 512:(b + 2) * 512].bitcast(BF16).rearrange("p (b x c) -> p b x c", b=2, x=2)[:, :, 0, :]
                    k.op("act" if half == 0 else "dve",
                         (lambda e: e.activation(out=xs_tok[q].ap[:, half * 1024:(half + 1) * 1024].rearrange("p (b c) -> p b c", b=2), in_=src, func=AF.Copy)) if half == 0 else
                         (lambda e: e.tensor_copy(out=xs_tok[q].ap[:, half * 1024:(half + 1) * 1024].rearrange("p (b c) -> p b c", b=2), in_=src)),
                         reads=[PB[b], PB[b + 1]], writes=[xs_tok[q]])
                if with_b:
                    for g in range(4):
                        k.op("pe", lambda e: e.transpose(out=pbh(3)[:, g * 128:(g + 1) * 128], in_=BT.ap[:, g, q * 128:(q + 1) * 128], identity=idb.ap[:]),
                             reads=[BT, idb], writes=[PB[3]])
                    k.op("dve", lambda e: e.tensor_copy(out=B_tok[q].ap[:], in_=pbh(3)[:, 0:512]), reads=[PB[3]], writes=[B_tok[q]])

            def state_update(q, dr, smt, wcol, dcol):
                k.op("dve", lambda e: e.tensor_tensor(out=wgt.ap[:], in0=smt.ap[:, wcol:wcol + 32], in1=dtt[q].ap[:, dr * 32:(dr + 1) * 32], op=ALU.mult),
                     reads=[smt, dtt[q]], writes=[wgt])
                k.op("pool", lambda e: e.tensor_tensor(out=xw.ap[:].rearrange("p (h c) -> p h c", c=64), in0=xs_tok[q].ap[:].rearrange("p (h c) -> p h c", c=64),
                                                       in1=wgt.ap[:, 0:32].unsqueeze(2).to_broadcast([128, 32, 64]), op=ALU.mult),
                     reads=[xs_tok[q], wgt], writes=[xw])
                for g in range(4):
                    sb_ = (1, 2, 6, 7)[g]
                    k.op("pe", lambda e: e.matmul(out=PB[sb_].ap[:, :], lhsT=B_tok[q].ap[:, g * 128:(g + 1) * 128], rhs=xw.ap[:, g * 512:(g + 1) * 512], start=True, stop=True),
                         reads=[B_tok[q], xw], writes=[PB[sb_]])
                k.op("dve", lambda e: e.tensor_tensor(out=hf.ap[:].rearrange("p (h c) -> p h c", c=64), in0=hf.ap[:].rearrange("p (h c) -> p h c", c=64),
                                                      in1=smt.ap[:, dcol:dcol + 32].unsqueeze(2).to_broadcast([128, 32, 64]), op=ALU.mult),
                     reads=[hf, smt], writes=[hf])
                k.op("dve", lambda e: e.tensor_tensor(out=hf.ap[:, 0:1024], in0=hf.ap[:, 0:1024], in1=pbf(1, 2), op=ALU.add), reads=[hf, PB[1], PB[2]], writes=[hf])
                k.op("dve", lambda e: e.tensor_tensor(out=hf.ap[:, 1024:2048], in0=hf.ap[:, 1024:2048], in1=pbf(6, 2), op=ALU.add), reads=[hf, PB[6], PB[7]], writes=[hf])
                k.op("act", lambda e: e.activation(out=h16.ap[:], in_=hf.ap[:], func=AF.Copy), reads=[hf], writes=[h16])

            def small_state(q, dr):
                tri = SU if dr == 0 else SL
                k.op("pe", lambda e: e.matmul(out=PB[0].ap[:, 0:32], lhsT=tri, rhs=dta[q].ap[:, dr * 32:(dr + 1) * 32], start=True, stop=True), reads=[pk, dta[q]], writes=[PB[0]])
                k.op("pe", lambda e: e.matmul(out=PB[0].ap[:, 32:64], lhsT=ONES, rhs=dta[q].ap[:, dr * 32:(dr + 1) * 32], start=True, stop=True), reads=[pk, dta[q]], writes=[PB[0]])
                k.op("act", lambda e: e.activation(out=sm.ap[:, 0:64], in_=PB[0].ap[:, 0:64], func=AF.Exp), reads=[PB[0]], writes=[sm])

            def states_superchunk(src_d, row0, tt, a_off, rl, dr, chunk_order, store_c0=None):
                nq = tt // 128
                for q in range(nq):
                    prep_chunk(src_d[row0 + q * 128: row0 + (q + 1) * 128, :], q, a_off)
                proj_fm(win16, C_XBC, 20, hxT, tt, conv_evac((xsT, BT, CT), 0, tt, rl), t_wA)
                for q in range(nq):
                    dt_chunk(q)
                    to_tok(q)
                for q in chunk_order:
                    if store_c0 is not None:
                        c = store_c0 + q
                        k.dma("sp", hb_s[c, :, :], h16.ap[:], h16, reads=[h16])
                    small_state(q, dr)
                    state_update(q, dr, sm, 0, 32)

            def zero_state():
                k.op("dve", lambda e: e.memset(hf.ap[:], 0.0), writes=[hf])
                k.op("dve", lambda e: e.memset(h16.ap[:], 0.0), writes=[h16])

            zero_state()
            states_superchunk(ctx_d, 0, CTXL, 32, CTXL, 1, [1, 0])
            nsc = L // TS
            for s in reversed(range(nsc)):
                states_superchunk(x_d, s * TS, TS, 0, 64, 1, list(reversed(range(NQ))), store_c0=s * NQ)
                if stage == 1 and s == nsc - 2:
                    break
            if dbg and stage == 1:
                o = dbg_out("d_hf", [128, D])
                k.dma("sp", o[:, :], hf.ap[:], hf, reads=[hf])
                o = dbg_out("d_xs", [128, D], BF16)
                k.dma("sp", o[:, :], xs_tok[0].ap[:], xs_tok[0], reads=[xs_tok[0]])
                o = dbg_out("d_dt", [128, 64])
                k.dma("sp", o[:, :], dtt[0].ap[:], dtt[0], reads=[dtt[0]])
            if stage == 1:
                k.barrier()
                return nc, dbg_d

            zero_state()
            states_superchunk(ctx_d, 0, CTXL, 32, CTXL, 0, [0, 1])

            k.op("pool", lambda e: e.memset(yz.ap[:], 0.0), writes=[yz])
            for c in range(NCH):
                k.dma("sp", y_s[c * 128:(c + 1) * 128, :], yz.ap[:], yz, reads=[yz])

            def ssd_chunk(s, q):
                c = s * NQ + q
                hbt = hb16[c % 2]
                k.dma("sp", hbt.ap[:], hb_s[c, :, :], hbt, writes=[hbt])
                cs = slice(q * 128, (q + 1) * 128)
                k.op("pe", lambda e: e.matmul(out=PB[0].ap[:, 0:32], lhsT=SU, rhs=dta[q].ap[:, 0:32], start=True, stop=True), reads=[pk, dta[q]], writes=[PB[0]])
                k.op("pe", lambda e: e.matmul(out=PB[0].ap[:, 32:64], lhsT=ONES, rhs=dta[q].ap[:, 0:32], start=True, stop=True), reads=[pk, dta[q]], writes=[PB[0]])
                k.op("pe", lambda e: e.matmul(out=PB[0].ap[:, 64:96], lhsT=TLE, rhs=dta[q].ap[:, 0:32], start=True, stop=True), reads=[pk, dta[q]], writes=[PB[0]])
                k.op("pe", lambda e: e.matmul(out=PB[0].ap[:, 96:128], lhsT=TGE, rhs=dta[q].ap[:, 32:64], start=True, stop=True), reads=[pk, dta[q]], writes=[PB[0]])
                k.op("act", lambda e: e.activation(out=sm.ap[:, 0:128], in_=PB[0].ap[:, 0:128], func=AF.Exp), reads=[PB[0]], writes=[sm])
                x3 = xs_tok[q].ap[:].rearrange("p (h c) -> p h c", c=64)

                def bc(t, c0):
                    return t.ap[:, c0:c0 + 32].unsqueeze(2).to_broadcast([128, 32, 64])
                k.op("pool", lambda e: e.tensor_tensor(out=xdt_f.ap[:].rearrange("p (h c) -> p h c", c=64), in0=x3, in1=bc(dtt[q], 0), op=ALU.mult), reads=[xs_tok[q], dtt[q]], writes=[xdt_f])
                k.op("pool", lambda e: e.tensor_tensor(out=xdt_b.ap[:].rearrange("p (h c) -> p h c", c=64), in0=x3, in1=bc(dtt[q], 32), op=ALU.mult), reads=[xs_tok[q], dtt[q]], writes=[xdt_b])
                k.op("pool", lambda e: e.tensor_tensor(out=xD.ap[:].rearrange("p (h c) -> p h c", c=64), in0=x3, in1=bc(pk, PK_SSD_D), op=ALU.mult), reads=[xs_tok[q], pk], writes=[xD])
                for g in range(4):
                    co = (g % 2) * 128
                    k.op("pe", lambda e: e.matmul(out=PB[0].ap[:, 128 + co:256 + co], lhsT=BT.ap[:, g, cs], rhs=CT.ap[:, g, cs], start=True, stop=True), reads=[BT, CT], writes=[PB[0]])
                    k.op("dve", lambda e: e.tensor_tensor(out=cbm.ap[:, 0:128], in0=PB[0].ap[:, 128 + co:256 + co], in1=TLE, op=ALU.mult), reads=[PB[0], pk], writes=[cbm])
                    k.op("dve", lambda e: e.tensor_tensor(out=cbm.ap[:, 128:256], in0=PB[0].ap[:, 128 + co:256 + co], in1=TGE, op=ALU.mult), reads=[PB[0], pk], writes=[cbm])
                    for (R, tri, dc) in ((Rf, TLE, 0), (Rb, TGE, 32)):
                        k.op("pool", lambda e: e.tensor_tensor(out=R.ap[:].rearrange("p (h i) -> p h i", i=128), in0=tri.unsqueeze(1).to_broadcast([128, 8, 128]),
                                                               in1=dta[q].ap[:, dc + g * 8:dc + g * 8 + 8].unsqueeze(2).to_broadcast([128, 8, 128]), op=ALU.mult),
                             reads=[pk, dta[q]], writes=[R])
                    for (R, trl, b0, Lt, Mt, mc) in ((Rf, SU, 1, Lf, Mf, 0), (Rb, SL, 1, Lb, Mb, 128)):
                        for hh in range(2):
                            k.op("pe", lambda e: e.matmul(out=PB[b0 + hh].ap[:, :], lhsT=trl, rhs=R.ap[:, hh * 512:(hh + 1) * 512], start=True, stop=True), reads=[pk, R], writes=[PB[b0 + hh]])
                        k.op("act", lambda e: e.activation(out=Lt.ap[:], in_=pbf(b0, 2), func=AF.Exp), reads=[PB[b0], PB[b0 + 1]], writes=[Lt])
                        k.op("dve", lambda e: e.tensor_tensor(out=Mt.ap[:].rearrange("p (h i) -> p h i", i=128), in0=Lt.ap[:].rearrange("p (h i) -> p h i", i=128),
                                                              in1=cbm.ap[:, mc:mc + 128].unsqueeze(1).to_broadcast([128, 8, 128]), op=ALU.mult),
                             reads=[Lt, cbm], writes=[Mt])
                    k.op("pe", lambda e: e.matmul(out=PB[5].ap[:, :], lhsT=idb.ap[:], rhs=xD.ap[:, g * 512:(g + 1) * 512], start=True, stop=False), reads=[idb, xD], writes=[PB[5]])
                    for (Mt, xdt) in ((Mf, xdt_f), (Mb, xdt_b)):
                        for h in range(8):
                            last = (Mt is Mb) and h == 7
                            k.op("pe", lambda e: e.matmul(out=PB[5].ap[:, h * 64:(h + 1) * 64], lhsT=Mt.ap[:, h * 128:(h + 1) * 128],
                                                          rhs=xdt.ap[:, (g * 8 + h) * 64:(g * 8 + h + 1) * 64], start=False, stop=last), reads=[Mt, xdt], writes=[PB[5]])
                    k.op("pe", lambda e: e.matmul(out=PB[6].ap[:, :], lhsT=CT.ap[:, g, cs], rhs=h16.ap[:, g * 512:(g + 1) * 512], start=True, stop=True), reads=[CT, h16], writes=[PB[6]])
                    k.op("pe", lambda e: e.matmul(out=PB[7].ap[:, :], lhsT=CT.ap[:, g, cs], rhs=hbt.ap[:, g * 512:(g + 1) * 512], start=True, stop=True), reads=[CT, hbt], writes=[PB[7]])

                    def bc8(c0):
                        return sm.ap[:, c0 + g * 8:c0 + g * 8 + 8].unsqueeze(2).to_broadcast([128, 8, 64])
                    k.op("dve", lambda e: e.tensor_tensor(out=yt1.ap[:].rearrange("p (h c) -> p h c", c=64), in0=PB[6].ap[:, :].rearrange("p (h c) -> p h c", c=64), in1=bc8(64), op=ALU.mult),
                         reads=[PB[6], sm], writes=[yt1])
                    k.op("dve", lambda e: e.tensor_tensor(out=yt2.ap[:].rearrange("p (h c) -> p h c", c=64), in0=PB[7].ap[:, :].rearrange("p (h c) -> p h c", c=64), in1=bc8(96), op=ALU.mult),
                         reads=[PB[7], sm], writes=[yt2])
                    k.op("pool", lambda e: e.tensor_tensor(out=yt1.ap[:], in0=yt1.ap[:], in1=yt2.ap[:], op=ALU.add), reads=[yt1, yt2], writes=[yt1])
                    k.op("dve", lambda e: e.tensor_tensor(out=yt1.ap[:], in0=PB[5].ap[:, :], in1=yt1.ap[:], op=ALU.add), reads=[PB[5], yt1], writes=[yt1])
                    k.op("pool", lambda e: e.tensor_tensor(out=yz.ap[:, g * 512:(g + 1) * 512], in0=yt1.ap[:], in1=zs.ap[:, q, g * 512:(g + 1) * 512], op=ALU.mult), reads=[yt1, zs], writes=[yz])
                    k.op("act", lambda e: e.activation(out=junk.ap[:, 0:512], in_=yz.ap[:, g * 512:(g + 1) * 512], func=AF.Square, accum_out=stat.ap[:, 4 + g:5 + g]), reads=[yz], writes=[junk, stat])
                    yield
                k.op("act", lambda e: e.activation(out=stat.ap[:, 8:12], in_=stat.ap[:, 4:8], func=AF.Sqrt, scale=1.0 / 512, bias=EPS), reads=[stat], writes=[stat])
                k.op("dve", lambda e: e.reciprocal(out=stat.ap[:, 12:16], in_=stat.ap[:, 8:12]), reads=[stat], writes=[stat])
                k.op("dve", lambda e: e.tensor_tensor(out=yzn.ap[:].rearrange("p (g c) -> p g c", c=512), in0=yz.ap[:].rearrange("p (g c) -> p g c", c=512),
                                                      in1=stat.ap[:, 12:16].unsqueeze(2).to_broadcast([128, 4, 512]), op=ALU.mult), reads=[yz, stat], writes=[yzn])
                for kb in range(16):
                    bb = 6 + kb // 8
                    k.op("pe", lambda e: e.transpose(out=pbh(bb)[:, (kb % 8) * 128:(kb % 8 + 1) * 128], in_=yzn.ap[:, kb * 128:(kb + 1) * 128], identity=idb.ap[:]), reads=[yzn, idb], writes=[PB[bb]])
                for kb in range(16):
                    bb = 6 + kb // 8
                    k.op("act", lambda e: e.activation(out=ynT.ap[:, kb, cs], in_=pbh(bb)[:, (kb % 8) * 128:(kb % 8 + 1) * 128], func=AF.Copy, scale=pk.ap[:, PK_SNW + kb:PK_SNW + kb + 1]),
                         reads=[PB[bb], pk], writes=[ynT])
                state_update(q, 0, sm, 0, 32)

            for q in range(NQ):
                prep_chunk(x_d[q * 128:(q + 1) * 128, :], q, 0)
            pending_tail = None
            outx1 = T("outx1", zs.ap[:].rearrange("p a b -> p (a b)").bitcast(F32), root=zs)
            outxs = [outx, outx1]
            for s in range(nsc):
                xg = gen_proj_fm(win16, C_XBC, 24, hxT, TS, conv_evac((xsT, BT, CT), 0, TS, 64), t_wA, [5, 6, 7] if pending_tail is not None else None)
                if pending_tail is not None:
                    a_live, b_live = True, True
                    while a_live or b_live:
                        if a_live:
                            try:
                                next(xg)
                            except StopIteration:
                                a_live = False
                        if b_live:
                            try:
                                next(pending_tail)
                            except StopIteration:
                                b_live = False
                else:
                    for _ in xg:
                        pass
                for g4 in range(8):
                    t = wload(win16[:, C_Z + g4 * 256: C_Z + (g4 + 1) * 256], t_wB)
                    for q in range(NQ):
                        b = next_pb()
                        for kb in range(16):
                            k.op("pe", lambda e: e.matmul(out=PB[b].ap[:, 0:256], lhsT=hxT.ap[:, kb, q * 128:(q + 1) * 128], rhs=t.ap[:, kb, :], start=(kb == 0), stop=(kb == 15)),
                                 reads=[hxT, t], writes=[PB[b]])
                        k.op("act", lambda e: e.activation(out=zs.ap[:, q, g4 * 256:(g4 + 1) * 256], in_=PB[b].ap[:, 0:256], func=AF.Silu), reads=[PB[b]], writes=[zs])
                for q in range(NQ):
                    dt_chunk(q)
                    to_tok(q)
                def ev_c(m, pb):
                    k.op("act", lambda e: e.activation(out=cv.ap[:, m, :], in_=pb.ap[:, 0:TS], func=AF.Copy), reads=[pb], writes=[cv])

                def ev_v(m, pb):
                    ct = ctmp[m % 2]
                    k.op("dve", lambda e: e.tensor_tensor(out=ct.ap[:], in0=pb.ap[:, 0:TS], in1=cv.ap[:, m, :], op=ALU.mult), reads=[pb, cv], writes=[ct])
                    w0 = pk.ap[:, PK_SCW + m:PK_SCW + m + 1]
                    w1 = pk.ap[:, PK_SCW + 16 + m:PK_SCW + 17 + m]
                    w2 = pk.ap[:, PK_SCW + 32 + m:PK_SCW + 33 + m]
                    k.op("act", lambda e: e.activation(out=cv.ap[:, m, :], in_=ct.ap[:], func=AF.Copy, scale=w1), reads=[ct, pk], writes=[cv])
                    c3 = cv.ap[:, m, :].rearrange("p (r c) -> p r c", c=64)
                    t3 = ct.ap[:].rearrange("p (r c) -> p r c", c=64)
                    k.op("dve", lambda e: e.scalar_tensor_tensor(out=c3[:, :, 1:64], in0=t3[:, :, 0:63], scalar=w0, in1=c3[:, :, 1:64], op0=ALU.mult, op1=ALU.add), reads=[ct, cv, pk], writes=[cv])
                    k.op("dve", lambda e: e.scalar_tensor_tensor(out=c3[:, :, 0:63], in0=t3[:, :, 1:64], scalar=w2, in1=c3[:, :, 0:63], op0=ALU.mult, op1=ALU.add), reads=[ct, cv, pk], writes=[cv])

                def ev_b(m, pb):
                    k.op("dve", lambda e: e.tensor_tensor(out=scyT.ap[:, m, :], in0=pb.ap[:, 0:TS], in1=cv.ap[:, m, :], op=ALU.mult), reads=[pb, cv], writes=[scyT])

                def ev_g(dst, boff):
                    def f(m, pb):
                        k.op("act", lambda e: e.activation(out=dst.ap[:, m, :], in_=pb.ap[:, 0:TS], func=AF.Sigmoid, bias=pk.ap[:, PK_BG + boff + m:PK_BG + boff + m + 1]),
                             reads=[pb, pk], writes=[dst])
                    return f

                def filler():
                    FB = [3, 4]
                    yield from gen_proj_fm(win16, C_SCC, 16, hxT, TS, ev_c, t_wC, FB)
                    yield from gen_proj_fm(win16, C_SCV, 16, hxT, TS, ev_v, t_wC, FB)
                    yield from gen_proj_fm(win16, C_SCB, 16, hxT, TS, ev_b, t_wC, FB)
                    yield from gen_proj_fm(win16, C_GS, 16, hxT, TS, ev_g(mtmp, 0), t_wC, FB)
                fill = filler()
                for q in range(NQ):
                    for _ in ssd_chunk(s, q):
                        pull(fill, 8)
                for _ in fill:
                    pass
                if dbg and stage == 2 and s == 0:
                    o = dbg_out("d_yz", [128, D])
                    k.dma("sp", o[:, :], yz.ap[:], yz, reads=[yz])
                    o = dbg_out("d_ynT", [128, 16 * TS], BF16)
                    k.dma("sp", o[:, :], ynT.ap[:].rearrange("p a b -> p (a b)"), ynT, reads=[ynT])
                    k.barrier()
                    return nc, dbg_d
                def ev_o1(m, pb):
                    k.op("dve", lambda e: e.tensor_tensor(out=mtmp.ap[:, m, :], in0=pb.ap[:, 0:TS], in1=mtmp.ap[:, m, :], op=ALU.mult), reads=[pb, mtmp], writes=[mtmp])
                proj_fm(wssd16, 0, 16, ynT, TS, ev_o1, t_wssd)
                proj_fm(win16, C_GC, 16, hxT, TS, ev_g(gcT, 16), t_wC)

                def ev_o2(m, pb):
                    ct = ctmp[m % 2]
                    k.op("dve", lambda e: e.tensor_tensor(out=ct.ap[:], in0=pb.ap[:, 0:TS], in1=gcT.ap[:, m, :], op=ALU.mult), reads=[pb, gcT], writes=[ct])
                    k.op("pool", lambda e: e.tensor_tensor(out=mergedT.ap[:, m, :], in0=ct.ap[:], in1=mtmp.ap[:, m, :], op=ALU.add), reads=[ct, mtmp], writes=[mergedT])
                proj_fm(wsc16, 0, 16, scyT, TS, ev_o2, t_wsc)

                if s + 1 < nsc:
                    for q in range(NQ):
                        prep_chunk(x_d[(s + 1) * TS + q * 128: (s + 1) * TS + (q + 1) * 128, :], q, 0)
                for q in range(NQ):
                    for g4 in range(8):
                        t = wload(wo16[:, g4 * 256:(g4 + 1) * 256], t_wo)
                        b = next_pb()
                        for kb in range(16):
                            k.op("pe", lambda e: e.matmul(out=PB[b].ap[:, 0:256], lhsT=mergedT.ap[:, kb, q * 128:(q + 1) * 128], rhs=t.ap[:, kb, :], start=(kb == 0), stop=(kb == 15)),
                                 reads=[mergedT, t], writes=[PB[b]])
                        k.op("act", lambda e: e.activation(out=outxs[q].ap[:, g4 * 256:(g4 + 1) * 256], in_=PB[b].ap[:, 0:256], func=AF.Copy), reads=[PB[b]], writes=[outxs[q]])

                def tail_gen(s=s):
                    for q in range(NQ):
                        c = s * NQ + q
                        ox = outxs[q]
                        xt = xc[0]
                        x1t = x1buf
                        k.dma("sp", xt.ap[:], x_d[c * 128:(c + 1) * 128, :], xt, writes=[xt])
                        k.op("act", lambda e: e.activation(out=junk.ap[:], in_=ox.ap[:], func=AF.Square, accum_out=stat.ap[:, 0:1]), reads=[ox], writes=[junk, stat])
                        k.op("act", lambda e: e.activation(out=stat.ap[:, 1:2], in_=stat.ap[:, 0:1], func=AF.Sqrt, scale=1.0 / D, bias=EPS), reads=[stat], writes=[stat])
                        k.op("dve", lambda e: e.reciprocal(out=stat.ap[:, 2:3], in_=stat.ap[:, 1:2]), reads=[stat], writes=[stat])
                        yield
                        k.op("dve", lambda e: e.tensor_tensor(out=ox.ap[:], in0=ox.ap[:], in1=G1.ap[:], op=ALU.mult), reads=[ox, G1], writes=[ox])
                        yield
                        k.op("dve", lambda e: e.scalar_tensor_tensor(out=x1t.ap[:], in0=ox.ap[:], scalar=stat.ap[:, 2:3], in1=xt.ap[:], op0=ALU.mult, op1=ALU.add), reads=[ox, stat, xt], writes=[x1t])
                        k.dma("sp", x1_s[c * 128:(c + 1) * 128, :], x1t.ap[:], x1t, reads=[x1t])
                        yield
                        k.op("act", lambda e: e.activation(out=junk.ap[:], in_=x1t.ap[:], func=AF.Square, accum_out=stat.ap[:, 0:1]), reads=[x1t], writes=[junk, stat])
                        k.op("act", lambda e: e.activation(out=stat.ap[:, 1:2], in_=stat.ap[:, 0:1], func=AF.Sqrt, scale=1.0 / D, bias=EPS), reads=[stat], writes=[stat])
                        k.op("dve", lambda e: e.reciprocal(out=stat.ap[:, 2:3], in_=stat.ap[:, 1:2]), reads=[stat], writes=[stat])
                        yield
                        k.op("dve", lambda e: e.scalar_tensor_tensor(out=ox.ap[:], in0=x1t.ap[:], scalar=stat.ap[:, 2:3], in1=A2.ap[:], op0=ALU.mult, op1=ALU.mult), reads=[x1t, stat, A2], writes=[ox])
                        yield
                        k.op("dve", lambda e: e.tensor_tensor(out=ox.ap[:], in0=ox.ap[:], in1=B2.ap[:], op=ALU.add), reads=[ox, B2], writes=[ox])
                        k.op("act", lambda e: e.activation(out=hx2.ap[:], in_=ox.ap[:], func=AF.Copy), reads=[ox], writes=[hx2])
                        k.dma("sp", hx2_s[c * 128:(c + 1) * 128, :], hx2.ap[:], hx2, reads=[hx2])
                        yield
                        for kb in range(16):
                            bb = kb // 4
                            k.op("pe", lambda e: e.transpose(out=PB[bb].ap[:, (kb % 4) * 128:(kb % 4 + 1) * 128], in_=ox.ap[:, kb * 128:(kb + 1) * 128], identity=ident), reads=[ox, pk], writes=[PB[bb]])
                        k.op("act", lambda e: e.activation(out=x1t.ap[:], in_=pbf(0, 4), func=AF.Copy), reads=[PB[0], PB[1], PB[2], PB[3]], writes=[x1t])
                        yield
                        for kb in range(16):
                            k.op("pe", lambda e: e.matmul(out=PB[4].ap[:, 0:16], lhsT=x1t.ap[:, kb * 128:(kb + 1) * 128], rhs=pk.ap[:, PK_WR + kb * 16:PK_WR + (kb + 1) * 16], start=(kb == 0), stop=(kb == 15)),
                                 reads=[x1t, pk], writes=[PB[4]])
                        k.op("dve", lambda e: e.tensor_reduce(out=lg.ap[:, 16:17], in_=PB[4].ap[:, 0:16], op=ALU.max, axis=mybir.AxisListType.X), reads=[PB[4]], writes=[lg])
                        k.op("dve", lambda e: e.tensor_scalar(out=lg.ap[:, 17:18], in0=lg.ap[:, 16:17], scalar1=-1.0, scalar2=None, op0=ALU.mult), reads=[lg], writes=[lg])
                        k.op("act", lambda e: e.activation(out=lg.ap[:, 0:16], in_=PB[4].ap[:, 0:16], func=AF.Exp, bias=lg.ap[:, 17:18], accum_out=lg.ap[:, 18:19]), reads=[PB[4], lg], writes=[lg])
                        k.op("dve", lambda e: e.reciprocal(out=lg.ap[:, 19:20], in_=lg.ap[:, 18:19]), reads=[lg], writes=[lg])
                        k.op("dve", lambda e: e.tensor_scalar(out=affAll.ap[:, c, :], in0=lg.ap[:, 0:16], scalar1=lg.ap[:, 19:20], scalar2=None, op0=ALU.mult), reads=[lg], writes=[affAll])
                        yield
                pending_tail = tail_gen()
                if stage == 2 and s == 1:
                    break
            for _ in pending_tail:
                pass
            if dbg and stage in (2, 3):
                o = dbg_out("d_aff", [128, NCH * NE])
                k.dma("sp", o[:, :], affAll.ap[:].rearrange("p a b -> p (a b)"), affAll, reads=[affAll])
            k.barrier()
        if stage in (2, 3):
            with contextlib.ExitStack() as esd:
                Sd = mk_alloc(esd)
                tt = Sd("tt", [128, D])
                for c in range(NCH):
                    k.dma("sp", tt.ap[:], x1_s[c * 128:(c + 1) * 128, :], tt, writes=[tt])
                    k.dma("sp", out_d[c * 128:(c + 1) * 128, :], tt.ap[:], tt, reads=[tt])
                k.barrier()
            return nc, dbg_d

        RG = [list(range(8))]
        NCOL = NE
        aff_loc = nc.dram_tensor("aff_loc", [NE, L], F32, kind="Internal").ap()
        aff_all = nc.dram_tensor("aff_all", [8 * NE, L], F32, kind="Internal").ap()
        t_hx2all = T("hx2_s", hx2_s)
        t_ys = T("y_s", y_s)
        ccn = [0]

        def allgather(in_ap, out_ap, t_in, t_out):
            if one_core:
                k.dma("pool", out_ap[0:in_ap.shape[0], :], in_ap, t_out, reads=[t_in], writes=[t_out])
                return
            nm = "cc%d" % ccn[0]
            ccn[0] += 1
            k.sems[nm] = nc.alloc_semaphore(nm)
            k._wait("pool", k._deps([t_in], [t_out]))
            ins = nc.gpsimd.collective_compute("AllGather", ALU.bypass, replica_groups=RG, ins=[in_ap], outs=[out_ap])
            ins.then_inc(k.sems[nm], 1)
            k._mark((nm, 1), [t_in], [t_out])


        esX = contextlib.ExitStack()
        es_all.enter_context(esX)
        SX = mk_alloc(esX)
        gidx32 = SX("gidx32", [128, NCOL * 4], I32)
        gval = SX("gval", [128, NCOL * 4])
        osrc32 = SX("osrc32", [128, 64], I32)
        with contextlib.ExitStack() as esE:
            SE = mk_alloc(esE)
            affT = SE("affT", [16, L])
            selT = SE("selT", [NE, L])
            cumT = SE("cumT", [NE, L])
            onesb = SE("onesb", [NE, L], BF16)
            bs = SE("bs", [NE, 8])
            tokA = SE("tokA", [128, 3, NCH * NCOL])
            key = SE("key", [128, NCH * NCOL])
            tv = SE("tv", [128, NCH * NCOL, 5], BF16)
            r1 = SE("r1", [128, NCH * NCOL])
            oh = [SE("oh%d" % i, [128, CAP], BF16) for i in range(2)]
            idxv = SE("idxv", [128, NCOL * 4, 5])
            idxf = SE("idxf", [128, NCOL * 4])
            for c4 in range(NCH // 4):
                for j in range(4):
                    c = c4 * 4 + j
                    k.op("pe", lambda e: e.transpose(out=PB[c4 % 2].ap[0:16, j * 128:(j + 1) * 128], in_=affAll.ap[:, c, :], identity=ident), reads=[affAll, pk], writes=[PB[c4 % 2]])
                k.op("act", lambda e: e.activation(out=affT.ap[:, c4 * 512:(c4 + 1) * 512], in_=PB[c4 % 2].ap[0:16, :], func=AF.Copy), reads=[PB[c4 % 2]], writes=[affT])
            affG = affT
            k.op("dve", lambda e: e.memset(bs.ap[:, 0:1], 0.0), writes=[bs])
            k.op("dve", lambda e: e.memset(bs.ap[:, 1:2], 2.0), writes=[bs])
            k.op("dve", lambda e: e.memset(onesb.ap[:], 1.0), writes=[onesb])
            for it in range(NBIS):
                k.op("dve", lambda e: e.tensor_scalar(out=bs.ap[:, 2:3], in0=bs.ap[:, 0:1], scalar1=bs.ap[:, 1:2], scalar2=0.5, op0=ALU.add, op1=ALU.mult), reads=[bs], writes=[bs])
                k.op("dve", lambda e: e.tensor_scalar(out=selT.ap[:], in0=affG.ap[:], scalar1=bs.ap[:, 2:3], scalar2=None, op0=ALU.is_ge, op1=ALU.add, accum_out=bs.ap[:, 3:4]),
                     reads=[affG, bs], writes=[selT, bs])
                k.op("dve", lambda e: e.tensor_scalar(out=bs.ap[:, 4:5], in0=bs.ap[:, 3:4], scalar1=float(CAP), scalar2=None, op0=ALU.is_ge), reads=[bs], writes=[bs])
                k.op("dve", lambda e: e.scalar_tensor_tensor(out=bs.ap[:, 5:6], in0=bs.ap[:, 4:5], scalar=2.0, in1=bs.ap[:, 2:3], op0=ALU.mult, op1=ALU.add), reads=[bs], writes=[bs])
                k.op("dve", lambda e: e.scalar_tensor_tensor(out=bs.ap[:, 0:1], in0=bs.ap[:, 2:3], scalar=bs.ap[:, 4:5], in1=bs.ap[:, 0:1], op0=ALU.mult, op1=ALU.max), reads=[bs], writes=[bs])
                k.op("dve", lambda e: e.tensor_tensor(out=bs.ap[:, 1:2], in0=bs.ap[:, 1:2], in1=bs.ap[:, 5:6], op=ALU.min), reads=[bs], writes=[bs])
            k.op("dve", lambda e: e.tensor_scalar(out=selT.ap[:], in0=affG.ap[:], scalar1=bs.ap[:, 0:1], scalar2=None, op0=ALU.is_ge), reads=[affG, bs], writes=[selT])
            k.op("dve", lambda e: e.tensor_tensor_scan(out=cumT.ap[:], data0=onesb.ap[:], data1=selT.ap[:], initial=0.0, op0=ALU.mult, op1=ALU.add), reads=[onesb, selT], writes=[cumT])
            k.op("dve", lambda e: e.tensor_tensor(out=affG.ap[:], in0=affG.ap[:], in1=selT.ap[:], op=ALU.mult), reads=[affG, selT], writes=[affG])
            selm = pk.ap[0:NE, PK_SELM:PK_SELM + NCOL]
            for w, srcT in enumerate((cumT, selT, affG)):
                for c in range(NCH):
                    bb = 2 + 2 * w + c // 16
                    oc = (c % 16) * 32
                    k.op("pe", lambda e: e.matmul(out=PB[bb].ap[:, oc:oc + NCOL], lhsT=srcT.ap[:, c * 128:(c + 1) * 128], rhs=selm, start=True, stop=True), reads=[srcT, pk], writes=[PB[bb]])
                k.op("act", lambda e: e.activation(out=tokA.ap[:, w, :].rearrange("p (c x) -> p c x", x=NCOL), in_=pbf(2 + 2 * w, 2).rearrange("p (c x) -> p c x", x=32)[:, :, 0:NCOL], func=AF.Copy), reads=[PB[2 + 2 * w], PB[3 + 2 * w]], writes=[tokA])
            k.op("dve", lambda e: e.tensor_tensor(out=key.ap[:], in0=tokA.ap[:, 0, :], in1=tokA.ap[:, 1, :], op=ALU.mult), reads=[tokA], writes=[key])
            k.op("dve", lambda e: e.tensor_scalar(out=key.ap[:], in0=key.ap[:], scalar1=-1.0, scalar2=None, op0=ALU.add), reads=[key], writes=[key])
            k.op("pool", lambda e: e.iota(tv.ap[:, :, 0].rearrange("p (c e) -> p c e", e=NCOL), pattern=[[1, NCH], [0, NCOL]], base=0, channel_multiplier=0, allow_small_or_imprecise_dtypes=True), writes=[tv])
            k.op("pool", lambda e: e.iota(tv.ap[:, :, 1], pattern=[[0, NCH * NCOL]], base=0, channel_multiplier=1, allow_small_or_imprecise_dtypes=True), writes=[tv])
            k.op("dve", lambda e: e.tensor_copy(out=tv.ap[:, :, 2], in_=tokA.ap[:, 2, :]), reads=[tokA], writes=[tv])
            k.op("dve", lambda e: e.tensor_tensor(out=r1.ap[:], in0=tokA.ap[:, 2, :], in1=tv.ap[:, :, 2], op=ALU.subtract), reads=[tokA, tv], writes=[r1])
            k.op("dve", lambda e: e.tensor_copy(out=tv.ap[:, :, 3], in_=r1.ap[:]), reads=[r1], writes=[tv])
            k.op("dve", lambda e: e.tensor_tensor(out=r1.ap[:], in0=r1.ap[:], in1=tv.ap[:, :, 3], op=ALU.subtract), reads=[r1, tv], writes=[r1])
            k.op("dve", lambda e: e.tensor_copy(out=tv.ap[:, :, 4], in_=r1.ap[:]), reads=[r1], writes=[tv])
            first = True
            n_oh = 0
            for col in range(NCOL):
                for c in range(NCH):
                    o_t = oh[n_oh % 2]
                    n_oh += 1
                    kc = c * NCOL + col
                    k.op("dve", lambda e: e.tensor_scalar(out=o_t.ap[:], in0=pk.ap[:, PK_IOTA:PK_IOTA + CAP], scalar1=key.ap[:, kc:kc + 1], scalar2=None, op0=ALU.is_equal), reads=[pk, key], writes=[o_t])
                    for jb in range(4):
                        oc = (col * 4 + jb) * 5
                        k.op("pe", lambda e: e.matmul(out=PB[0].ap[:, oc:oc + 5], lhsT=o_t.ap[:, jb * 128:(jb + 1) * 128], rhs=tv.ap[:, kc, :], start=first, stop=(col == NCOL - 1 and c == NCH - 1 and jb == 3)),
                             reads=[o_t, tv], writes=[PB[0]])
                        first = False
            k.op("act", lambda e: e.activation(out=idxv.ap[:].rearrange("p a b -> p (a b)"), in_=PB[0].ap[:, 0:NCOL * 20], func=AF.Copy), reads=[PB[0]], writes=[idxv])
            k.op("dve", lambda e: e.scalar_tensor_tensor(out=idxf.ap[:], in0=idxv.ap[:, :, 0], scalar=128.0, in1=idxv.ap[:, :, 1], op0=ALU.mult, op1=ALU.add), reads=[idxv], writes=[idxf])
            k.op("dve", lambda e: e.tensor_copy(out=gidx32.ap[:], in_=idxf.ap[:]), reads=[idxf], writes=[gidx32])
            k.op("dve", lambda e: e.tensor_tensor(out=gval.ap[:], in0=idxv.ap[:, :, 2], in1=idxv.ap[:, :, 3], op=ALU.add), reads=[idxv], writes=[gval])
            k.op("dve", lambda e: e.tensor_tensor(out=gval.ap[:], in0=gval.ap[:], in1=idxv.ap[:, :, 4], op=ALU.add), reads=[gval, idxv], writes=[gval])
            if dbg and stage == 4:
                o = dbg_out("d_idx", [128, NCOL * 4], I32)
                k.dma("sp", o[:, :], gidx32.ap[:], gidx32, reads=[gidx32])
                o = dbg_out("d_gval", [128, NCOL * 4])
                k.dma("sp", o[:, :], gval.ap[:], gval, reads=[gval])
                o = dbg_out("d_bs", [NE, 8])
                k.dma("sp", o[:, :], bs.ap[:], bs, reads=[bs])
                k.barrier()
                return nc, dbg_d
            k.barrier()

        with contextlib.ExitStack() as esF:
            SF = mk_alloc(esF)
            xg = [SF("xg%d" % i, [128, D], BF16) for i in range(4)]
            xgT = SF("xgT", [128, 16, CAP], BF16)
            wq = [SF("wq%d" % i, [128, 16, 256], BF16) for i in range(6)]
            actT = SF("actT", [128, 16, CAP], BF16)
            sg = [SF("sg%d" % i, [128, CAP]) for i in range(2)]
            ost = [SF("ost%d" % i, [128, D], BF16) for i in range(4)]
            wqc = [0]

            def wload2(src_ap):
                t = wq[wqc[0] % 6]
                wqc[0] += 1
                k.dma("pool", t.ap[:], src_ap.rearrange("(kb p) c -> p kb c", p=128), t, writes=[t])
                return t

            pbc = [0]

            def npb():
                b = pbc[0] % 8
                pbc[0] += 1
                return b
            n_e = 2 if (dbg and stage == 5) else NE
            for le in range(n_e):
                for b_ in range(1):
                    col = le
                    for jb in range(4):
                        ic = col * 4 + jb
                        k.dma("pool", None, None, xg[jb], reads=[gidx32, t_hx2all], writes=[xg[jb]],
                              fn=lambda e: e.indirect_dma_start(out=xg[jb].ap[:], out_offset=None, in_=hx2_s[:, :],
                                                                in_offset=bass.IndirectOffsetOnAxis(ap=gidx32.ap[:, ic:ic + 1], axis=0)))
                    for kb in range(16):
                        bb = npb()
                        for jb in range(4):
                            k.op("pe", lambda e: e.transpose(out=pbh(bb)[:, jb * 128:(jb + 1) * 128], in_=xg[jb].ap[:, kb * 128:(kb + 1) * 128], identity=idb.ap[:]), reads=[xg[jb], idb], writes=[PB[bb]])
                        k.op("act" if kb % 2 == 0 else "dve",
                             (lambda e: e.activation(out=xgT.ap[:, kb, :], in_=pbh(bb)[:, 0:CAP], func=AF.Copy)) if kb % 2 == 0 else (lambda e: e.tensor_copy(out=xgT.ap[:, kb, :], in_=pbh(bb)[:, 0:CAP])),
                             reads=[PB[bb]], writes=[xgT])
                    for fg in range(8):
                        w1t = wload2(w_e1_d[le, :, fg * 256:(fg + 1) * 256])
                        w3t = wload2(w_e3_d[le, :, fg * 256:(fg + 1) * 256])
                        for j in range(2):
                            fb = fg * 2 + j
                            bg, bu = npb(), npb()
                            for kb in range(16):
                                k.op("pe", lambda e: e.matmul(out=PB[bg].ap[:, :], lhsT=w1t.ap[:, kb, j * 128:(j + 1) * 128], rhs=xgT.ap[:, kb, :], start=(kb == 0), stop=(kb == 15)), reads=[w1t, xgT], writes=[PB[bg]])
                            for kb in range(16):
                                k.op("pe", lambda e: e.matmul(out=PB[bu].ap[:, :], lhsT=w3t.ap[:, kb, j * 128:(j + 1) * 128], rhs=xgT.ap[:, kb, :], start=(kb == 0), stop=(kb == 15)), reads=[w3t, xgT], writes=[PB[bu]])
                            sgt = sg[fb % 2]
                            k.op("act", lambda e: e.activation(out=sgt.ap[:], in_=PB[bg].ap[:, :], func=AF.Silu), reads=[PB[bg]], writes=[sgt])
                            k.op("dve", lambda e: e.tensor_tensor(out=actT.ap[:, fb, :], in0=PB[bu].ap[:, :], in1=sgt.ap[:], op=ALU.mult), reads=[PB[bu], sgt], writes=[actT])
                    for dg in range(8):
                        w2t = wload2(w_e2_d[le, :, dg * 256:(dg + 1) * 256])
                        for jb in range(4):
                            bo = npb()
                            for fb in range(16):
                                k.op("pe", lambda e: e.matmul(out=PB[bo].ap[:, 0:256], lhsT=actT.ap[:, fb, jb * 128:(jb + 1) * 128], rhs=w2t.ap[:, fb, :], start=(fb == 0), stop=(fb == 15)), reads=[actT, w2t], writes=[PB[bo]])
                            ic = col * 4 + jb
                            k.op("act", lambda e: e.activation(out=ost[jb].ap[:, dg * 256:(dg + 1) * 256], in_=PB[bo].ap[:, 0:256], func=AF.Copy, scale=gval.ap[:, ic:ic + 1]), reads=[PB[bo], gval], writes=[ost[jb]])
                    for jb in range(4):
                        sc = col * 4 + jb
                        k.dma("pool", None, None, t_ys, reads=[ost[jb], gidx32], writes=[t_ys],
                              fn=lambda e: e.indirect_dma_start(out=y_s[:, :], out_offset=bass.IndirectOffsetOnAxis(ap=gidx32.ap[:, sc:sc + 1], axis=0), in_=ost[jb].ap[:], in_offset=None, compute_op=ALU.add))
            k.barrier()

        with contextlib.ExitStack() as esG:
            SG = mk_alloc(esG)
            yt = [SG("yfin%d" % i, [128, D]) for i in range(2)]
            x1t = [SG("x1t%d" % i, [128, D]) for i in range(2)]
            G2 = SG("G2f", [128, D])
            st2 = SG("st2", [128, 8])
            jk = SG("jk", [128, D], BF16)
            k.dma("sp", G2.ap[:], g2_s[:, :], G2, writes=[G2])
            for c in range(NCH):
                y_t, x_t = yt[c % 2], x1t[c % 2]
                k.dma("sp", y_t.ap[:], y_s[c * 128:(c + 1) * 128, :], y_t, reads=[t_ys], writes=[y_t])
                k.dma("sp", x_t.ap[:], x1_s[c * 128:(c + 1) * 128, :], x_t, writes=[x_t])
                k.op("act", lambda e: e.activation(out=jk.ap[:], in_=y_t.ap[:], func=AF.Square, accum_out=st2.ap[:, 0:1]), reads=[y_t], writes=[jk, st2])
                k.op("act", lambda e: e.activation(out=st2.ap[:, 1:2], in_=st2.ap[:, 0:1], func=AF.Sqrt, scale=1.0 / D, bias=EPS), reads=[st2], writes=[st2])
                k.op("dve", lambda e: e.reciprocal(out=st2.ap[:, 2:3], in_=st2.ap[:, 1:2]), reads=[st2], writes=[st2])
                k.op("dve", lambda e: e.tensor_tensor(out=y_t.ap[:], in0=y_t.ap[:], in1=G2.ap[:], op=ALU.mult), reads=[y_t, G2], writes=[y_t])
                k.op("dve", lambda e: e.scalar_tensor_tensor(out=y_t.ap[:], in0=y_t.ap[:], scalar=st2.ap[:, 2:3], in1=x_t.ap[:], op0=ALU.mult, op1=ALU.add), reads=[y_t, st2, x_t], writes=[y_t])
                k.dma("sp", out_d[c * 128:(c + 1) * 128, :], y_t.ap[:], y_t, reads=[y_t])
            if dbg:
                o = dbg_out("d_ys", [L, D])
                tt2 = T("d_ys", o)
                k.dma("sp", o[:, :], y_s[:, :], tt2, reads=[t_ys], writes=[tt2])
            k.barrier()

    return nc, dbg_d


def _fm(v, nb):
    return np.ascontiguousarray(np.asarray(v, np.float32).reshape(nb, 128).T)


def _host_inputs(r, x, c, ctx, c_ctx, w_mod, b_mod, norm_pre_mix, norm_post_mix, w_in, ssd_conv_w,
                 ssd_conv_b, dt_bias, a_log, ssd_d, ssd_norm_w, w_ssd_out, sc_conv_w, w_sc_out,
                 b_gate, w_o, norm_pre_ffn, norm_post_ffn, w_router, w_e1, w_e3, w_e2):
    f = np.float32
    b = r % 4
    pk = np.zeros((128, NPK), f)
    ii = np.arange(128)
    pk[:, PK_ID:PK_ID + 128] = np.eye(128, dtype=f)
    pk[:, PK_SU:PK_SU + 128] = (ii[:, None] > ii[None, :])
    pk[:, PK_SL:PK_SL + 128] = (ii[:, None] < ii[None, :])
    pk[:, PK_TLE:PK_TLE + 128] = (ii[:, None] <= ii[None, :])
    pk[:, PK_TGE:PK_TGE + 128] = (ii[:, None] >= ii[None, :])
    pk[:, PK_ONE:PK_ONE + 128] = 1.0
    cv = np.stack([_fm(c[b], 16), _fm(c_ctx, 16)], axis=2).reshape(128, 32)
    pk[:, PK_C:PK_C + 32] = cv
    pk[:, PK_BMOD:PK_BMOD + 16] = _fm(b_mod[0, 0:D], 16)
    pk[:, PK_BMOD + 16:PK_BMOD + 32] = _fm(b_mod[0, D:2 * D], 16)
    pk[:, PK_NPM:PK_NPM + 16] = _fm(norm_pre_mix[0], 16)
    pk[:, PK_SNW:PK_SNW + 16] = _fm(ssd_norm_w[0], 16)
    for t in range(3):
        pk[:, PK_CW + t * 24:PK_CW + (t + 1) * 24] = _fm(ssd_conv_w[0, t], 24)
        pk[:, PK_SCW + t * 16:PK_SCW + (t + 1) * 16] = _fm(sc_conv_w[0, t], 16)
    pk[:, PK_CB:PK_CB + 24] = _fm(ssd_conv_b[0], 24)
    pk[:, PK_BG:PK_BG + 32] = _fm(b_gate[0], 32)
    pk[:, PK_DTB:PK_DTB + 64] = np.asarray(dt_bias[0], f).reshape(1, 64)
    pk[:, PK_ALOG:PK_ALOG + 64] = np.asarray(a_log[0], f).reshape(1, 64)
    pk[:, PK_SSD_D:PK_SSD_D + 32] = np.asarray(ssd_d[0], f).reshape(1, 32)
    pk[:, PK_WR:PK_WR + 256] = np.asarray(w_router[0], f).reshape(16, 128, 16).transpose(1, 0, 2).reshape(128, 256)
    pk[:, PK_IOTA:PK_IOTA + 512] = np.arange(512, dtype=f)[None, :]
    pk[:, PK_PIDX] = ii
    pk[0:NE, PK_SELM:PK_SELM + NE] = np.eye(NE, dtype=f)
    reps = np.empty((128, 7 * D), f)
    reps[:, 0:D] = np.asarray(norm_post_mix[0], f)[None, :]
    reps[:, D:2 * D] = np.asarray(norm_pre_ffn[0], f)[None, :]
    reps[:, 2 * D:3 * D] = np.asarray(norm_post_ffn[0], f)[None, :]
    reps[:, 3 * D:7 * D] = np.asarray(b_mod[0, 2 * D:6 * D], f)[None, :]
    return {
        "x": np.ascontiguousarray(x[b]), "ctx": np.ascontiguousarray(ctx[b]),
        "w_in": np.ascontiguousarray(w_in[0]), "w_mod": np.ascontiguousarray(w_mod[0]),
        "w_ssd_out": np.ascontiguousarray(w_ssd_out[0]), "w_sc_out": np.ascontiguousarray(w_sc_out[0]),
        "w_o": np.ascontiguousarray(w_o[0]),
        "w_e1": np.ascontiguousarray(w_e1[0]), "w_e3": np.ascontiguousarray(w_e3[0]), "w_e2": np.ascontiguousarray(w_e2[0]),
        "pk": pk, "reps": reps, "idb": np.eye(128, dtype=np.float32).astype(ml_dtypes.bfloat16),
    }


def kernel(**inputs):
    inputs = {k_: np.asarray(v) for k_, v in inputs.items()}
    nc, _ = build_program()
    in_maps = [_host_inputs(r, **inputs) for r in range(8)]
    res = run_bass_kernel_spmd(nc, in_maps, core_ids=list(range(8)))
    out = np.stack([np.asarray(res.results[b]["out"], np.float32) for b in range(4)], axis=0)
    return out
```

```python
import contextlib
import numpy as np
import ml_dtypes
import concourse.bass as bass
import concourse.mybir as mybir
from concourse.bass_utils import run_bass_kernel_spmd

F32 = mybir.dt.float32
BF16 = mybir.dt.bfloat16
I32 = mybir.dt.int32
AF = mybir.ActivationFunctionType
ALU = mybir.AluOpType

D = 2048
L = 4096
NCH = L // 128
CTXL = 256
PROJ = 15424
C_Z, C_XBC, C_DT, C_SCB, C_SCC, C_SCV, C_GS, C_GC = 0, 2048, 5120, 5184, 7232, 9280, 11328, 13376
NE = 16
CAP = 512
EPS = 1e-6
TS = 256
NBIS = 34

PK_ID, PK_SU, PK_SL, PK_TLE, PK_TGE, PK_ONE = 0, 128, 256, 384, 512, 640
PK_C, PK_BMOD, PK_NPM, PK_SNW, PK_CW, PK_CB, PK_SCW, PK_BG = 768, 800, 832, 848, 864, 936, 960, 1008
PK_DTB, PK_ALOG, PK_SSD_D, PK_WR, PK_IOTA, PK_PIDX = 1040, 1104, 1168, 1200, 1456, 1968
PK_SELM, PK_GOFF, PK_OSRC, NPK = 1984, 2008, 2104, 2168


class T:
    __slots__ = ("name", "ap", "w", "r", "dsem", "dcnt", "root")

    def __init__(self, name, ap, root=None):
        self.name = name
        self.ap = ap
        self.w = None
        self.r = []
        self.dsem = None
        self.dcnt = 0
        self.root = root if root is not None else self


class K:
    def __init__(self, nc):
        self.nc = nc
        self.engs = {"pe": nc.tensor, "act": nc.scalar, "dve": nc.vector, "pool": nc.gpsimd, "sp": nc.sync}
        self.sems = {}
        self.cnt = {}
        for k in ("pe", "act", "dve", "pool"):
            self.sems[k] = nc.alloc_semaphore("s_" + k)
            self.cnt[k] = 0
        self.waited = {}
        self.nsem = 0
        self.dsems = []

    def _deps(self, reads, writes):
        deps = {}

        def add(d):
            if d is None:
                return
            k, v = d
            if deps.get(k, 0) < v:
                deps[k] = v
        for t in reads:
            add(t.root.w)
        for t in writes:
            add(t.root.w)
            for r in t.root.r:
                add(r)
        return deps

    def _wait(self, ek, deps):
        eng = self.engs[ek]
        for k, v in deps.items():
            if ek == "pe" and k == "pe":
                continue
            if self.waited.get((ek, k), 0) >= v:
                continue
            eng.wait_ge(self.sems[k], v)
            self.waited[(ek, k)] = v

    def _mark(self, me, reads, writes):
        for t in reads:
            t.root.r.append(me)
        for t in writes:
            t.root.w = me
            t.root.r = []

    def op(self, ek, fn, reads=(), writes=()):
        self._wait(ek, self._deps(reads, writes))
        ins = fn(self.engs[ek])
        self.cnt[ek] += 1
        ins.then_inc(self.sems[ek], 1)
        self._mark((ek, self.cnt[ek]), reads, writes)
        return ins

    def _dsem(self, semt):
        semt = semt.root
        if semt.dsem is None:
            semt.dsem = "d%d_%s" % (self.nsem, semt.name)
            self.sems[semt.dsem] = self.nc.alloc_semaphore(semt.dsem)
            self.dsems.append(semt)
            self.nsem += 1
        return semt

    def dma(self, qk, out_ap, in_ap, semt, reads=(), writes=(), fn=None, **kw):
        semt = self._dsem(semt)
        self._wait(qk, self._deps(reads, writes))
        if fn is None:
            ins = self.engs[qk].dma_start(out=out_ap, in_=in_ap, **kw)
        else:
            ins = fn(self.engs[qk])
        semt.dcnt += 1
        ins.then_inc(self.sems[semt.dsem], 16)
        me = (semt.dsem, 16 * semt.dcnt)
        self._mark(me, reads, writes)
        return me

    def barrier(self):
        tot = {k: self.cnt[k] for k in ("pe", "act", "dve", "pool")}
        for t in self.dsems:
            tot[t.dsem] = 16 * t.dcnt
        for ek in ("pe", "act", "dve", "pool", "sp"):
            eng = self.engs[ek]
            for k, v in tot.items():
                if v == 0 or self.waited.get((ek, k), 0) >= v:
                    continue
                eng.wait_ge(self.sems[k], v)
                self.waited[(ek, k)] = v


def build_program(stage=99, dbg=False, one_core=False):
    nc = bass.Bass("TRN2", target_bir_lowering=False)
    k = K(nc)

    def din(name, shape, dt=F32):
        return nc.dram_tensor(name, list(shape), dt, kind="ExternalInput").ap()

    x_d = din("x", [L, D])
    ctx_d = din("ctx", [CTXL, D])
    w_in_d = din("w_in", [D, PROJ])
    w_mod_d = din("w_mod", [D, 6 * D])
    w_ssd_d = din("w_ssd_out", [D, D])
    w_sc_d = din("w_sc_out", [D, D])
    w_o_d = din("w_o", [D, D])
    we_shape = [NE, D, D] if stage >= 5 else [2, 8, 8]
    w_e1_d = din("w_e1", we_shape)
    w_e3_d = din("w_e3", we_shape)
    w_e2_d = din("w_e2", we_shape)
    pk_d = din("pk", [128, NPK])
    reps_d = din("reps", [128, 7 * D])
    idb_d = din("idb", [128, 128], BF16)
    out_d = nc.dram_tensor("out", [L, D], F32, kind="ExternalOutput").ap()
    hb_s = nc.dram_tensor("hb_s", [NCH, 128, D], BF16, kind="Internal").ap()
    x1_s = nc.dram_tensor("x1_s", [L, D], F32, kind="Internal").ap()
    hx2_s = nc.dram_tensor("hx2_s", [L, D], BF16, kind="Internal").ap()
    y_s = nc.dram_tensor("y_s", [L, D], F32, kind="Internal").ap()
    g2_s = nc.dram_tensor("g2_s", [128, D], F32, kind="Internal").ap()
    win16 = nc.dram_tensor("win16", [D, PROJ], BF16, kind="Internal").ap()
    wssd16 = nc.dram_tensor("wssd16", [D, D], BF16, kind="Internal").ap()
    wsc16 = nc.dram_tensor("wsc16", [D, D], BF16, kind="Internal").ap()
    wo16 = nc.dram_tensor("wo16", [D, D], BF16, kind="Internal").ap()
    dbg_d = {}

    def dbg_out(name, shape, dt=F32):
        dbg_d[name] = nc.dram_tensor(name, list(shape), dt, kind="ExternalOutput").ap()
        return dbg_d[name]

    es_all = contextlib.ExitStack()
    with es_all:
        def mk_alloc(es):
            def S(name, shape, dt=F32):
                return T(name, es.enter_context(nc.sbuf_tensor("sb_" + name, list(shape), dt)))
            return S
        SP = mk_alloc(es_all)
        psum = es_all.enter_context(nc.psum_tensor("psum", [128, 4096], F32))
        PB = [T("pb%d" % i, psum[:, i * 512:(i + 1) * 512]) for i in range(8)]

        def pbf(i, n=1):
            return psum[:, i * 512:(i + n) * 512]

        def pbh(i, n=1):
            return psum[:, i * 512:(i + n) * 512].bitcast(BF16)

        pk = SP("pk", [128, NPK])
        idb = SP("idb", [128, 128], BF16)
        G1 = SP("G1", [128, D])
        A2 = SP("A2", [128, D])
        B2 = SP("B2", [128, D])
        modfm = SP("modfm", [128, 64])
        arep = SP("arep", [128, 64])
        wdt = SP("wdt", [128, 16, 64], BF16)
        affAll = SP("affAll", [128, NCH, NE])
        hf = SP("hf", [128, D])
        h16 = SP("h16", [128, D], BF16)

        def pkc(c0, n):
            return pk.ap[:, c0:c0 + n]
        ident = pkc(PK_ID, 128)
        SU, SL, TLE, TGE, ONES = pkc(PK_SU, 128), pkc(PK_SL, 128), pkc(PK_TLE, 128), pkc(PK_TGE, 128), pkc(PK_ONE, 128)

        k.dma("sp", pk.ap[:], pk_d[:, :], pk, writes=[pk])
        k.dma("sp", idb.ap[:], idb_d[:, :], idb, writes=[idb])
        k.dma("pool", wdt.ap[:], w_in_d[:, C_DT:C_DT + 64].rearrange("(kb p) c -> p kb c", p=128), wdt, writes=[wdt])
        t_wA, t_wB, t_wC = T("winA", win16), T("winB", win16), T("winC", win16)
        t_wssd, t_wsc, t_wo = T("wssd16", wssd16), T("wsc16", wsc16), T("wo16", wo16)

        def cast_region(dst, srcw, c0, c1, piece, rows_per, tt):
            for r0 in range(0, D, rows_per):
                k.dma("pool", dst[r0:r0 + rows_per, c0:c1].rearrange("r (a c) -> r a c", c=piece),
                      srcw[r0:r0 + rows_per, c0:c1].rearrange("r (a c) -> r a c", c=piece), tt, writes=[tt])
        cast_region(win16, w_in_d, C_XBC, C_DT, 1536, 512, t_wA)
        cast_region(win16, w_in_d, C_Z, C_XBC, 2048, 1024, t_wB)
        cast_region(win16, w_in_d, C_SCB, PROJ, 2048, 256, t_wC)
        cast_region(wssd16, w_ssd_d, 0, D, 2048, 1024, t_wssd)
        cast_region(wsc16, w_sc_d, 0, D, 2048, 1024, t_wsc)
        cast_region(wo16, w_o_d, 0, D, 2048, 1024, t_wo)

        with contextlib.ExitStack() as es0:
            S0 = mk_alloc(es0)
            csil = S0("csil", [128, 32])
            crep = S0("crep", [128, 16, 128])
            wm = [S0("wm%d" % i, [128, 16, 512]) for i in range(2)]
            brp = [S0("brp%d" % i, [128, 512]) for i in range(2)]
            nrp = [S0("nrp%d" % i, [128, 512]) for i in range(2)]
            tmp0 = S0("tmp0", [128, 512])
            G2 = S0("G2", [128, D])
            k.op("act", lambda e: e.activation(out=csil.ap[:], in_=pkc(PK_C, 32), func=AF.Silu), reads=[pk], writes=[csil])
            k.op("dve", lambda e: e.tensor_copy(out=crep.ap[:], in_=csil.ap[:, 0:32].rearrange("p (kb w) -> p kb w", w=2)[:, :, 0:1].to_broadcast([128, 16, 128])),
                 reads=[csil], writes=[crep])
            k.op("act", lambda e: e.activation(out=arep.ap[:], in_=pkc(PK_ALOG, 64), func=AF.Exp), reads=[pk], writes=[arep])
            k.op("dve", lambda e: e.tensor_scalar(out=arep.ap[:], in0=arep.ap[:], scalar1=-1.0, scalar2=None, op0=ALU.mult), reads=[arep], writes=[arep])
            ngrp = 24
            for g in range(ngrp):
                w_t = wm[g % 2]
                k.dma("sp", w_t.ap[:], w_mod_d[:, g * 512:(g + 1) * 512].rearrange("(kb p) c -> p kb c", p=128), w_t, writes=[w_t])
                if g < 8:
                    pb = PB[g % 2]
                    for j in range(4):
                        for kb in range(16):
                            k.op("pe", lambda e: e.matmul(out=pb.ap[:, 2 * j:2 * j + 2], lhsT=w_t.ap[:, kb, j * 128:(j + 1) * 128],
                                                          rhs=csil.ap[:, 2 * kb:2 * kb + 2], start=(kb == 0), stop=(kb == 15)),
                                 reads=[w_t, csil], writes=[pb])
                    db0 = (g % 4) * 4
                    boff = PK_BMOD + (0 if g < 4 else 16) + db0
                    for which in range(2):
                        dst0 = which * 32 + (16 if g < 4 else 0) + db0
                        k.op("dve", lambda e: e.tensor_tensor(out=modfm.ap[:, dst0:dst0 + 4],
                                                              in0=pb.ap[:, 0:8].rearrange("p (j w) -> p w j", w=2)[:, which, :],
                                                              in1=pk.ap[:, boff:boff + 4], op=ALU.add),
                             reads=[pb, pk], writes=[modfm])
                else:
                    gi = g - 8
                    ty, cg = gi // 4, gi % 4
                    b_t, n_t = brp[g % 2], nrp[g % 2]
                    k.dma("sp", b_t.ap[:], reps_d[:, 3 * D + gi * 512: 3 * D + (gi + 1) * 512], b_t, writes=[b_t])
                    if ty != 1:
                        nsel = {0: 0, 2: 1, 3: 2}[ty]
                        k.dma("sp", n_t.ap[:], reps_d[:, nsel * D + cg * 512: nsel * D + (cg + 1) * 512], n_t, writes=[n_t])
                    pb = PB[2 + g % 2]
                    for kb in range(16):
                        k.op("pe", lambda e: e.matmul(out=pb.ap[:, :], lhsT=crep.ap[:, kb, :], rhs=w_t.ap[:, kb, :], start=(kb == 0), stop=(kb == 15)),
                             reads=[w_t, crep], writes=[pb])
                    dst = {0: G1, 1: B2, 2: A2, 3: G2}[ty]
                    dsl = dst.ap[:, cg * 512:(cg + 1) * 512]
                    if ty == 1:
                        k.op("dve", lambda e: e.tensor_tensor(out=dsl, in0=pb.ap[:, :], in1=b_t.ap[:], op=ALU.add), reads=[pb, b_t], writes=[dst])
                    else:
                        k.op("dve", lambda e: e.tensor_tensor(out=tmp0.ap[:], in0=pb.ap[:, :], in1=b_t.ap[:], op=ALU.add), reads=[pb, b_t], writes=[tmp0])
                        if ty == 2:
                            k.op("dve", lambda e: e.scalar_tensor_tensor(out=dsl, in0=tmp0.ap[:], scalar=1.0, in1=n_t.ap[:], op0=ALU.add, op1=ALU.mult),
                                 reads=[tmp0, n_t], writes=[dst])
                        else:
                            k.op("dve", lambda e: e.tensor_tensor(out=dsl, in0=tmp0.ap[:], in1=n_t.ap[:], op=ALU.mult), reads=[tmp0, n_t], writes=[dst])
            for which in range(2):
                k.op("dve", lambda e: e.scalar_tensor_tensor(out=modfm.ap[:, which * 32:which * 32 + 16], in0=modfm.ap[:, which * 32:which * 32 + 16],
                                                             scalar=1.0, in1=pkc(PK_NPM, 16), op0=ALU.add, op1=ALU.mult),
                     reads=[modfm, pk], writes=[modfm])
            k.dma("sp", g2_s[:, :], G2.ap[:], G2, reads=[G2])
            k.barrier()
        if dbg and stage == 0:
            o1 = dbg_out("d_modfm", [128, 64])
            o2 = dbg_out("d_G1", [128, D])
            o3 = dbg_out("d_A2", [128, D])
            k.dma("sp", o1[:, :], modfm.ap[:], modfm, reads=[modfm])
            k.dma("sp", o2[:, :], G1.ap[:], G1, reads=[G1])
            k.dma("sp", o3[:, :], A2.ap[:], A2, reads=[A2])
        if stage == 0:
            k.barrier()
            return nc, dbg_d

        esM = contextlib.ExitStack()
        with esM:
            SM = mk_alloc(esM)
            NQ = TS // 128
            xc1 = SM("xc", [128, D])
            xc = [xc1, xc1]
            xn = [xc1, xc1]
            stat = SM("stat", [128, 16])
            hxT = SM("hxT", [128, 16, TS], BF16)
            wst = [SM("wst%d" % i, [128, 16, 256], BF16) for i in range(3)]
            xsT = SM("xsT", [128, 16, TS], BF16)
            BT = SM("BT", [128, 4, TS], BF16)
            CT = SM("CT", [128, 4, TS], BF16)
            ctmp = [SM("ctmp%d" % i, [128, TS]) for i in range(2)]
            zs = SM("zs", [128, NQ, D], BF16)
            cv = SM("cv", [128, 16, TS])
            scyT = SM("scyT", [128, 16, TS], BF16)
            gcT = SM("gcT", [128, 16, TS], BF16)
            ynT = SM("ynT", [128, 16, TS], BF16)
            xs_tok = [SM("xs_tok%d" % i, [128, D], BF16) for i in range(NQ)]
            B_tok = [SM("B_tok%d" % i, [128, 512], BF16) for i in range(NQ)]
            dtt = [SM("dtt%d" % i, [128, 64]) for i in range(NQ)]
            dta = [SM("dta%d" % i, [128, 64]) for i in range(NQ)]
            sm = SM("sm", [128, 128])
            wgt = SM("wgt", [128, 32])
            xdt_f = SM("xdt_f", [128, D], BF16)
            xdt_b = SM("xdt_b", [128, D], BF16)
            xD = SM("xD", [128, D], BF16)
            Rf = SM("Rf", [128, 1024])
            Rb = SM("Rb", [128, 1024])
            Lf = SM("Lf", [128, 1024], BF16)
            Lb = SM("Lb", [128, 1024], BF16)
            cbm = SM("cbm", [128, 256], BF16)
            yt1 = SM("yt1", [128, 512])
            yt2 = SM("yt2", [128, 512])
            yz = SM("yz", [128, D])
            yzn = SM("yzn", [128, D], BF16)
            hb1 = SM("hb16", [128, D], BF16)
            hb16 = [hb1, hb1]
            lg = SM("lg", [128, 32])
            mergedT = T("mergedT", xsT.ap, root=xsT)
            mtmp = T("mtmp", cv.ap, root=cv)
            outx = T("outx", yz.ap, root=yz)
            x1buf = T("x1buf", cv.ap[:, 0:8, :].rearrange("p a b -> p (a b)"), root=cv)
            xw = T("xw", xdt_f.ap, root=xdt_f)
            hx2 = T("hx2", xdt_b.ap, root=xdt_b)
            junk = T("junk", yzn.ap, root=yzn)
            Mf = T("Mf", Lf.ap, root=Lf)
            Mb = T("Mb", Lb.ap, root=Lb)

            wctr = [0]

            def wload(src_ap, treg):
                t = wst[wctr[0] % 3]
                wctr[0] += 1
                k.dma("sp", t.ap[:], src_ap.rearrange("(kb p) c -> p kb c", p=128), t, reads=[treg], writes=[t])
                return t

            pbctr = [0]

            def next_pb(lo=0, n=8):
                b = lo + pbctr[0] % n
                pbctr[0] += 1
                return b

            def gen_proj_fm(wsrc, col0, nblk, rhsT, tt, evac, treg=None, banks=None):
                for g0 in range(0, nblk, 2):
                    t = wload(wsrc[:, col0 + g0 * 128: col0 + g0 * 128 + 256], treg)
                    for j in range(2):
                        m = g0 + j
                        if banks is None:
                            b = next_pb()
                        else:
                            b = banks[pbctr[0] % len(banks)]
                            pbctr[0] += 1
                        for kb in range(16):
                            k.op("pe", lambda e: e.matmul(out=PB[b].ap[:, 0:tt], lhsT=t.ap[:, kb, j * 128:(j + 1) * 128], rhs=rhsT.ap[:, kb, 0:tt],
                                                          start=(kb == 0), stop=(kb == 15)), reads=[t, rhsT], writes=[PB[b]])
                        evac(m, PB[b])
                        yield

            def proj_fm(*a, **kw):
                for _ in gen_proj_fm(*a, **kw):
                    pass

            def pull(gen, n):
                for _ in range(n):
                    try:
                        next(gen)
                    except StopIteration:
                        return

            def prep_chunk(src_rows, q, a_off):
                xt, xnt = xc[q % 2], xn[q % 2]
                k.dma("sp", xt.ap[:], src_rows, xt, writes=[xt])
                k.op("act", lambda e: e.activation(out=junk.ap[:], in_=xt.ap[:], func=AF.Square, accum_out=stat.ap[:, 0:1]), reads=[xt], writes=[junk, stat])
                k.op("act", lambda e: e.activation(out=stat.ap[:, 1:2], in_=stat.ap[:, 0:1], func=AF.Sqrt, scale=1.0 / D, bias=EPS), reads=[stat], writes=[stat])
                k.op("dve", lambda e: e.reciprocal(out=stat.ap[:, 2:3], in_=stat.ap[:, 1:2]), reads=[stat], writes=[stat])
                k.op("act", lambda e: e.activation(out=xnt.ap[:], in_=xt.ap[:], func=AF.Copy, scale=stat.ap[:, 2:3]), reads=[xt, stat], writes=[xnt])
                b0 = 4 * (q % 2)
                for kb in range(16):
                    b = b0 + kb // 4
                    k.op("pe", lambda e: e.transpose(out=PB[b].ap[:, (kb % 4) * 128:(kb % 4 + 1) * 128], in_=xnt.ap[:, kb * 128:(kb + 1) * 128], identity=ident),
                         reads=[xnt, pk], writes=[PB[b]])
                for kb in range(16):
                    b = b0 + kb // 4
                    k.op("act", lambda e: e.activation(out=hxT.ap[:, kb, q * 128:(q + 1) * 128], in_=PB[b].ap[:, (kb % 4) * 128:(kb % 4 + 1) * 128], func=AF.Identity,
                                                       scale=modfm.ap[:, a_off + kb:a_off + kb + 1], bias=modfm.ap[:, a_off + 16 + kb:a_off + 17 + kb]),
                         reads=[PB[b], modfm], writes=[hxT])

            def conv_evac(dstT, blk0, tt, rl):
                nr = tt // rl

                def f(m, pb):
                    cb = blk0 + m
                    ct = ctmp[m % 2]
                    w0 = pk.ap[:, PK_CW + cb:PK_CW + cb + 1]
                    w1 = pk.ap[:, PK_CW + 24 + cb:PK_CW + 25 + cb]
                    w2 = pk.ap[:, PK_CW + 48 + cb:PK_CW + 49 + cb]
                    bb = pk.ap[:, PK_CB + cb:PK_CB + cb + 1]
                    k.op("act", lambda e: e.activation(out=ct.ap[:, 0:tt], in_=pb.ap[:, 0:tt], func=AF.Identity, scale=w1, bias=bb), reads=[pb, pk], writes=[ct])
                    pv = pb.ap[:, 0:tt].rearrange("p (r c) -> p r c", c=rl)
                    cvw = ct.ap[:, 0:tt].rearrange("p (r c) -> p r c", c=rl)
                    k.op("dve", lambda e: e.scalar_tensor_tensor(out=cvw[:, :, 1:rl], in0=pv[:, :, 0:rl - 1], scalar=w0, in1=cvw[:, :, 1:rl], op0=ALU.mult, op1=ALU.add),
                         reads=[pb, ct, pk], writes=[ct])
                    k.op("dve", lambda e: e.scalar_tensor_tensor(out=cvw[:, :, 0:rl - 1], in0=pv[:, :, 1:rl], scalar=w2, in1=cvw[:, :, 0:rl - 1], op0=ALU.mult, op1=ALU.add),
                         reads=[pb, ct, pk], writes=[ct])
                    if cb < 16:
                        dst, di = dstT[0], cb
                    elif cb < 20:
                        dst, di = dstT[1], cb - 16
                    else:
                        dst, di = dstT[2], cb - 20
                    k.op("act", lambda e: e.activation(out=dst.ap[:, di, 0:tt], in_=ct.ap[:, 0:tt], func=AF.Silu), reads=[ct], writes=[dst])
                return f

            def dt_chunk(q):
                pb = PB[0]
                for kb in range(16):
                    k.op("pe", lambda e: e.matmul(out=pb.ap[:, 0:64], lhsT=hxT.ap[:, kb, q * 128:(q + 1) * 128], rhs=wdt.ap[:, kb, :], start=(kb == 0), stop=(kb == 15)),
                         reads=[hxT, wdt], writes=[pb])
                k.op("dve", lambda e: e.tensor_tensor(out=dtt[q].ap[:], in0=pb.ap[:, 0:64], in1=pkc(PK_DTB, 64), op=ALU.add), reads=[pb, pk], writes=[dtt[q]])
                k.op("act", lambda e: e.activation(out=dtt[q].ap[:], in_=dtt[q].ap[:], func=AF.Exp), reads=[dtt[q]], writes=[dtt[q]])
                k.op("act", lambda e: e.activation(out=dtt[q].ap[:], in_=dtt[q].ap[:], func=AF.Ln, bias=1.0), reads=[dtt[q]], writes=[dtt[q]])
                k.op("dve", lambda e: e.tensor_tensor(out=dta[q].ap[:], in0=dtt[q].ap[:], in1=arep.ap[:], op=ALU.mult), reads=[dtt[q], arep], writes=[dta[q]])

            def to_tok(q, with_b=True):
                for half in range(2):
                    b = 4 + 2 * half
                    for i in range(8):
                        cbk = half * 8 + i
                        bb = b + i // 4
                        k.op("pe", lambda e: e.transpose(out=pbh(bb)[:, (i % 4) * 128:(i % 4 + 1) * 128], in_=xsT.ap[:, cbk, q * 128:(q + 1) * 128], identity=idb.ap[:]),
                             reads=[xsT, idb], writes=[PB[bb]])
                for half in range(2):
                    b = 4 + 2 * half
                    src = psum[:, b * 512:(b + 2) * 512].bitcast(BF16).rearrange("p (b x c) -> p b x c", b=2, x=2)[:, :, 0, :]
                    k.op("act" if half == 0 else "dve",
                         (lambda e: e.activation(out=xs_tok[q].ap[:, half * 1024:(half + 1) * 1024].rearrange("p (b c) -> p b c", b=2), in_=src, func=AF.Copy)) if half == 0 else
                         (lambda e: e.tensor_copy(out=xs_tok[q].ap[:, half * 1024:(half + 1) * 1024].rearrange("p (b c) -> p b c", b=2), in_=src)),
                         reads=[PB[b], PB[b + 1]], writes=[xs_tok[q]])
                if with_b:
                    for g in range(4):
                        k.op("pe", lambda e: e.transpose(out=pbh(3)[:, g * 128:(g + 1) * 128], in_=BT.ap[:, g, q * 128:(q + 1) * 128], identity=idb.ap[:]),
                             reads=[BT, idb], writes=[PB[3]])
                    k.op("dve", lambda e: e.tensor_copy(out=B_tok[q].ap[:], in_=pbh(3)[:, 0:512]), reads=[PB[3]], writes=[B_tok[q]])

            def state_update(q, dr, smt, wcol, dcol):
                k.op("dve", lambda e: e.tensor_tensor(out=wgt.ap[:], in0=smt.ap[:, wcol:wcol + 32], in1=dtt[q].ap[:, dr * 32:(dr + 1) * 32], op=ALU.mult),
                     reads=[smt, dtt[q]], writes=[wgt])
                k.op("pool", lambda e: e.tensor_tensor(out=xw.ap[:].rearrange("p (h c) -> p h c", c=64), in0=xs_tok[q].ap[:].rearrange("p (h c) -> p h c", c=64),
                                                       in1=wgt.ap[:, 0:32].unsqueeze(2).to_broadcast([128, 32, 64]), op=ALU.mult),
                     reads=[xs_tok[q], wgt], writes=[xw])
                for g in range(4):
                    sb_ = (1, 2, 6, 7)[g]
                    k.op("pe", lambda e: e.matmul(out=PB[sb_].ap[:, :], lhsT=B_tok[q].ap[:, g * 128:(g + 1) * 128], rhs=xw.ap[:, g * 512:(g + 1) * 512], start=True, stop=True),
                         reads=[B_tok[q], xw], writes=[PB[sb_]])
                k.op("dve", lambda e: e.tensor_tensor(out=hf.ap[:].rearrange("p (h c) -> p h c", c=64), in0=hf.ap[:].rearrange("p (h c) -> p h c", c=64),
                                                      in1=smt.ap[:, dcol:dcol + 32].unsqueeze(2).to_broadcast([128, 32, 64]), op=ALU.mult),
                     reads=[hf, smt], writes=[hf])
                k.op("dve", lambda e: e.tensor_tensor(out=hf.ap[:, 0:1024], in0=hf.ap[:, 0:1024], in1=pbf(1, 2), op=ALU.add), reads=[hf, PB[1], PB[2]], writes=[hf])
                k.op("dve", lambda e: e.tensor_tensor(out=hf.ap[:, 1024:2048], in0=hf.ap[:, 1024:2048], in1=pbf(6, 2), op=ALU.add), reads=[hf, PB[6], PB[7]], writes=[hf])
                k.op("act", lambda e: e.activation(out=h16.ap[:], in_=hf.ap[:], func=AF.Copy), reads=[hf], writes=[h16])

            def small_state(q, dr):
                tri = SU if dr == 0 else SL
                k.op("pe", lambda e: e.matmul(out=PB[0].ap[:, 0:32], lhsT=tri, rhs=dta[q].ap[:, dr * 32:(dr + 1) * 32], start=True, stop=True), reads=[pk, dta[q]], writes=[PB[0]])
                k.op("pe", lambda e: e.matmul(out=PB[0].ap[:, 32:64], lhsT=ONES, rhs=dta[q].ap[:, dr * 32:(dr + 1) * 32], start=True, stop=True), reads=[pk, dta[q]], writes=[PB[0]])
                k.op("act", lambda e: e.activation(out=sm.ap[:, 0:64], in_=PB[0].ap[:, 0:64], func=AF.Exp), reads=[PB[0]], writes=[sm])

            def states_superchunk(src_d, row0, tt, a_off, rl, dr, chunk_order, store_c0=None):
                nq = tt // 128
                for q in range(nq):
                    prep_chunk(src_d[row0 + q * 128: row0 + (q + 1) * 128, :], q, a_off)
                proj_fm(win16, C_XBC, 20, hxT, tt, conv_evac((xsT, BT, CT), 0, tt, rl), t_wA)
                for q in range(nq):
                    dt_chunk(q)
                    to_tok(q)
                for q in chunk_order:
                    if store_c0 is not None:
                        c = store_c0 + q
                        k.dma("sp", hb_s[c, :, :], h16.ap[:], h16, reads=[h16])
                    small_state(q, dr)
                    state_update(q, dr, sm, 0, 32)

            def zero_state():
                k.op("dve", lambda e: e.memset(hf.ap[:], 0.0), writes=[hf])
                k.op("dve", lambda e: e.memset(h16.ap[:], 0.0), writes=[h16])

            zero_state()
            states_superchunk(ctx_d, 0, CTXL, 32, CTXL, 1, [1, 0])
            nsc = L // TS
            for s in reversed(range(nsc)):
                states_superchunk(x_d, s * TS, TS, 0, 64, 1, list(reversed(range(NQ))), store_c0=s * NQ)
                if stage == 1 and s == nsc - 2:
                    break
            if dbg and stage == 1:
                o = dbg_out("d_hf", [128, D])
                k.dma("sp", o[:, :], hf.ap[:], hf, reads=[hf])
                o = dbg_out("d_xs", [128, D], BF16)
                k.dma("sp", o[:, :], xs_tok[0].ap[:], xs_tok[0], reads=[xs_tok[0]])
                o = dbg_out("d_dt", [128, 64])
                k.dma("sp", o[:, :], dtt[0].ap[:], dtt[0], reads=[dtt[0]])
            if stage == 1:
                k.barrier()
                return nc, dbg_d

            zero_state()
            states_superchunk(ctx_d, 0, CTXL, 32, CTXL, 0, [0, 1])

            k.op("pool", lambda e: e.memset(yz.ap[:], 0.0), writes=[yz])
            for c in range(NCH):
                k.dma("sp", y_s[c * 128:(c + 1) * 128, :], yz.ap[:], yz, reads=[yz])

            def ssd_chunk(s, q):
                c = s * NQ + q
                hbt = hb16[c % 2]
                k.dma("sp", hbt.ap[:], hb_s[c, :, :], hbt, writes=[hbt])
                cs = slice(q * 128, (q + 1) * 128)
                k.op("pe", lambda e: e.matmul(out=PB[0].ap[:, 0:32], lhsT=SU, rhs=dta[q].ap[:, 0:32], start=True, stop=True), reads=[pk, dta[q]], writes=[PB[0]])
                k.op("pe", lambda e: e.matmul(out=PB[0].ap[:, 32:64], lhsT=ONES, rhs=dta[q].ap[:, 0:32], start=True, stop=True), reads=[pk, dta[q]], writes=[PB[0]])
                k.op("pe", lambda e: e.matmul(out=PB[0].ap[:, 64:96], lhsT=TLE, rhs=dta[q].ap[:, 0:32], start=True, stop=True), reads=[pk, dta[q]], writes=[PB[0]])
                k.op("pe", lambda e: e.matmul(out=PB[0].ap[:, 96:128], lhsT=TGE, rhs=dta[q].ap[:, 32:64], start=True, stop=True), reads=[pk, dta[q]], writes=[PB[0]])
                k.op("act", lambda e: e.activation(out=sm.ap[:, 0:128], in_=PB[0].ap[:, 0:128], func=AF.Exp), reads=[PB[0]], writes=[sm])
                x3 = xs_tok[q].ap[:].rearrange("p (h c) -> p h c", c=64)

                def bc(t, c0):
                    return t.ap[:, c0:c0 + 32].unsqueeze(2).to_broadcast([128, 32, 64])
                k.op("pool", lambda e: e.tensor_tensor(out=xdt_f.ap[:].rearrange("p (h c) -> p h c", c=64), in0=x3, in1=bc(dtt[q], 0), op=ALU.mult), reads=[xs_tok[q], dtt[q]], writes=[xdt_f])
                k.op("pool", lambda e: e.tensor_tensor(out=xdt_b.ap[:].rearrange("p (h c) -> p h c", c=64), in0=x3, in1=bc(dtt[q], 32), op=ALU.mult), reads=[xs_tok[q], dtt[q]], writes=[xdt_b])
                k.op("pool", lambda e: e.tensor_tensor(out=xD.ap[:].rearrange("p (h c) -> p h c", c=64), in0=x3, in1=bc(pk, PK_SSD_D), op=ALU.mult), reads=[xs_tok[q], pk], writes=[xD])
                for g in range(4):
                    co = (g % 2) * 128
                    k.op("pe", lambda e: e.matmul(out=PB[0].ap[:, 128 + co:256 + co], lhsT=BT.ap[:, g, cs], rhs=CT.ap[:, g, cs], start=True, stop=True), reads=[BT, CT], writes=[PB[0]])
                    k.op("dve", lambda e: e.tensor_tensor(out=cbm.ap[:, 0:128], in0=PB[0].ap[:, 128 + co:256 + co], in1=TLE, op=ALU.mult), reads=[PB[0], pk], writes=[cbm])
                    k.op("dve", lambda e: e.tensor_tensor(out=cbm.ap[:, 128:256], in0=PB[0].ap[:, 128 + co:256 + co], in1=TGE, op=ALU.mult), reads=[PB[0], pk], writes=[cbm])
                    for (R, tri, dc) in ((Rf, TLE, 0), (Rb, TGE, 32)):
                        k.op("pool", lambda e: e.tensor_tensor(out=R.ap[:].rearrange("p (h i) -> p h i", i=128), in0=tri.unsqueeze(1).to_broadcast([128, 8, 128]),
                                                               in1=dta[q].ap[:, dc + g * 8:dc + g * 8 + 8].unsqueeze(2).to_broadcast([128, 8, 128]), op=ALU.mult),
                             reads=[pk, dta[q]], writes=[R])
                    yield
                    for (R, trl, b0, Lt, Mt, mc) in ((Rf, SU, 1, Lf, Mf, 0), (Rb, SL, 1, Lb, Mb, 128)):
                        for hh in range(2):
                            k.op("pe", lambda e: e.matmul(out=PB[b0 + hh].ap[:, :], lhsT=trl, rhs=R.ap[:, hh * 512:(hh + 1) * 512], start=True, stop=True), reads=[pk, R], writes=[PB[b0 + hh]])
                        k.op("act", lambda e: e.activation(out=Lt.ap[:], in_=pbf(b0, 2), func=AF.Exp), reads=[PB[b0], PB[b0 + 1]], writes=[Lt])
                        k.op("dve", lambda e: e.tensor_tensor(out=Mt.ap[:].rearrange("p (h i) -> p h i", i=128), in0=Lt.ap[:].rearrange("p (h i) -> p h i", i=128),
                                                              in1=cbm.ap[:, mc:mc + 128].unsqueeze(1).to_broadcast([128, 8, 128]), op=ALU.mult),
                             reads=[Lt, cbm], writes=[Mt])
                    yield
                    k.op("pe", lambda e: e.matmul(out=PB[5].ap[:, :], lhsT=idb.ap[:], rhs=xD.ap[:, g * 512:(g + 1) * 512], start=True, stop=False), reads=[idb, xD], writes=[PB[5]])
                    for (Mt, xdt) in ((Mf, xdt_f), (Mb, xdt_b)):
                        for h in range(8):
                            last = (Mt is Mb) and h == 7
                            k.op("pe", lambda e: e.matmul(out=PB[5].ap[:, h * 64:(h + 1) * 64], lhsT=Mt.ap[:, h * 128:(h + 1) * 128],
                                                          rhs=xdt.ap[:, (g * 8 + h) * 64:(g * 8 + h + 1) * 64], start=False, stop=last), reads=[Mt, xdt], writes=[PB[5]])
                    k.op("pe", lambda e: e.matmul(out=PB[6].ap[:, :], lhsT=CT.ap[:, g, cs], rhs=h16.ap[:, g * 512:(g + 1) * 512], start=True, stop=True), reads=[CT, h16], writes=[PB[6]])
                    k.op("pe", lambda e: e.matmul(out=PB[7].ap[:, :], lhsT=CT.ap[:, g, cs], rhs=hbt.ap[:, g * 512:(g + 1) * 512], start=True, stop=True), reads=[CT, hbt], writes=[PB[7]])

                    def bc8(c0):
                        return sm.ap[:, c0 + g * 8:c0 + g * 8 + 8].unsqueeze(2).to_broadcast([128, 8, 64])
                    k.op("dve", lambda e: e.tensor_tensor(out=yt1.ap[:].rearrange("p (h c) -> p h c", c=64), in0=PB[6].ap[:, :].rearrange("p (h c) -> p h c", c=64), in1=bc8(64), op=ALU.mult),
                         reads=[PB[6], sm], writes=[yt1])
                    k.op("dve", lambda e: e.tensor_tensor(out=yt2.ap[:].rearrange("p (h c) -> p h c", c=64), in0=PB[7].ap[:, :].rearrange("p (h c) -> p h c", c=64), in1=bc8(96), op=ALU.mult),
                         reads=[PB[7], sm], writes=[yt2])
                    k.op("pool", lambda e: e.tensor_tensor(out=yt1.ap[:], in0=yt1.ap[:], in1=yt2.ap[:], op=ALU.add), reads=[yt1, yt2], writes=[yt1])
                    k.op("dve", lambda e: e.tensor_tensor(out=yt1.ap[:], in0=PB[5].ap[:, :], in1=yt1.ap[:], op=ALU.add), reads=[PB[5], yt1], writes=[yt1])
                    k.op("pool", lambda e: e.tensor_tensor(out=yz.ap[:, g * 512:(g + 1) * 512], in0=yt1.ap[:], in1=zs.ap[:, q, g * 512:(g + 1) * 512], op=ALU.mult), reads=[yt1, zs], writes=[yz])
                    k.op("act", lambda e: e.activation(out=junk.ap[:, 0:512], in_=yz.ap[:, g * 512:(g + 1) * 512], func=AF.Square, accum_out=stat.ap[:, 4 + g:5 + g]), reads=[yz], writes=[junk, stat])
                    yield
                k.op("act", lambda e: e.activation(out=stat.ap[:, 8:12], in_=stat.ap[:, 4:8], func=AF.Sqrt, scale=1.0 / 512, bias=EPS), reads=[stat], writes=[stat])
                k.op("dve", lambda e: e.reciprocal(out=stat.ap[:, 12:16], in_=stat.ap[:, 8:12]), reads=[stat], writes=[stat])
                k.op("dve", lambda e: e.tensor_tensor(out=yzn.ap[:].rearrange("p (g c) -> p g c", c=512), in0=yz.ap[:].rearrange("p (g c) -> p g c", c=512),
                                                      in1=stat.ap[:, 12:16].unsqueeze(2).to_broadcast([128, 4, 512]), op=ALU.mult), reads=[yz, stat], writes=[yzn])
                for kb in range(16):
                    bb = 6 + kb // 8
                    k.op("pe", lambda e: e.transpose(out=pbh(bb)[:, (kb % 8) * 128:(kb % 8 + 1) * 128], in_=yzn.ap[:, kb * 128:(kb + 1) * 128], identity=idb.ap[:]), reads=[yzn, idb], writes=[PB[bb]])
                for kb in range(16):
                    bb = 6 + kb // 8
                    k.op("act", lambda e: e.activation(out=ynT.ap[:, kb, cs], in_=pbh(bb)[:, (kb % 8) * 128:(kb % 8 + 1) * 128], func=AF.Copy, scale=pk.ap[:, PK_SNW + kb:PK_SNW + kb + 1]),
                         reads=[PB[bb], pk], writes=[ynT])
                state_update(q, 0, sm, 0, 32)

            for q in range(NQ):
                prep_chunk(x_d[q * 128:(q + 1) * 128, :], q, 0)
            pending_tail = None
            outx1 = T("outx1", zs.ap[:].rearrange("p a b -> p (a b)").bitcast(F32), root=zs)
            outxs = [outx, outx1]
            for s in range(nsc):
                xg = gen_proj_fm(win16, C_XBC, 24, hxT, TS, conv_evac((xsT, BT, CT), 0, TS, 64), t_wA, [5, 6, 7] if pending_tail is not None else None)
                if pending_tail is not None:
                    for nfill in pending_tail:
                        pull(xg, nfill)
                for _ in xg:
                    pass
                for g4 in range(8):
                    t = wload(win16[:, C_Z + g4 * 256: C_Z + (g4 + 1) * 256], t_wB)
                    for q in range(NQ):
                        b = next_pb()
                        for kb in range(16):
                            k.op("pe", lambda e: e.matmul(out=PB[b].ap[:, 0:256], lhsT=hxT.ap[:, kb, q * 128:(q + 1) * 128], rhs=t.ap[:, kb, :], start=(kb == 0), stop=(kb == 15)),
                                 reads=[hxT, t], writes=[PB[b]])
                        k.op("act", lambda e: e.activation(out=zs.ap[:, q, g4 * 256:(g4 + 1) * 256], in_=PB[b].ap[:, 0:256], func=AF.Silu), reads=[PB[b]], writes=[zs])
                for q in range(NQ):
                    dt_chunk(q)
                    to_tok(q)
                def ev_c(m, pb):
                    k.op("act", lambda e: e.activation(out=cv.ap[:, m, :], in_=pb.ap[:, 0:TS], func=AF.Copy), reads=[pb], writes=[cv])

                def ev_v(m, pb):
                    ct = ctmp[m % 2]
                    k.op("dve", lambda e: e.tensor_tensor(out=ct.ap[:], in0=pb.ap[:, 0:TS], in1=cv.ap[:, m, :], op=ALU.mult), reads=[pb, cv], writes=[ct])
                    w0 = pk.ap[:, PK_SCW + m:PK_SCW + m + 1]
                    w1 = pk.ap[:, PK_SCW + 16 + m:PK_SCW + 17 + m]
                    w2 = pk.ap[:, PK_SCW + 32 + m:PK_SCW + 33 + m]
                    k.op("act", lambda e: e.activation(out=cv.ap[:, m, :], in_=ct.ap[:], func=AF.Copy, scale=w1), reads=[ct, pk], writes=[cv])
                    c3 = cv.ap[:, m, :].rearrange("p (r c) -> p r c", c=64)
                    t3 = ct.ap[:].rearrange("p (r c) -> p r c", c=64)
                    k.op("dve", lambda e: e.scalar_tensor_tensor(out=c3[:, :, 1:64], in0=t3[:, :, 0:63], scalar=w0, in1=c3[:, :, 1:64], op0=ALU.mult, op1=ALU.add), reads=[ct, cv, pk], writes=[cv])
                    k.op("dve", lambda e: e.scalar_tensor_tensor(out=c3[:, :, 0:63], in0=t3[:, :, 1:64], scalar=w2, in1=c3[:, :, 0:63], op0=ALU.mult, op1=ALU.add), reads=[ct, cv, pk], writes=[cv])

                def ev_b(m, pb):
                    k.op("dve", lambda e: e.tensor_tensor(out=scyT.ap[:, m, :], in0=pb.ap[:, 0:TS], in1=cv.ap[:, m, :], op=ALU.mult), reads=[pb, cv], writes=[scyT])

                def ev_g(dst, boff):
                    def f(m, pb):
                        k.op("act", lambda e: e.activation(out=dst.ap[:, m, :], in_=pb.ap[:, 0:TS], func=AF.Sigmoid, bias=pk.ap[:, PK_BG + boff + m:PK_BG + boff + m + 1]),
                             reads=[pb, pk], writes=[dst])
                    return f

                def filler():
                    FB = [3, 4]
                    yield from gen_proj_fm(win16, C_SCC, 16, hxT, TS, ev_c, t_wC, FB)
                    yield from gen_proj_fm(win16, C_SCV, 16, hxT, TS, ev_v, t_wC, FB)
                    yield from gen_proj_fm(win16, C_SCB, 16, hxT, TS, ev_b, t_wC, FB)
                    yield from gen_proj_fm(win16, C_GS, 16, hxT, TS, ev_g(mtmp, 0), t_wC, FB)
                fill = filler()
                nyld = 0
                for q in range(NQ):
                    for _ in ssd_chunk(s, q):
                        pull(fill, (3, 3, 2)[nyld % 3])
                        nyld += 1
                for _ in fill:
                    pass
                if dbg and stage == 2 and s == 0:
                    o = dbg_out("d_yz", [128, D])
                    k.dma("sp", o[:, :], yz.ap[:], yz, reads=[yz])
                    o = dbg_out("d_ynT", [128, 16 * TS], BF16)
                    k.dma("sp", o[:, :], ynT.ap[:].rearrange("p a b -> p (a b)"), ynT, reads=[ynT])
                    k.barrier()
                    return nc, dbg_d
                def ev_o1(m, pb):
                    k.op("dve", lambda e: e.tensor_tensor(out=mtmp.ap[:, m, :], in0=pb.ap[:, 0:TS], in1=mtmp.ap[:, m, :], op=ALU.mult), reads=[pb, mtmp], writes=[mtmp])
                proj_fm(wssd16, 0, 16, ynT, TS, ev_o1, t_wssd)
                proj_fm(win16, C_GC, 16, hxT, TS, ev_g(gcT, 16), t_wC)

                def ev_o2(m, pb):
                    ct = ctmp[m % 2]
                    k.op("dve", lambda e: e.tensor_tensor(out=ct.ap[:], in0=pb.ap[:, 0:TS], in1=gcT.ap[:, m, :], op=ALU.mult), reads=[pb, gcT], writes=[ct])
                    k.op("pool", lambda e: e.tensor_tensor(out=mergedT.ap[:, m, :], in0=ct.ap[:], in1=mtmp.ap[:, m, :], op=ALU.add), reads=[ct, mtmp], writes=[mergedT])
                proj_fm(wsc16, 0, 16, scyT, TS, ev_o2, t_wsc)

                if s + 1 < nsc:
                    for q in range(NQ):
                        prep_chunk(x_d[(s + 1) * TS + q * 128: (s + 1) * TS + (q + 1) * 128, :], q, 0)
                for q in range(NQ):
                    for g4 in range(8):
                        t = wload(wo16[:, g4 * 256:(g4 + 1) * 256], t_wo)
                        b = next_pb()
                        for kb in range(16):
                            k.op("pe", lambda e: e.matmul(out=PB[b].ap[:, 0:256], lhsT=mergedT.ap[:, kb, q * 128:(q + 1) * 128], rhs=t.ap[:, kb, :], start=(kb == 0), stop=(kb == 15)),
                                 reads=[mergedT, t], writes=[PB[b]])
                        k.op("act", lambda e: e.activation(out=outxs[q].ap[:, g4 * 256:(g4 + 1) * 256], in_=PB[b].ap[:, 0:256], func=AF.Copy), reads=[PB[b]], writes=[outxs[q]])

                def tail_gen(s=s):
                    for q in range(NQ):
                        c = s * NQ + q
                        ox = outxs[q]
                        xt = xc[0]
                        x1t = x1buf
                        k.dma("sp", xt.ap[:], x_d[c * 128:(c + 1) * 128, :], xt, writes=[xt])
                        k.op("act", lambda e: e.activation(out=junk.ap[:], in_=ox.ap[:], func=AF.Square, accum_out=stat.ap[:, 0:1]), reads=[ox], writes=[junk, stat])
                        k.op("act", lambda e: e.activation(out=stat.ap[:, 1:2], in_=stat.ap[:, 0:1], func=AF.Sqrt, scale=1.0 / D, bias=EPS), reads=[stat], writes=[stat])
                        k.op("dve", lambda e: e.reciprocal(out=stat.ap[:, 2:3], in_=stat.ap[:, 1:2]), reads=[stat], writes=[stat])
                        k.op("dve", lambda e: e.tensor_tensor(out=ox.ap[:], in0=ox.ap[:], in1=G1.ap[:], op=ALU.mult), reads=[ox, G1], writes=[ox])
                        k.op("dve", lambda e: e.scalar_tensor_tensor(out=x1t.ap[:], in0=ox.ap[:], scalar=stat.ap[:, 2:3], in1=xt.ap[:], op0=ALU.mult, op1=ALU.add), reads=[ox, stat, xt], writes=[x1t])
                        k.dma("sp", x1_s[c * 128:(c + 1) * 128, :], x1t.ap[:], x1t, reads=[x1t])
                        k.op("act", lambda e: e.activation(out=junk.ap[:], in_=x1t.ap[:], func=AF.Square, accum_out=stat.ap[:, 0:1]), reads=[x1t], writes=[junk, stat])
                        k.op("act", lambda e: e.activation(out=stat.ap[:, 1:2], in_=stat.ap[:, 0:1], func=AF.Sqrt, scale=1.0 / D, bias=EPS), reads=[stat], writes=[stat])
                        k.op("dve", lambda e: e.reciprocal(out=stat.ap[:, 2:3], in_=stat.ap[:, 1:2]), reads=[stat], writes=[stat])
                        k.op("dve", lambda e: e.scalar_tensor_tensor(out=ox.ap[:], in0=x1t.ap[:], scalar=stat.ap[:, 2:3], in1=A2.ap[:], op0=ALU.mult, op1=ALU.mult), reads=[x1t, stat, A2], writes=[ox])
                        k.op("dve", lambda e: e.tensor_tensor(out=ox.ap[:], in0=ox.ap[:], in1=B2.ap[:], op=ALU.add), reads=[ox, B2], writes=[ox])
                        k.op("act", lambda e: e.activation(out=hx2.ap[:], in_=ox.ap[:], func=AF.Copy), reads=[ox], writes=[hx2])
                        k.dma("sp", hx2_s[c * 128:(c + 1) * 128, :], hx2.ap[:], hx2, reads=[hx2])
                        yield (8 if q == 0 else 6)
                        for kb in range(16):
                            bb = kb // 4
                            k.op("pe", lambda e: e.transpose(out=PB[bb].ap[:, (kb % 4) * 128:(kb % 4 + 1) * 128], in_=ox.ap[:, kb * 128:(kb + 1) * 128], identity=ident), reads=[ox, pk], writes=[PB[bb]])
                        k.op("act", lambda e: e.activation(out=x1t.ap[:], in_=pbf(0, 4), func=AF.Copy), reads=[PB[0], PB[1], PB[2], PB[3]], writes=[x1t])
                        yield (4 if q == 0 else 3)
                        for kb in range(16):
                            k.op("pe", lambda e: e.matmul(out=PB[4].ap[:, 0:16], lhsT=x1t.ap[:, kb * 128:(kb + 1) * 128], rhs=pk.ap[:, PK_WR + kb * 16:PK_WR + (kb + 1) * 16], start=(kb == 0), stop=(kb == 15)),
                                 reads=[x1t, pk], writes=[PB[4]])
                        k.op("dve", lambda e: e.tensor_reduce(out=lg.ap[:, 16:17], in_=PB[4].ap[:, 0:16], op=ALU.max, axis=mybir.AxisListType.X), reads=[PB[4]], writes=[lg])
                        k.op("dve", lambda e: e.tensor_scalar(out=lg.ap[:, 17:18], in0=lg.ap[:, 16:17], scalar1=-1.0, scalar2=None, op0=ALU.mult), reads=[lg], writes=[lg])
                        k.op("act", lambda e: e.activation(out=lg.ap[:, 0:16], in_=PB[4].ap[:, 0:16], func=AF.Exp, bias=lg.ap[:, 17:18], accum_out=lg.ap[:, 18:19]), reads=[PB[4], lg], writes=[lg])
                        k.op("dve", lambda e: e.reciprocal(out=lg.ap[:, 19:20], in_=lg.ap[:, 18:19]), reads=[lg], writes=[lg])
                        k.op("dve", lambda e: e.tensor_scalar(out=affAll.ap[:, c, :], in0=lg.ap[:, 0:16], scalar1=lg.ap[:, 19:20], scalar2=None, op0=ALU.mult), reads=[lg], writes=[affAll])
                pending_tail = tail_gen()
                if stage == 2 and s == 1:
                    break
            for _ in pending_tail:
                pass
            if dbg and stage in (2, 3):
                o = dbg_out("d_aff", [128, NCH * NE])
                k.dma("sp", o[:, :], affAll.ap[:].rearrange("p a b -> p (a b)"), affAll, reads=[affAll])
            k.barrier()
        if stage in (2, 3):
            with contextlib.ExitStack() as esd:
                Sd = mk_alloc(esd)
                tt = Sd("tt", [128, D])
                for c in range(NCH):
                    k.dma("sp", tt.ap[:], x1_s[c * 128:(c + 1) * 128, :], tt, writes=[tt])
                    k.dma("sp", out_d[c * 128:(c + 1) * 128, :], tt.ap[:], tt, reads=[tt])
                k.barrier()
            return nc, dbg_d

        RG = [list(range(8))]
        NCOL = NE
        aff_loc = nc.dram_tensor("aff_loc", [NE, L], F32, kind="Internal").ap()
        aff_all = nc.dram_tensor("aff_all", [8 * NE, L], F32, kind="Internal").ap()
        t_hx2all = T("hx2_s", hx2_s)
        t_ys = T("y_s", y_s)
        ccn = [0]

        def allgather(in_ap, out_ap, t_in, t_out):
            if one_core:
                k.dma("pool", out_ap[0:in_ap.shape[0], :], in_ap, t_out, reads=[t_in], writes=[t_out])
                return
            nm = "cc%d" % ccn[0]
            ccn[0] += 1
            k.sems[nm] = nc.alloc_semaphore(nm)
            k._wait("pool", k._deps([t_in], [t_out]))
            ins = nc.gpsimd.collective_compute("AllGather", ALU.bypass, replica_groups=RG, ins=[in_ap], outs=[out_ap])
            ins.then_inc(k.sems[nm], 1)
            k._mark((nm, 1), [t_in], [t_out])


        esX = contextlib.ExitStack()
        es_all.enter_context(esX)
        SX = mk_alloc(esX)
        gidx32 = SX("gidx32", [128, NCOL * 4], I32)
        gval = SX("gval", [128, NCOL * 4])
        osrc32 = SX("osrc32", [128, 64], I32)
        with contextlib.ExitStack() as esE:
            SE = mk_alloc(esE)
            affT = SE("affT", [16, L])
            selT = SE("selT", [NE, L])
            cumT = SE("cumT", [NE, L])
            onesb = SE("onesb", [NE, L], BF16)
            bs = SE("bs", [NE, 8])
            tokA = SE("tokA", [128, 3, NCH * NCOL])
            key = SE("key", [128, NCH * NCOL])
            tv = SE("tv", [128, NCH * NCOL, 5], BF16)
            r1 = SE("r1", [128, NCH * NCOL])
            oh = [SE("oh%d" % i, [128, CAP], BF16) for i in range(2)]
            idxv = SE("idxv", [128, NCOL * 4, 5])
            idxf = SE("idxf", [128, NCOL * 4])
            for c4 in range(NCH // 4):
                for j in range(4):
                    c = c4 * 4 + j
                    k.op("pe", lambda e: e.transpose(out=PB[c4 % 2].ap[0:16, j * 128:(j + 1) * 128], in_=affAll.ap[:, c, :], identity=ident), reads=[affAll, pk], writes=[PB[c4 % 2]])
                k.op("act", lambda e: e.activation(out=affT.ap[:, c4 * 512:(c4 + 1) * 512], in_=PB[c4 % 2].ap[0:16, :], func=AF.Copy), reads=[PB[c4 % 2]], writes=[affT])
            affG = affT
            k.op("dve", lambda e: e.memset(bs.ap[:, 0:1], 0.0), writes=[bs])
            k.op("dve", lambda e: e.memset(bs.ap[:, 1:2], 2.0), writes=[bs])
            k.op("dve", lambda e: e.memset(onesb.ap[:], 1.0), writes=[onesb])
            for it in range(NBIS):
                k.op("dve", lambda e: e.tensor_scalar(out=bs.ap[:, 2:3], in0=bs.ap[:, 0:1], scalar1=bs.ap[:, 1:2], scalar2=0.5, op0=ALU.add, op1=ALU.mult), reads=[bs], writes=[bs])
                k.op("dve", lambda e: e.tensor_scalar(out=selT.ap[:], in0=affG.ap[:], scalar1=bs.ap[:, 2:3], scalar2=None, op0=ALU.is_ge, op1=ALU.add, accum_out=bs.ap[:, 3:4]),
                     reads=[affG, bs], writes=[selT, bs])
                k.op("dve", lambda e: e.tensor_scalar(out=bs.ap[:, 4:5], in0=bs.ap[:, 3:4], scalar1=float(CAP), scalar2=None, op0=ALU.is_ge), reads=[bs], writes=[bs])
                k.op("dve", lambda e: e.scalar_tensor_tensor(out=bs.ap[:, 5:6], in0=bs.ap[:, 4:5], scalar=2.0, in1=bs.ap[:, 2:3], op0=ALU.mult, op1=ALU.add), reads=[bs], writes=[bs])
                k.op("dve", lambda e: e.scalar_tensor_tensor(out=bs.ap[:, 0:1], in0=bs.ap[:, 2:3], scalar=bs.ap[:, 4:5], in1=bs.ap[:, 0:1], op0=ALU.mult, op1=ALU.max), reads=[bs], writes=[bs])
                k.op("dve", lambda e: e.tensor_tensor(out=bs.ap[:, 1:2], in0=bs.ap[:, 1:2], in1=bs.ap[:, 5:6], op=ALU.min), reads=[bs], writes=[bs])
            k.op("dve", lambda e: e.tensor_scalar(out=selT.ap[:], in0=affG.ap[:], scalar1=bs.ap[:, 0:1], scalar2=None, op0=ALU.is_ge), reads=[affG, bs], writes=[selT])
            k.op("dve", lambda e: e.tensor_tensor_scan(out=cumT.ap[:], data0=onesb.ap[:], data1=selT.ap[:], initial=0.0, op0=ALU.mult, op1=ALU.add), reads=[onesb, selT], writes=[cumT])
            k.op("dve", lambda e: e.tensor_tensor(out=affG.ap[:], in0=affG.ap[:], in1=selT.ap[:], op=ALU.mult), reads=[affG, selT], writes=[affG])
            selm = pk.ap[0:NE, PK_SELM:PK_SELM + NCOL]
            for w, srcT in enumerate((cumT, selT, affG)):
                for c in range(NCH):
                    bb = 2 + 2 * w + c // 16
                    oc = (c % 16) * 32
                    k.op("pe", lambda e: e.matmul(out=PB[bb].ap[:, oc:oc + NCOL], lhsT=srcT.ap[:, c * 128:(c + 1) * 128], rhs=selm, start=True, stop=True), reads=[srcT, pk], writes=[PB[bb]])
                k.op("act", lambda e: e.activation(out=tokA.ap[:, w, :].rearrange("p (c x) -> p c x", x=NCOL), in_=pbf(2 + 2 * w, 2).rearrange("p (c x) -> p c x", x=32)[:, :, 0:NCOL], func=AF.Copy), reads=[PB[2 + 2 * w], PB[3 + 2 * w]], writes=[tokA])
            k.op("dve", lambda e: e.tensor_tensor(out=key.ap[:], in0=tokA.ap[:, 0, :], in1=tokA.ap[:, 1, :], op=ALU.mult), reads=[tokA], writes=[key])
            k.op("dve", lambda e: e.tensor_scalar(out=key.ap[:], in0=key.ap[:], scalar1=-1.0, scalar2=None, op0=ALU.add), reads=[key], writes=[key])
            k.op("pool", lambda e: e.iota(tv.ap[:, :, 0].rearrange("p (c e) -> p c e", e=NCOL), pattern=[[1, NCH], [0, NCOL]], base=0, channel_multiplier=0, allow_small_or_imprecise_dtypes=True), writes=[tv])
            k.op("pool", lambda e: e.iota(tv.ap[:, :, 1], pattern=[[0, NCH * NCOL]], base=0, channel_multiplier=1, allow_small_or_imprecise_dtypes=True), writes=[tv])
            k.op("dve", lambda e: e.tensor_copy(out=tv.ap[:, :, 2], in_=tokA.ap[:, 2, :]), reads=[tokA], writes=[tv])
            k.op("dve", lambda e: e.tensor_tensor(out=r1.ap[:], in0=tokA.ap[:, 2, :], in1=tv.ap[:, :, 2], op=ALU.subtract), reads=[tokA, tv], writes=[r1])
            k.op("dve", lambda e: e.tensor_copy(out=tv.ap[:, :, 3], in_=r1.ap[:]), reads=[r1], writes=[tv])
            k.op("dve", lambda e: e.tensor_tensor(out=r1.ap[:], in0=r1.ap[:], in1=tv.ap[:, :, 3], op=ALU.subtract), reads=[r1, tv], writes=[r1])
            k.op("dve", lambda e: e.tensor_copy(out=tv.ap[:, :, 4], in_=r1.ap[:]), reads=[r1], writes=[tv])
            first = True
            n_oh = 0
            for col in range(NCOL):
                for c in range(NCH):
                    o_t = oh[n_oh % 2]
                    n_oh += 1
                    kc = c * NCOL + col
                    k.op("dve", lambda e: e.tensor_scalar(out=o_t.ap[:], in0=pk.ap[:, PK_IOTA:PK_IOTA + CAP], scalar1=key.ap[:, kc:kc + 1], scalar2=None, op0=ALU.is_equal), reads=[pk, key], writes=[o_t])
                    for jb in range(4):
                        oc = (col * 4 + jb) * 5
                        k.op("pe", lambda e: e.matmul(out=PB[0].ap[:, oc:oc + 5], lhsT=o_t.ap[:, jb * 128:(jb + 1) * 128], rhs=tv.ap[:, kc, :], start=first, stop=(col == NCOL - 1 and c == NCH - 1 and jb == 3)),
                             reads=[o_t, tv], writes=[PB[0]])
                        first = False
            k.op("act", lambda e: e.activation(out=idxv.ap[:].rearrange("p a b -> p (a b)"), in_=PB[0].ap[:, 0:NCOL * 20], func=AF.Copy), reads=[PB[0]], writes=[idxv])
            k.op("dve", lambda e: e.scalar_tensor_tensor(out=idxf.ap[:], in0=idxv.ap[:, :, 0], scalar=128.0, in1=idxv.ap[:, :, 1], op0=ALU.mult, op1=ALU.add), reads=[idxv], writes=[idxf])
            k.op("dve", lambda e: e.tensor_copy(out=gidx32.ap[:], in_=idxf.ap[:]), reads=[idxf], writes=[gidx32])
            k.op("dve", lambda e: e.tensor_tensor(out=gval.ap[:], in0=idxv.ap[:, :, 2], in1=idxv.ap[:, :, 3], op=ALU.add), reads=[idxv], writes=[gval])
            k.op("dve", lambda e: e.tensor_tensor(out=gval.ap[:], in0=gval.ap[:], in1=idxv.ap[:, :, 4], op=ALU.add), reads=[gval, idxv], writes=[gval])
            if dbg and stage == 4:
                o = dbg_out("d_idx", [128, NCOL * 4], I32)
                k.dma("sp", o[:, :], gidx32.ap[:], gidx32, reads=[gidx32])
                o = dbg_out("d_gval", [128, NCOL * 4])
                k.dma("sp", o[:, :], gval.ap[:], gval, reads=[gval])
                o = dbg_out("d_bs", [NE, 8])
                k.dma("sp", o[:, :], bs.ap[:], bs, reads=[bs])
                k.barrier()
                return nc, dbg_d
            k.barrier()

        with contextlib.ExitStack() as esF:
            SF = mk_alloc(esF)
            xg = [SF("xg%d" % i, [128, D], BF16) for i in range(4)]
            xgT = SF("xgT", [128, 16, CAP], BF16)
            wq = [SF("wq%d" % i, [128, 16, 256], BF16) for i in range(6)]
            actT = SF("actT", [128, 16, CAP], BF16)
            sg = [SF("sg%d" % i, [128, CAP]) for i in range(2)]
            ost = [SF("ost%d" % i, [128, D], BF16) for i in range(4)]
            wqc = [0]

            def wload2(src_ap):
                t = wq[wqc[0] % 6]
                wqc[0] += 1
                k.dma("pool", t.ap[:], src_ap.rearrange("(kb p) c -> p kb c", p=128), t, writes=[t])
                return t

            pbc = [0]

            def npb():
                b = pbc[0] % 8
                pbc[0] += 1
                return b
            n_e = 2 if (dbg and stage == 5) else NE
            def gather(col):
                for jb in range(4):
                    ic = col * 4 + jb
                    k.dma("pool", None, None, xg[jb], reads=[gidx32, t_hx2all], writes=[xg[jb]],
                          fn=lambda e: e.indirect_dma_start(out=xg[jb].ap[:], out_offset=None, in_=hx2_s[:, :],
                                                            in_offset=bass.IndirectOffsetOnAxis(ap=gidx32.ap[:, ic:ic + 1], axis=0)))

            def scatter(col):
                for jb in range(4):
                    sc = col * 4 + jb
                    k.dma("pool", None, None, t_ys, reads=[ost[jb], gidx32], writes=([t_ys] if jb == 0 else []),
                          fn=lambda e: e.indirect_dma_start(out=y_s[:, :], out_offset=bass.IndirectOffsetOnAxis(ap=gidx32.ap[:, sc:sc + 1], axis=0), in_=ost[jb].ap[:], in_offset=None, compute_op=ALU.add))
                t_ys.root.w = (t_ys.root.dsem, 16 * t_ys.root.dcnt)
                t_ys.root.r = []

            gather(0)
            for le in range(n_e):
                for b_ in range(1):
                    col = le
                    for kb in range(16):
                        bb = npb()
                        for jb in range(4):
                            k.op("pe", lambda e: e.transpose(out=pbh(bb)[:, jb * 128:(jb + 1) * 128], in_=xg[jb].ap[:, kb * 128:(kb + 1) * 128], identity=idb.ap[:]), reads=[xg[jb], idb], writes=[PB[bb]])
                        k.op("act" if kb % 2 == 0 else "dve",
                             (lambda e: e.activation(out=xgT.ap[:, kb, :], in_=pbh(bb)[:, 0:CAP], func=AF.Copy)) if kb % 2 == 0 else (lambda e: e.tensor_copy(out=xgT.ap[:, kb, :], in_=pbh(bb)[:, 0:CAP])),
                             reads=[PB[bb]], writes=[xgT])
                    if le + 1 < n_e:
                        gather(le + 1)
                    if le > 0:
                        scatter(le - 1)
                    for fg in range(8):
                        w1t = wload2(w_e1_d[le, :, fg * 256:(fg + 1) * 256])
                        w3t = wload2(w_e3_d[le, :, fg * 256:(fg + 1) * 256])
                        for j in range(2):
                            fb = fg * 2 + j
                            bg, bu = npb(), npb()
                            for kb in range(16):
                                k.op("pe", lambda e: e.matmul(out=PB[bg].ap[:, :], lhsT=w1t.ap[:, kb, j * 128:(j + 1) * 128], rhs=xgT.ap[:, kb, :], start=(kb == 0), stop=(kb == 15)), reads=[w1t, xgT], writes=[PB[bg]])
                            for kb in range(16):
                                k.op("pe", lambda e: e.matmul(out=PB[bu].ap[:, :], lhsT=w3t.ap[:, kb, j * 128:(j + 1) * 128], rhs=xgT.ap[:, kb, :], start=(kb == 0), stop=(kb == 15)), reads=[w3t, xgT], writes=[PB[bu]])
                            sgt = sg[fb % 2]
                            k.op("act", lambda e: e.activation(out=sgt.ap[:], in_=PB[bg].ap[:, :], func=AF.Silu), reads=[PB[bg]], writes=[sgt])
                            k.op("dve", lambda e: e.tensor_tensor(out=actT.ap[:, fb, :], in0=PB[bu].ap[:, :], in1=sgt.ap[:], op=ALU.mult), reads=[PB[bu], sgt], writes=[actT])
                    for dg in range(8):
                        w2t = wload2(w_e2_d[le, :, dg * 256:(dg + 1) * 256])
                        for jb in range(4):
                            bo = npb()
                            for fb in range(16):
                                k.op("pe", lambda e: e.matmul(out=PB[bo].ap[:, 0:256], lhsT=actT.ap[:, fb, jb * 128:(jb + 1) * 128], rhs=w2t.ap[:, fb, :], start=(fb == 0), stop=(fb == 15)), reads=[actT, w2t], writes=[PB[bo]])
                            ic = col * 4 + jb
                            k.op("act", lambda e: e.activation(out=ost[jb].ap[:, dg * 256:(dg + 1) * 256], in_=PB[bo].ap[:, 0:256], func=AF.Copy, scale=gval.ap[:, ic:ic + 1]), reads=[PB[bo], gval], writes=[ost[jb]])
            scatter(n_e - 1)
            k.barrier()

        with contextlib.ExitStack() as esG:
            SG = mk_alloc(esG)
            yt = [SG("yfin%d" % i, [128, D]) for i in range(2)]
            x1t = [SG("x1t%d" % i, [128, D]) for i in range(2)]
            G2 = SG("G2f", [128, D])
            st2 = SG("st2", [128, 8])
            jk = SG("jk", [128, D], BF16)
            k.dma("sp", G2.ap[:], g2_s[:, :], G2, writes=[G2])
            for c in range(NCH):
                y_t, x_t = yt[c % 2], x1t[c % 2]
                k.dma("sp", y_t.ap[:], y_s[c * 128:(c + 1) * 128, :], y_t, reads=[t_ys], writes=[y_t])
                k.dma("sp", x_t.ap[:], x1_s[c * 128:(c + 1) * 128, :], x_t, writes=[x_t])
                k.op("act", lambda e: e.activation(out=jk.ap[:], in_=y_t.ap[:], func=AF.Square, accum_out=st2.ap[:, 0:1]), reads=[y_t], writes=[jk, st2])
                k.op("act", lambda e: e.activation(out=st2.ap[:, 1:2], in_=st2.ap[:, 0:1], func=AF.Sqrt, scale=1.0 / D, bias=EPS), reads=[st2], writes=[st2])
                k.op("dve", lambda e: e.reciprocal(out=st2.ap[:, 2:3], in_=st2.ap[:, 1:2]), reads=[st2], writes=[st2])
                k.op("dve", lambda e: e.tensor_tensor(out=y_t.ap[:], in0=y_t.ap[:], in1=G2.ap[:], op=ALU.mult), reads=[y_t, G2], writes=[y_t])
                k.op("dve", lambda e: e.scalar_tensor_tensor(out=y_t.ap[:], in0=y_t.ap[:], scalar=st2.ap[:, 2:3], in1=x_t.ap[:], op0=ALU.mult, op1=ALU.add), reads=[y_t, st2, x_t], writes=[y_t])
                k.dma("sp", out_d[c * 128:(c + 1) * 128, :], y_t.ap[:], y_t, reads=[y_t])
            if dbg:
                o = dbg_out("d_ys", [L, D])
                tt2 = T("d_ys", o)
                k.dma("sp", o[:, :], y_s[:, :], tt2, reads=[t_ys], writes=[tt2])
            k.barrier()

    return nc, dbg_d


def _fm(v, nb):
    return np.ascontiguousarray(np.asarray(v, np.float32).reshape(nb, 128).T)


def _host_inputs(r, x, c, ctx, c_ctx, w_mod, b_mod, norm_pre_mix, norm_post_mix, w_in, ssd_conv_w,
                 ssd_conv_b, dt_bias, a_log, ssd_d, ssd_norm_w, w_ssd_out, sc_conv_w, w_sc_out,
                 b_gate, w_o, norm_pre_ffn, norm_post_ffn, w_router, w_e1, w_e3, w_e2):
    f = np.float32
    b = r % 4
    pk = np.zeros((128, NPK), f)
    ii = np.arange(128)
    pk[:, PK_ID:PK_ID + 128] = np.eye(128, dtype=f)
    pk[:, PK_SU:PK_SU + 128] = (ii[:, None] > ii[None, :])
    pk[:, PK_SL:PK_SL + 128] = (ii[:, None] < ii[None, :])
    pk[:, PK_TLE:PK_TLE + 128] = (ii[:, None] <= ii[None, :])
    pk[:, PK_TGE:PK_TGE + 128] = (ii[:, None] >= ii[None, :])
    pk[:, PK_ONE:PK_ONE + 128] = 1.0
    cv = np.stack([_fm(c[b], 16), _fm(c_ctx, 16)], axis=2).reshape(128, 32)
    pk[:, PK_C:PK_C + 32] = cv
    pk[:, PK_BMOD:PK_BMOD + 16] = _fm(b_mod[0, 0:D], 16)
    pk[:, PK_BMOD + 16:PK_BMOD + 32] = _fm(b_mod[0, D:2 * D], 16)
    pk[:, PK_NPM:PK_NPM + 16] = _fm(norm_pre_mix[0], 16)
    pk[:, PK_SNW:PK_SNW + 16] = _fm(ssd_norm_w[0], 16)
    for t in range(3):
        pk[:, PK_CW + t * 24:PK_CW + (t + 1) * 24] = _fm(ssd_conv_w[0, t], 24)
        pk[:, PK_SCW + t * 16:PK_SCW + (t + 1) * 16] = _fm(sc_conv_w[0, t], 16)
    pk[:, PK_CB:PK_CB + 24] = _fm(ssd_conv_b[0], 24)
    pk[:, PK_BG:PK_BG + 32] = _fm(b_gate[0], 32)
    pk[:, PK_DTB:PK_DTB + 64] = np.asarray(dt_bias[0], f).reshape(1, 64)
    pk[:, PK_ALOG:PK_ALOG + 64] = np.asarray(a_log[0], f).reshape(1, 64)
    pk[:, PK_SSD_D:PK_SSD_D + 32] = np.asarray(ssd_d[0], f).reshape(1, 32)
    pk[:, PK_WR:PK_WR + 256] = np.asarray(w_router[0], f).reshape(16, 128, 16).transpose(1, 0, 2).reshape(128, 256)
    pk[:, PK_IOTA:PK_IOTA + 512] = np.arange(512, dtype=f)[None, :]
    pk[:, PK_PIDX] = ii
    pk[0:NE, PK_SELM:PK_SELM + NE] = np.eye(NE, dtype=f)
    reps = np.empty((128, 7 * D), f)
    reps[:, 0:D] = np.asarray(norm_post_mix[0], f)[None, :]
    reps[:, D:2 * D] = np.asarray(norm_pre_ffn[0], f)[None, :]
    reps[:, 2 * D:3 * D] = np.asarray(norm_post_ffn[0], f)[None, :]
    reps[:, 3 * D:7 * D] = np.asarray(b_mod[0, 2 * D:6 * D], f)[None, :]
    return {
        "x": np.ascontiguousarray(x[b]), "ctx": np.ascontiguousarray(ctx[b]),
        "w_in": np.ascontiguousarray(w_in[0]), "w_mod": np.ascontiguousarray(w_mod[0]),
        "w_ssd_out": np.ascontiguousarray(w_ssd_out[0]), "w_sc_out": np.ascontiguousarray(w_sc_out[0]),
        "w_o": np.ascontiguousarray(w_o[0]),
        "w_e1": np.ascontiguousarray(w_e1[0]), "w_e3": np.ascontiguousarray(w_e3[0]), "w_e2": np.ascontiguousarray(w_e2[0]),
        "pk": pk, "reps": reps, "idb": np.eye(128, dtype=np.float32).astype(ml_dtypes.bfloat16),
    }


def kernel(**inputs):
    inputs = {k_: np.asarray(v) for k_, v in inputs.items()}
    nc, _ = build_program()
    in_maps = [_host_inputs(r, **inputs) for r in range(8)]
    res = run_bass_kernel_spmd(nc, in_maps, core_ids=list(range(8)))
    out = np.stack([np.asarray(res.results[b]["out"], np.float32) for b in range(4)], axis=0)
    return out
```

```python
import contextlib
import numpy as np
import ml_dtypes
import concourse.bass as bass
import concourse.mybir as mybir
from concourse.bass_utils import run_bass_kernel_spmd

F32 = mybir.dt.float32
BF16 = mybir.dt.bfloat16
I32 = mybir.dt.int32
AF = mybir.ActivationFunctionType
ALU = mybir.AluOpType

D = 2048
L = 4096
NCH = L // 128
CTXL = 256
PROJ = 15424
C_Z, C_XBC, C_DT, C_SCB, C_SCC, C_SCV, C_GS, C_GC = 0, 2048, 5120, 5184, 7232, 9280, 11328, 13376
NE = 16
CAP = 512
EPS = 1e-6
TS = 256
NBIS = 34

PK_ID, PK_SU, PK_SL, PK_TLE, PK_TGE, PK_ONE = 0, 128, 256, 384, 512, 640
PK_C, PK_BMOD, PK_NPM, PK_SNW, PK_CW, PK_CB, PK_SCW, PK_BG = 768, 800, 832, 848, 864, 936, 960, 1008
PK_DTB, PK_ALOG, PK_SSD_D, PK_WR, PK_IOTA, PK_PIDX = 1040, 1104, 1168, 1200, 1456, 1968
PK_SELM, PK_GOFF, PK_OSRC, NPK = 1984, 2008, 2104, 2168


class T:
    __slots__ = ("name", "ap", "w", "r", "dsem", "dcnt", "root")

    def __init__(self, name, ap, root=None):
        self.name = name
        self.ap = ap
        self.w = None
        self.r = []
        self.dsem = None
        self.dcnt = 0
        self.root = root if root is not None else self


class K:
    def __init__(self, nc):
        self.nc = nc
        self.engs = {"pe": nc.tensor, "act": nc.scalar, "dve": nc.vector, "pool": nc.gpsimd, "sp": nc.sync}
        self.sems = {}
        self.cnt = {}
        for k in ("pe", "act", "dve", "pool"):
            self.sems[k] = nc.alloc_semaphore("s_" + k)
            self.cnt[k] = 0
        self.waited = {}
        self.nsem = 0
        self.dsems = []

    def _deps(self, reads, writes):
        deps = {}

        def add(d):
            if d is None:
                return
            k, v = d
            if deps.get(k, 0) < v:
                deps[k] = v
        for t in reads:
            add(t.root.w)
        for t in writes:
            add(t.root.w)
            for r in t.root.r:
                add(r)
        return deps

    def _wait(self, ek, deps):
        eng = self.engs[ek]
        for k, v in deps.items():
            if ek == "pe" and k == "pe":
                continue
            if self.waited.get((ek, k), 0) >= v:
                continue
            eng.wait_ge(self.sems[k], v)
            self.waited[(ek, k)] = v

    def _mark(self, me, reads, writes):
        for t in reads:
            t.root.r.append(me)
        for t in writes:
            t.root.w = me
            t.root.r = []

    def op(self, ek, fn, reads=(), writes=()):
        self._wait(ek, self._deps(reads, writes))
        ins = fn(self.engs[ek])
        self.cnt[ek] += 1
        ins.then_inc(self.sems[ek], 1)
        self._mark((ek, self.cnt[ek]), reads, writes)
        return ins

    def _dsem(self, semt):
        semt = semt.root
        if semt.dsem is None:
            semt.dsem = "d%d_%s" % (self.nsem, semt.name)
            self.sems[semt.dsem] = self.nc.alloc_semaphore(semt.dsem)
            self.dsems.append(semt)
            self.nsem += 1
        return semt

    def dma(self, qk, out_ap, in_ap, semt, reads=(), writes=(), fn=None, **kw):
        semt = self._dsem(semt)
        self._wait(qk, self._deps(reads, writes))
        if fn is None:
            ins = self.engs[qk].dma_start(out=out_ap, in_=in_ap, **kw)
        else:
            ins = fn(self.engs[qk])
        semt.dcnt += 1
        ins.then_inc(self.sems[semt.dsem], 16)
        me = (semt.dsem, 16 * semt.dcnt)
        self._mark(me, reads, writes)
        return me

    def barrier(self):
        tot = {k: self.cnt[k] for k in ("pe", "act", "dve", "pool")}
        for t in self.dsems:
            tot[t.dsem] = 16 * t.dcnt
        for ek in ("pe", "act", "dve", "pool", "sp"):
            eng = self.engs[ek]
            for k, v in tot.items():
                if v == 0 or self.waited.get((ek, k), 0) >= v:
                    continue
                eng.wait_ge(self.sems[k], v)
                self.waited[(ek, k)] = v


def build_program(stage=99, dbg=False, one_core=False):
    nc = bass.Bass("TRN2", target_bir_lowering=False)
    k = K(nc)

    def din(name, shape, dt=F32):
        return nc.dram_tensor(name, list(shape), dt, kind="ExternalInput").ap()

    x_d = din("x", [L, D])
    ctx_d = din("ctx", [CTXL, D])
    w_in_d = din("w_in", [D, PROJ])
    w_mod_d = din("w_mod", [D, 6 * D])
    w_ssd_d = din("w_ssd_out", [D, D])
    w_sc_d = din("w_sc_out", [D, D])
    w_o_d = din("w_o", [D, D])
    we_shape = [NE, D, D] if stage >= 5 else [2, 8, 8]
    w_e1_d = din("w_e1", we_shape)
    w_e3_d = din("w_e3", we_shape)
    w_e2_d = din("w_e2", we_shape)
    pk_d = din("pk", [128, NPK])
    reps_d = din("reps", [128, 7 * D])
    idb_d = din("idb", [128, 128], BF16)
    out_d = nc.dram_tensor("out", [L, D], F32, kind="ExternalOutput").ap()
    hb_s = nc.dram_tensor("hb_s", [NCH, 128, D], BF16, kind="Internal").ap()
    x1_s = nc.dram_tensor("x1_s", [L, D], F32, kind="Internal").ap()
    hx2_s = nc.dram_tensor("hx2_s", [L, D], BF16, kind="Internal").ap()
    y_s = nc.dram_tensor("y_s", [L, D], F32, kind="Internal").ap()
    g2_s = nc.dram_tensor("g2_s", [128, D], F32, kind="Internal").ap()
    win16 = nc.dram_tensor("win16", [D, PROJ], BF16, kind="Internal").ap()
    wssd16 = nc.dram_tensor("wssd16", [D, D], BF16, kind="Internal").ap()
    wsc16 = nc.dram_tensor("wsc16", [D, D], BF16, kind="Internal").ap()
    wo16 = nc.dram_tensor("wo16", [D, D], BF16, kind="Internal").ap()
    dbg_d = {}

    def dbg_out(name, shape, dt=F32):
        dbg_d[name] = nc.dram_tensor(name, list(shape), dt, kind="ExternalOutput").ap()
        return dbg_d[name]

    es_all = contextlib.ExitStack()
    with es_all:
        def mk_alloc(es):
            def S(name, shape, dt=F32):
                return T(name, es.enter_context(nc.sbuf_tensor("sb_" + name, list(shape), dt)))
            return S
        SP = mk_alloc(es_all)
        psum = es_all.enter_context(nc.psum_tensor("psum", [128, 4096], F32))
        PB = [T("pb%d" % i, psum[:, i * 512:(i + 1) * 512]) for i in range(8)]

        def pbf(i, n=1):
            return psum[:, i * 512:(i + n) * 512]

        def pbh(i, n=1):
            return psum[:, i * 512:(i + n) * 512].bitcast(BF16)

        pk = SP("pk", [128, NPK])
        idb = SP("idb", [128, 128], BF16)
        G1 = SP("G1", [128, D])
        A2 = SP("A2", [128, D])
        B2 = SP("B2", [128, D])
        modfm = SP("modfm", [128, 64])
        arep = SP("arep", [128, 64])
        wdt = SP("wdt", [128, 16, 64], BF16)
        affAll = SP("affAll", [128, NCH, NE])
        hf = SP("hf", [128, D])
        h16 = SP("h16", [128, D], BF16)

        def pkc(c0, n):
            return pk.ap[:, c0:c0 + n]
        ident = pkc(PK_ID, 128)
        SU, SL, TLE, TGE, ONES = pkc(PK_SU, 128), pkc(PK_SL, 128), pkc(PK_TLE, 128), pkc(PK_TGE, 128), pkc(PK_ONE, 128)

        k.dma("sp", pk.ap[:], pk_d[:, :], pk, writes=[pk])
        k.dma("sp", idb.ap[:], idb_d[:, :], idb, writes=[idb])
        k.dma("pool", wdt.ap[:], w_in_d[:, C_DT:C_DT + 64].rearrange("(kb p) c -> p kb c", p=128), wdt, writes=[wdt])
        t_wA, t_wB, t_wC = T("winA", win16), T("winB", win16), T("winC", win16)
        t_wssd, t_wsc, t_wo = T("wssd16", wssd16), T("wsc16", wsc16), T("wo16", wo16)

        def cast_region(dst, srcw, c0, c1, piece, rows_per, tt):
            for r0 in range(0, D, rows_per):
                k.dma("pool", dst[r0:r0 + rows_per, c0:c1].rearrange("r (a c) -> r a c", c=piece),
                      srcw[r0:r0 + rows_per, c0:c1].rearrange("r (a c) -> r a c", c=piece), tt, writes=[tt])
        cast_region(win16, w_in_d, C_XBC, C_DT, 1536, 512, t_wA)
        cast_region(win16, w_in_d, C_Z, C_XBC, 2048, 1024, t_wB)
        cast_region(win16, w_in_d, C_SCB, PROJ, 2048, 256, t_wC)
        cast_region(wssd16, w_ssd_d, 0, D, 2048, 1024, t_wssd)
        cast_region(wsc16, w_sc_d, 0, D, 2048, 1024, t_wsc)
        cast_region(wo16, w_o_d, 0, D, 2048, 1024, t_wo)

        with contextlib.ExitStack() as es0:
            S0 = mk_alloc(es0)
            csil = S0("csil", [128, 32])
            crep = S0("crep", [128, 16, 128])
            wm = [S0("wm%d" % i, [128, 16, 512]) for i in range(2)]
            brp = [S0("brp%d" % i, [128, 512]) for i in range(2)]
            nrp = [S0("nrp%d" % i, [128, 512]) for i in range(2)]
            tmp0 = S0("tmp0", [128, 512])
            G2 = S0("G2", [128, D])
            k.op("act", lambda e: e.activation(out=csil.ap[:], in_=pkc(PK_C, 32), func=AF.Silu), reads=[pk], writes=[csil])
            k.op("dve", lambda e: e.tensor_copy(out=crep.ap[:], in_=csil.ap[:, 0:32].rearrange("p (kb w) -> p kb w", w=2)[:, :, 0:1].to_broadcast([128, 16, 128])),
                 reads=[csil], writes=[crep])
            k.op("act", lambda e: e.activation(out=arep.ap[:], in_=pkc(PK_ALOG, 64), func=AF.Exp), reads=[pk], writes=[arep])
            k.op("dve", lambda e: e.tensor_scalar(out=arep.ap[:], in0=arep.ap[:], scalar1=-1.0, scalar2=None, op0=ALU.mult), reads=[arep], writes=[arep])
            ngrp = 24
            for g in range(ngrp):
                w_t = wm[g % 2]
                k.dma("sp", w_t.ap[:], w_mod_d[:, g * 512:(g + 1) * 512].rearrange("(kb p) c -> p kb c", p=128), w_t, writes=[w_t])
                if g < 8:
                    pb = PB[g % 2]
                    for j in range(4):
                        for kb in range(16):
                            k.op("pe", lambda e: e.matmul(out=pb.ap[:, 2 * j:2 * j + 2], lhsT=w_t.ap[:, kb, j * 128:(j + 1) * 128],
                                                          rhs=csil.ap[:, 2 * kb:2 * kb + 2], start=(kb == 0), stop=(kb == 15)),
                                 reads=[w_t, csil], writes=[pb])
                    db0 = (g % 4) * 4
                    boff = PK_BMOD + (0 if g < 4 else 16) + db0
                    for which in range(2):
                        dst0 = which * 32 + (16 if g < 4 else 0) + db0
                        k.op("dve", lambda e: e.tensor_tensor(out=modfm.ap[:, dst0:dst0 + 4],
                                                              in0=pb.ap[:, 0:8].rearrange("p (j w) -> p w j", w=2)[:, which, :],
                                                              in1=pk.ap[:, boff:boff + 4], op=ALU.add),
                             reads=[pb, pk], writes=[modfm])
                else:
                    gi = g - 8
                    ty, cg = gi // 4, gi % 4
                    b_t, n_t = brp[g % 2], nrp[g % 2]
                    k.dma("sp", b_t.ap[:], reps_d[:, 3 * D + gi * 512: 3 * D + (gi + 1) * 512], b_t, writes=[b_t])
                    if ty != 1:
                        nsel = {0: 0, 2: 1, 3: 2}[ty]
                        k.dma("sp", n_t.ap[:], reps_d[:, nsel * D + cg * 512: nsel * D + (cg + 1) * 512], n_t, writes=[n_t])
                    pb = PB[2 + g % 2]
                    for kb in range(16):
                        k.op("pe", lambda e: e.matmul(out=pb.ap[:, :], lhsT=crep.ap[:, kb, :], rhs=w_t.ap[:, kb, :], start=(kb == 0), stop=(kb == 15)),
                             reads=[w_t, crep], writes=[pb])
                    dst = {0: G1, 1: B2, 2: A2, 3: G2}[ty]
                    dsl = dst.ap[:, cg * 512:(cg + 1) * 512]
                    if ty == 1:
                        k.op("dve", lambda e: e.tensor_tensor(out=dsl, in0=pb.ap[:, :], in1=b_t.ap[:], op=ALU.add), reads=[pb, b_t], writes=[dst])
                    else:
                        k.op("dve", lambda e: e.tensor_tensor(out=tmp0.ap[:], in0=pb.ap[:, :], in1=b_t.ap[:], op=ALU.add), reads=[pb, b_t], writes=[tmp0])
                        if ty == 2:
                            k.op("dve", lambda e: e.scalar_tensor_tensor(out=dsl, in0=tmp0.ap[:], scalar=1.0, in1=n_t.ap[:], op0=ALU.add, op1=ALU.mult),
                                 reads=[tmp0, n_t], writes=[dst])
                        else:
                            k.op("dve", lambda e: e.tensor_tensor(out=dsl, in0=tmp0.ap[:], in1=n_t.ap[:], op=ALU.mult), reads=[tmp0, n_t], writes=[dst])
            for which in range(2):
                k.op("dve", lambda e: e.scalar_tensor_tensor(out=modfm.ap[:, which * 32:which * 32 + 16], in0=modfm.ap[:, which * 32:which * 32 + 16],
                                                             scalar=1.0, in1=pkc(PK_NPM, 16), op0=ALU.add, op1=ALU.mult),
                     reads=[modfm, pk], writes=[modfm])
            k.dma("sp", g2_s[:, :], G2.ap[:], G2, reads=[G2])
            k.barrier()
        if dbg and stage == 0:
            o1 = dbg_out("d_modfm", [128, 64])
            o2 = dbg_out("d_G1", [128, D])
            o3 = dbg_out("d_A2", [128, D])
            k.dma("sp", o1[:, :], modfm.ap[:], modfm, reads=[modfm])
            k.dma("sp", o2[:, :], G1.ap[:], G1, reads=[G1])
            k.dma("sp", o3[:, :], A2.ap[:], A2, reads=[A2])
        if stage == 0:
            k.barrier()
            return nc, dbg_d

        esM = contextlib.ExitStack()
        with esM:
            SM = mk_alloc(esM)
            NQ = TS // 128
            xc1 = SM("xc", [128, D])
            xc = [xc1, xc1]
            xn = [xc1, xc1]
            stat = SM("stat", [128, 16])
            hxT = SM("hxT", [128, 16, TS], BF16)
            wst = [SM("wst%d" % i, [128, 16, 256], BF16) for i in range(3)]
            xsT = SM("xsT", [128, 16, TS], BF16)
            BT = SM("BT", [128, 4, TS], BF16)
            CT = SM("CT", [128, 4, TS], BF16)
            ctmp = [SM("ctmp%d" % i, [128, TS]) for i in range(2)]
            zs = SM("zs", [128, NQ, D], BF16)
            cv = SM("cv", [128, 16, TS])
            scyT = SM("scyT", [128, 16, TS], BF16)
            gcT = SM("gcT", [128, 16, TS], BF16)
            ynT = SM("ynT", [128, 16, TS], BF16)
            xs_tok = [SM("xs_tok%d" % i, [128, D], BF16) for i in range(NQ)]
            B_tok = [SM("B_tok%d" % i, [128, 512], BF16) for i in range(NQ)]
            dtt = [SM("dtt%d" % i, [128, 64]) for i in range(NQ)]
            dta = [SM("dta%d" % i, [128, 64]) for i in range(NQ)]
            sm = SM("sm", [128, 128])
            wgt = SM("wgt", [128, 32])
            xdt_f = SM("xdt_f", [128, D], BF16)
            xdt_b = SM("xdt_b", [128, D], BF16)
            xD = SM("xD", [128, D], BF16)
            Rf = SM("Rf", [128, 1024])
            Rb = SM("Rb", [128, 1024])
            Lf = SM("Lf", [128, 1024], BF16)
            Lb = SM("Lb", [128, 1024], BF16)
            cbm = SM("cbm", [128, 256], BF16)
            yt1 = SM("yt1", [128, 512])
            yt2 = SM("yt2", [128, 512])
            yz = SM("yz", [128, D])
            yzn = SM("yzn", [128, D], BF16)
            hb1 = SM("hb16", [128, D], BF16)
            hb16 = [hb1, hb1]
            lg = SM("lg", [128, 32])
            mergedT = T("mergedT", xsT.ap, root=xsT)
            mtmp = T("mtmp", cv.ap, root=cv)
            outx = T("outx", yz.ap, root=yz)
            x1buf = T("x1buf", cv.ap[:, 0:8, :].rearrange("p a b -> p (a b)"), root=cv)
            xw = T("xw", xdt_f.ap, root=xdt_f)
            hx2 = T("hx2", xdt_b.ap, root=xdt_b)
            junk = T("junk", yzn.ap, root=yzn)
            Mf = T("Mf", Lf.ap, root=Lf)
            Mb = T("Mb", Lb.ap, root=Lb)

            wctr = [0]

            def wload(src_ap, treg):
                t = wst[wctr[0] % 3]
                wctr[0] += 1
                k.dma("sp", t.ap[:], src_ap.rearrange("(kb p) c -> p kb c", p=128), t, reads=[treg], writes=[t])
                return t

            pbctr = [0]

            def next_pb(lo=0, n=8):
                b = lo + pbctr[0] % n
                pbctr[0] += 1
                return b

            def gen_proj_fm(wsrc, col0, nblk, rhsT, tt, evac, treg=None, banks=None):
                for g0 in range(0, nblk, 2):
                    t = wload(wsrc[:, col0 + g0 * 128: col0 + g0 * 128 + 256], treg)
                    for j in range(2):
                        m = g0 + j
                        if banks is None:
                            b = next_pb()
                        else:
                            b = banks[pbctr[0] % len(banks)]
                            pbctr[0] += 1
                        for kb in range(16):
                            k.op("pe", lambda e: e.matmul(out=PB[b].ap[:, 0:tt], lhsT=t.ap[:, kb, j * 128:(j + 1) * 128], rhs=rhsT.ap[:, kb, 0:tt],
                                                          start=(kb == 0), stop=(kb == 15)), reads=[t, rhsT], writes=[PB[b]])
                        evac(m, PB[b])
                        yield

            def proj_fm(*a, **kw):
                for _ in gen_proj_fm(*a, **kw):
                    pass

            def pull(gen, n):
                for _ in range(n):
                    try:
                        next(gen)
                    except StopIteration:
                        return

            def prep_chunk(src_rows, q, a_off):
                xt, xnt = xc[q % 2], xn[q % 2]
                k.dma("sp", xt.ap[:], src_rows, xt, writes=[xt])
                k.op("act", lambda e: e.activation(out=junk.ap[:], in_=xt.ap[:], func=AF.Square, accum_out=stat.ap[:, 0:1]), reads=[xt], writes=[junk, stat])
                k.op("act", lambda e: e.activation(out=stat.ap[:, 1:2], in_=stat.ap[:, 0:1], func=AF.Sqrt, scale=1.0 / D, bias=EPS), reads=[stat], writes=[stat])
                k.op("dve", lambda e: e.reciprocal(out=stat.ap[:, 2:3], in_=stat.ap[:, 1:2]), reads=[stat], writes=[stat])
                k.op("act", lambda e: e.activation(out=xnt.ap[:], in_=xt.ap[:], func=AF.Copy, scale=stat.ap[:, 2:3]), reads=[xt, stat], writes=[xnt])
                b0 = 4 * (q % 2)
                for kb in range(16):
                    b = b0 + kb // 4
                    k.op("pe", lambda e: e.transpose(out=PB[b].ap[:, (kb % 4) * 128:(kb % 4 + 1) * 128], in_=xnt.ap[:, kb * 128:(kb + 1) * 128], identity=ident),
                         reads=[xnt, pk], writes=[PB[b]])
                for kb in range(16):
                    b = b0 + kb // 4
                    k.op("act", lambda e: e.activation(out=hxT.ap[:, kb, q * 128:(q + 1) * 128], in_=PB[b].ap[:, (kb % 4) * 128:(kb % 4 + 1) * 128], func=AF.Identity,
                                                       scale=modfm.ap[:, a_off + kb:a_off + kb + 1], bias=modfm.ap[:, a_off + 16 + kb:a_off + 17 + kb]),
                         reads=[PB[b], modfm], writes=[hxT])

            def conv_evac(dstT, blk0, tt, rl):
                nr = tt // rl

                def f(m, pb):
                    cb = blk0 + m
                    ct = ctmp[m % 2]
                    w0 = pk.ap[:, PK_CW + cb:PK_CW + cb + 1]
                    w1 = pk.ap[:, PK_CW + 24 + cb:PK_CW + 25 + cb]
                    w2 = pk.ap[:, PK_CW + 48 + cb:PK_CW + 49 + cb]
                    bb = pk.ap[:, PK_CB + cb:PK_CB + cb + 1]
                    k.op("act", lambda e: e.activation(out=ct.ap[:, 0:tt], in_=pb.ap[:, 0:tt], func=AF.Identity, scale=w1, bias=bb), reads=[pb, pk], writes=[ct])
                    pv = pb.ap[:, 0:tt].rearrange("p (r c) -> p r c", c=rl)
                    cvw = ct.ap[:, 0:tt].rearrange("p (r c) -> p r c", c=rl)
                    k.op("dve", lambda e: e.scalar_tensor_tensor(out=cvw[:, :, 1:rl], in0=pv[:, :, 0:rl - 1], scalar=w0, in1=cvw[:, :, 1:rl], op0=ALU.mult, op1=ALU.add),
                         reads=[pb, ct, pk], writes=[ct])
                    k.op("dve", lambda e: e.scalar_tensor_tensor(out=cvw[:, :, 0:rl - 1], in0=pv[:, :, 1:rl], scalar=w2, in1=cvw[:, :, 0:rl - 1], op0=ALU.mult, op1=ALU.add),
                         reads=[pb, ct, pk], writes=[ct])
                    if cb < 16:
                        dst, di = dstT[0], cb
                    elif cb < 20:
                        dst, di = dstT[1], cb - 16
                    else:
                        dst, di = dstT[2], cb - 20
                    k.op("act", lambda e: e.activation(out=dst.ap[:, di, 0:tt], in_=ct.ap[:, 0:tt], func=AF.Silu), reads=[ct], writes=[dst])
                return f

            def dt_chunk(q):
                pb = PB[0]
                for kb in range(16):
                    k.op("pe", lambda e: e.matmul(out=pb.ap[:, 0:64], lhsT=hxT.ap[:, kb, q * 128:(q + 1) * 128], rhs=wdt.ap[:, kb, :], start=(kb == 0), stop=(kb == 15)),
                         reads=[hxT, wdt], writes=[pb])
                k.op("dve", lambda e: e.tensor_tensor(out=dtt[q].ap[:], in0=pb.ap[:, 0:64], in1=pkc(PK_DTB, 64), op=ALU.add), reads=[pb, pk], writes=[dtt[q]])
                k.op("act", lambda e: e.activation(out=dtt[q].ap[:], in_=dtt[q].ap[:], func=AF.Exp), reads=[dtt[q]], writes=[dtt[q]])
                k.op("act", lambda e: e.activation(out=dtt[q].ap[:], in_=dtt[q].ap[:], func=AF.Ln, bias=1.0), reads=[dtt[q]], writes=[dtt[q]])
                k.op("dve", lambda e: e.tensor_tensor(out=dta[q].ap[:], in0=dtt[q].ap[:], in1=arep.ap[:], op=ALU.mult), reads=[dtt[q], arep], writes=[dta[q]])

            def to_tok(q, with_b=True):
                for half in range(2):
                    b = 4 + 2 * half
                    for i in range(8):
                        cbk = half * 8 + i
                        bb = b + i // 4
                        k.op("pe", lambda e: e.transpose(out=pbh(bb)[:, (i % 4) * 128:(i % 4 + 1) * 128], in_=xsT.ap[:, cbk, q * 128:(q + 1) * 128], identity=idb.ap[:]),
                             reads=[xsT, idb], writes=[PB[bb]])
                for half in range(2):
                    b = 4 + 2 * half
                    src = psum[:, b * 512:(b + 2) * 512].bitcast(BF16).rearrange("p (b x c) -> p b x c", b=2, x=2)[:, :, 0, :]
                    k.op("act" if half == 0 else "dve",
                         (lambda e: e.activation(out=xs_tok[q].ap[:, half * 1024:(half + 1) * 1024].rearrange("p (b c) -> p b c", b=2), in_=src, func=AF.Copy)) if half == 0 else
                         (lambda e: e.tensor_copy(out=xs_tok[q].ap[:, half * 1024:(half + 1) * 1024].rearrange("p (b c) -> p b c", b=2), in_=src)),
                         reads=[PB[b], PB[b + 1]], writes=[xs_tok[q]])
                if with_b:
                    for g in range(4):
                        k.op("pe", lambda e: e.transpose(out=pbh(3)[:, g * 128:(g + 1) * 128], in_=BT.ap[:, g, q * 128:(q + 1) * 128], identity=idb.ap[:]),
                             reads=[BT, idb], writes=[PB[3]])
                    k.op("dve", lambda e: e.tensor_copy(out=B_tok[q].ap[:], in_=pbh(3)[:, 0:512]), reads=[PB[3]], writes=[B_tok[q]])

            def state_update(q, dr, smt, wcol, dcol):
                k.op("dve", lambda e: e.tensor_tensor(out=wgt.ap[:], in0=smt.ap[:, wcol:wcol + 32], in1=dtt[q].ap[:, dr * 32:(dr + 1) * 32], op=ALU.mult),
                     reads=[smt, dtt[q]], writes=[wgt])
                k.op("pool", lambda e: e.tensor_tensor(out=xw.ap[:].rearrange("p (h c) -> p h c", c=64), in0=xs_tok[q].ap[:].rearrange("p (h c) -> p h c", c=64),
                                                       in1=wgt.ap[:, 0:32].unsqueeze(2).to_broadcast([128, 32, 64]), op=ALU.mult),
                     reads=[xs_tok[q], wgt], writes=[xw])
                for g in range(4):
                    sb_ = (1, 2, 6, 7)[g]
                    k.op("pe", lambda e: e.matmul(out=PB[sb_].ap[:, :], lhsT=B_tok[q].ap[:, g * 128:(g + 1) * 128], rhs=xw.ap[:, g * 512:(g + 1) * 512], start=True, stop=True),
                         reads=[B_tok[q], xw], writes=[PB[sb_]])
                k.op("dve", lambda e: e.tensor_tensor(out=hf.ap[:].rearrange("p (h c) -> p h c", c=64), in0=hf.ap[:].rearrange("p (h c) -> p h c", c=64),
                                                      in1=smt.ap[:, dcol:dcol + 32].unsqueeze(2).to_broadcast([128, 32, 64]), op=ALU.mult),
                     reads=[hf, smt], writes=[hf])
                k.op("dve", lambda e: e.tensor_tensor(out=hf.ap[:, 0:1024], in0=hf.ap[:, 0:1024], in1=pbf(1, 2), op=ALU.add), reads=[hf, PB[1], PB[2]], writes=[hf])
                k.op("dve", lambda e: e.tensor_tensor(out=hf.ap[:, 1024:2048], in0=hf.ap[:, 1024:2048], in1=pbf(6, 2), op=ALU.add), reads=[hf, PB[6], PB[7]], writes=[hf])
                k.op("act", lambda e: e.activation(out=h16.ap[:], in_=hf.ap[:], func=AF.Copy), reads=[hf], writes=[h16])

            def small_state(q, dr):
                tri = SU if dr == 0 else SL
                k.op("pe", lambda e: e.matmul(out=PB[0].ap[:, 0:32], lhsT=tri, rhs=dta[q].ap[:, dr * 32:(dr + 1) * 32], start=True, stop=True), reads=[pk, dta[q]], writes=[PB[0]])
                k.op("pe", lambda e: e.matmul(out=PB[0].ap[:, 32:64], lhsT=ONES, rhs=dta[q].ap[:, dr * 32:(dr + 1) * 32], start=True, stop=True), reads=[pk, dta[q]], writes=[PB[0]])
                k.op("act", lambda e: e.activation(out=sm.ap[:, 0:64], in_=PB[0].ap[:, 0:64], func=AF.Exp), reads=[PB[0]], writes=[sm])

            def states_superchunk(src_d, row0, tt, a_off, rl, dr, chunk_order, store_c0=None):
                nq = tt // 128
                for q in range(nq):
                    prep_chunk(src_d[row0 + q * 128: row0 + (q + 1) * 128, :], q, a_off)
                proj_fm(win16, C_XBC, 20, hxT, tt, conv_evac((xsT, BT, CT), 0, tt, rl), t_wA)
                for q in range(nq):
                    dt_chunk(q)
                    to_tok(q)
                for q in chunk_order:
                    if store_c0 is not None:
                        c = store_c0 + q
                        k.dma("sp", hb_s[c, :, :], h16.ap[:], h16, reads=[h16])
                    small_state(q, dr)
                    state_update(q, dr, sm, 0, 32)

            def zero_state():
                k.op("dve", lambda e: e.memset(hf.ap[:], 0.0), writes=[hf])
                k.op("dve", lambda e: e.memset(h16.ap[:], 0.0), writes=[h16])

            zero_state()
            states_superchunk(ctx_d, 0, CTXL, 32, CTXL, 1, [1, 0])
            nsc = L // TS
            for s in reversed(range(nsc)):
                states_superchunk(x_d, s * TS, TS, 0, 64, 1, list(reversed(range(NQ))), store_c0=s * NQ)
                if stage == 1 and s == nsc - 2:
                    break
            if dbg and stage == 1:
                o = dbg_out("d_hf", [128, D])
                k.dma("sp", o[:, :], hf.ap[:], hf, reads=[hf])
                o = dbg_out("d_xs", [128, D], BF16)
                k.dma("sp", o[:, :], xs_tok[0].ap[:], xs_tok[0], reads=[xs_tok[0]])
                o = dbg_out("d_dt", [128, 64])
                k.dma("sp", o[:, :], dtt[0].ap[:], dtt[0], reads=[dtt[0]])
            if stage == 1:
                k.barrier()
                return nc, dbg_d

            zero_state()
            states_superchunk(ctx_d, 0, CTXL, 32, CTXL, 0, [0, 1])

            k.op("pool", lambda e: e.memset(yz.ap[:], 0.0), writes=[yz])
            for c in range(NCH):
                k.dma("sp", y_s[c * 128:(c + 1) * 128, :], yz.ap[:], yz, reads=[yz])

            def ssd_chunk(s, q):
                c = s * NQ + q
                hbt = hb16[c % 2]
                k.dma("sp", hbt.ap[:], hb_s[c, :, :], hbt, writes=[hbt])
                cs = slice(q * 128, (q + 1) * 128)
                k.op("pe", lambda e: e.matmul(out=PB[0].ap[:, 0:32], lhsT=SU, rhs=dta[q].ap[:, 0:32], start=True, stop=True), reads=[pk, dta[q]], writes=[PB[0]])
                k.op("pe", lambda e: e.matmul(out=PB[0].ap[:, 32:64], lhsT=ONES, rhs=dta[q].ap[:, 0:32], start=True, stop=True), reads=[pk, dta[q]], writes=[PB[0]])
                k.op("pe", lambda e: e.matmul(out=PB[0].ap[:, 64:96], lhsT=TLE, rhs=dta[q].ap[:, 0:32], start=True, stop=True), reads=[pk, dta[q]], writes=[PB[0]])
                k.op("pe", lambda e: e.matmul(out=PB[0].ap[:, 96:128], lhsT=TGE, rhs=dta[q].ap[:, 32:64], start=True, stop=True), reads=[pk, dta[q]], writes=[PB[0]])
                k.op("act", lambda e: e.activation(out=sm.ap[:, 0:128], in_=PB[0].ap[:, 0:128], func=AF.Exp), reads=[PB[0]], writes=[sm])
                x3 = xs_tok[q].ap[:].rearrange("p (h c) -> p h c", c=64)

                def bc(t, c0):
                    return t.ap[:, c0:c0 + 32].unsqueeze(2).to_broadcast([128, 32, 64])
                k.op("pool", lambda e: e.tensor_tensor(out=xdt_f.ap[:].rearrange("p (h c) -> p h c", c=64), in0=x3, in1=bc(dtt[q], 0), op=ALU.mult), reads=[xs_tok[q], dtt[q]], writes=[xdt_f])
                k.op("pool", lambda e: e.tensor_tensor(out=xdt_b.ap[:].rearrange("p (h c) -> p h c", c=64), in0=x3, in1=bc(dtt[q], 32), op=ALU.mult), reads=[xs_tok[q], dtt[q]], writes=[xdt_b])
                k.op("pool", lambda e: e.tensor_tensor(out=xD.ap[:].rearrange("p (h c) -> p h c", c=64), in0=x3, in1=bc(pk, PK_SSD_D), op=ALU.mult), reads=[xs_tok[q], pk], writes=[xD])
                for g in range(4):
                    co = (g % 2) * 128
                    k.op("pe", lambda e: e.matmul(out=PB[0].ap[:, 128 + co:256 + co], lhsT=BT.ap[:, g, cs], rhs=CT.ap[:, g, cs], start=True, stop=True), reads=[BT, CT], writes=[PB[0]])
                    k.op("dve", lambda e: e.tensor_tensor(out=cbm.ap[:, 0:128], in0=PB[0].ap[:, 128 + co:256 + co], in1=TLE, op=ALU.mult), reads=[PB[0], pk], writes=[cbm])
                    k.op("dve", lambda e: e.tensor_tensor(out=cbm.ap[:, 128:256], in0=PB[0].ap[:, 128 + co:256 + co], in1=TGE, op=ALU.mult), reads=[PB[0], pk], writes=[cbm])
                    for (R, tri, dc) in ((Rf, TLE, 0), (Rb, TGE, 32)):
                        k.op("pool", lambda e: e.tensor_tensor(out=R.ap[:].rearrange("p (h i) -> p h i", i=128), in0=tri.unsqueeze(1).to_broadcast([128, 8, 128]),
                                                               in1=dta[q].ap[:, dc + g * 8:dc + g * 8 + 8].unsqueeze(2).to_broadcast([128, 8, 128]), op=ALU.mult),
                             reads=[pk, dta[q]], writes=[R])
                    yield
                    for (R, trl, b0, Lt, Mt, mc) in ((Rf, SU, 1, Lf, Mf, 0), (Rb, SL, 1, Lb, Mb, 128)):
                        for hh in range(2):
                            k.op("pe", lambda e: e.matmul(out=PB[b0 + hh].ap[:, :], lhsT=trl, rhs=R.ap[:, hh * 512:(hh + 1) * 512], start=True, stop=True), reads=[pk, R], writes=[PB[b0 + hh]])
                        k.op("act", lambda e: e.activation(out=Lt.ap[:], in_=pbf(b0, 2), func=AF.Exp), reads=[PB[b0], PB[b0 + 1]], writes=[Lt])
                        k.op("dve", lambda e: e.tensor_tensor(out=Mt.ap[:].rearrange("p (h i) -> p h i", i=128), in0=Lt.ap[:].rearrange("p (h i) -> p h i", i=128),
                                                              in1=cbm.ap[:, mc:mc + 128].unsqueeze(1).to_broadcast([128, 8, 128]), op=ALU.mult),
                             reads=[Lt, cbm], writes=[Mt])
                    yield
                    k.op("pe", lambda e: e.matmul(out=PB[5].ap[:, :], lhsT=idb.ap[:], rhs=xD.ap[:, g * 512:(g + 1) * 512], start=True, stop=False), reads=[idb, xD], writes=[PB[5]])
                    for (Mt, xdt) in ((Mf, xdt_f), (Mb, xdt_b)):
                        for h in range(8):
                            last = (Mt is Mb) and h == 7
                            k.op("pe", lambda e: e.matmul(out=PB[5].ap[:, h * 64:(h + 1) * 64], lhsT=Mt.ap[:, h * 128:(h + 1) * 128],
                                                          rhs=xdt.ap[:, (g * 8 + h) * 64:(g * 8 + h + 1) * 64], start=False, stop=last), reads=[Mt, xdt], writes=[PB[5]])
                    k.op("pe", lambda e: e.matmul(out=PB[6].ap[:, :], lhsT=CT.ap[:, g, cs], rhs=h16.ap[:, g * 512:(g + 1) * 512], start=True, stop=True), reads=[CT, h16], writes=[PB[6]])
                    k.op("pe", lambda e: e.matmul(out=PB[7].ap[:, :], lhsT=CT.ap[:, g, cs], rhs=hbt.ap[:, g * 512:(g + 1) * 512], start=True, stop=True), reads=[CT, hbt], writes=[PB[7]])

                    def bc8(c0):
                        return sm.ap[:, c0 + g * 8:c0 + g * 8 + 8].unsqueeze(2).to_broadcast([128, 8, 64])
                    k.op("dve", lambda e: e.tensor_tensor(out=yt1.ap[:].rearrange("p (h c) -> p h c", c=64), in0=PB[6].ap[:, :].rearrange("p (h c) -> p h c", c=64), in1=bc8(64), op=ALU.mult),
                         reads=[PB[6], sm], writes=[yt1])
                    k.op("dve", lambda e: e.tensor_tensor(out=yt2.ap[:].rearrange("p (h c) -> p h c", c=64), in0=PB[7].ap[:, :].rearrange("p (h c) -> p h c", c=64), in1=bc8(96), op=ALU.mult),
                         reads=[PB[7], sm], writes=[yt2])
                    k.op("pool", lambda e: e.tensor_tensor(out=yt1.ap[:], in0=yt1.ap[:], in1=yt2.ap[:], op=ALU.add), reads=[yt1, yt2], writes=[yt1])
                    k.op("dve", lambda e: e.tensor_tensor(out=yt1.ap[:], in0=PB[5].ap[:, :], in1=yt1.ap[:], op=ALU.add), reads=[PB[5], yt1], writes=[yt1])
                    k.op("pool", lambda e: e.tensor_tensor(out=yz.ap[:, g * 512:(g + 1) * 512], in0=yt1.ap[:], in1=zs.ap[:, q, g * 512:(g + 1) * 512], op=ALU.mult), reads=[yt1, zs], writes=[yz])
                    k.op("act", lambda e: e.activation(out=junk.ap[:, 0:512], in_=yz.ap[:, g * 512:(g + 1) * 512], func=AF.Square, accum_out=stat.ap[:, 4 + g:5 + g]), reads=[yz], writes=[junk, stat])
                    yield
                k.op("act", lambda e: e.activation(out=stat.ap[:, 8:12], in_=stat.ap[:, 4:8], func=AF.Sqrt, scale=1.0 / 512, bias=EPS), reads=[stat], writes=[stat])
                k.op("dve", lambda e: e.reciprocal(out=stat.ap[:, 12:16], in_=stat.ap[:, 8:12]), reads=[stat], writes=[stat])
                k.op("dve", lambda e: e.tensor_tensor(out=yzn.ap[:].rearrange("p (g c) -> p g c", c=512), in0=yz.ap[:].rearrange("p (g c) -> p g c", c=512),
                                                      in1=stat.ap[:, 12:16].unsqueeze(2).to_broadcast([128, 4, 512]), op=ALU.mult), reads=[yz, stat], writes=[yzn])
                for kb in range(16):
                    bb = 6 + kb // 8
                    k.op("pe", lambda e: e.transpose(out=pbh(bb)[:, (kb % 8) * 128:(kb % 8 + 1) * 128], in_=yzn.ap[:, kb * 128:(kb + 1) * 128], identity=idb.ap[:]), reads=[yzn, idb], writes=[PB[bb]])
                for kb in range(16):
                    bb = 6 + kb // 8
                    k.op("act", lambda e: e.activation(out=ynT.ap[:, kb, cs], in_=pbh(bb)[:, (kb % 8) * 128:(kb % 8 + 1) * 128], func=AF.Copy, scale=pk.ap[:, PK_SNW + kb:PK_SNW + kb + 1]),
                         reads=[PB[bb], pk], writes=[ynT])
                state_update(q, 0, sm, 0, 32)

            for q in range(NQ):
                prep_chunk(x_d[q * 128:(q + 1) * 128, :], q, 0)
            pending_tail = None
            outx1 = T("outx1", zs.ap[:].rearrange("p a b -> p (a b)").bitcast(F32), root=zs)
            outxs = [outx, outx1]
            for s in range(nsc):
                xg = gen_proj_fm(win16, C_XBC, 24, hxT, TS, conv_evac((xsT, BT, CT), 0, TS, 64), t_wA, [5, 6, 7] if pending_tail is not None else None)
                if pending_tail is not None:
                    for nfill in pending_tail:
                        pull(xg, nfill)
                for _ in xg:
                    pass
                for g4 in range(8):
                    t = wload(win16[:, C_Z + g4 * 256: C_Z + (g4 + 1) * 256], t_wB)
                    for q in range(NQ):
                        b = next_pb()
                        for kb in range(16):
                            k.op("pe", lambda e: e.matmul(out=PB[b].ap[:, 0:256], lhsT=hxT.ap[:, kb, q * 128:(q + 1) * 128], rhs=t.ap[:, kb, :], start=(kb == 0), stop=(kb == 15)),
                                 reads=[hxT, t], writes=[PB[b]])
                        k.op("act", lambda e: e.activation(out=zs.ap[:, q, g4 * 256:(g4 + 1) * 256], in_=PB[b].ap[:, 0:256], func=AF.Silu), reads=[PB[b]], writes=[zs])
                for q in range(NQ):
                    dt_chunk(q)
                    to_tok(q)
                def ev_c(m, pb):
                    k.op("act", lambda e: e.activation(out=cv.ap[:, m, :], in_=pb.ap[:, 0:TS], func=AF.Copy), reads=[pb], writes=[cv])

                def ev_v(m, pb):
                    ct = ctmp[m % 2]
                    k.op("dve", lambda e: e.tensor_tensor(out=ct.ap[:], in0=pb.ap[:, 0:TS], in1=cv.ap[:, m, :], op=ALU.mult), reads=[pb, cv], writes=[ct])
                    w0 = pk.ap[:, PK_SCW + m:PK_SCW + m + 1]
                    w1 = pk.ap[:, PK_SCW + 16 + m:PK_SCW + 17 + m]
                    w2 = pk.ap[:, PK_SCW + 32 + m:PK_SCW + 33 + m]
                    k.op("act", lambda e: e.activation(out=cv.ap[:, m, :], in_=ct.ap[:], func=AF.Copy, scale=w1), reads=[ct, pk], writes=[cv])
                    c3 = cv.ap[:, m, :].rearrange("p (r c) -> p r c", c=64)
                    t3 = ct.ap[:].rearrange("p (r c) -> p r c", c=64)
                    k.op("dve", lambda e: e.scalar_tensor_tensor(out=c3[:, :, 1:64], in0=t3[:, :, 0:63], scalar=w0, in1=c3[:, :, 1:64], op0=ALU.mult, op1=ALU.add), reads=[ct, cv, pk], writes=[cv])
                    k.op("dve", lambda e: e.scalar_tensor_tensor(out=c3[:, :, 0:63], in0=t3[:, :, 1:64], scalar=w2, in1=c3[:, :, 0:63], op0=ALU.mult, op1=ALU.add), reads=[ct, cv, pk], writes=[cv])

                def ev_b(m, pb):
                    k.op("dve", lambda e: e.tensor_tensor(out=scyT.ap[:, m, :], in0=pb.ap[:, 0:TS], in1=cv.ap[:, m, :], op=ALU.mult), reads=[pb, cv], writes=[scyT])

                def ev_g(dst, boff):
                    def f(m, pb):
                        k.op("act", lambda e: e.activation(out=dst.ap[:, m, :], in_=pb.ap[:, 0:TS], func=AF.Sigmoid, bias=pk.ap[:, PK_BG + boff + m:PK_BG + boff + m + 1]),
                             reads=[pb, pk], writes=[dst])
                    return f

                def filler():
                    FB = [3, 4]
                    yield from gen_proj_fm(win16, C_SCC, 16, hxT, TS, ev_c, t_wC, FB)
                    yield from gen_proj_fm(win16, C_SCV, 16, hxT, TS, ev_v, t_wC, FB)
                    yield from gen_proj_fm(win16, C_SCB, 16, hxT, TS, ev_b, t_wC, FB)
                    yield from gen_proj_fm(win16, C_GS, 16, hxT, TS, ev_g(mtmp, 0), t_wC, FB)
                fill = filler()
                nyld = 0
                for q in range(NQ):
                    for _ in ssd_chunk(s, q):
                        pull(fill, (3, 3, 2)[nyld % 3])
                        nyld += 1
                for _ in fill:
                    pass
                if dbg and stage == 2 and s == 0:
                    o = dbg_out("d_yz", [128, D])
                    k.dma("sp", o[:, :], yz.ap[:], yz, reads=[yz])
                    o = dbg_out("d_ynT", [128, 16 * TS], BF16)
                    k.dma("sp", o[:, :], ynT.ap[:].rearrange("p a b -> p (a b)"), ynT, reads=[ynT])
                    k.barrier()
                    return nc, dbg_d
                def ev_o1(m, pb):
                    k.op("dve", lambda e: e.tensor_tensor(out=mtmp.ap[:, m, :], in0=pb.ap[:, 0:TS], in1=mtmp.ap[:, m, :], op=ALU.mult), reads=[pb, mtmp], writes=[mtmp])
                proj_fm(wssd16, 0, 16, ynT, TS, ev_o1, t_wssd)
                proj_fm(win16, C_GC, 16, hxT, TS, ev_g(gcT, 16), t_wC)

                def ev_o2(m, pb):
                    ct = ctmp[m % 2]
                    k.op("dve", lambda e: e.tensor_tensor(out=ct.ap[:], in0=pb.ap[:, 0:TS], in1=gcT.ap[:, m, :], op=ALU.mult), reads=[pb, gcT], writes=[ct])
                    k.op("pool", lambda e: e.tensor_tensor(out=mergedT.ap[:, m, :], in0=ct.ap[:], in1=mtmp.ap[:, m, :], op=ALU.add), reads=[ct, mtmp], writes=[mergedT])
                proj_fm(wsc16, 0, 16, scyT, TS, ev_o2, t_wsc)

                if s + 1 < nsc:
                    for q in range(NQ):
                        prep_chunk(x_d[(s + 1) * TS + q * 128: (s + 1) * TS + (q + 1) * 128, :], q, 0)
                for g4 in range(8):
                    t = wload(wo16[:, g4 * 256:(g4 + 1) * 256], t_wo)
                    for q in range(NQ):
                        b = next_pb()
                        for kb in range(16):
                            k.op("pe", lambda e: e.matmul(out=PB[b].ap[:, 0:256], lhsT=mergedT.ap[:, kb, q * 128:(q + 1) * 128], rhs=t.ap[:, kb, :], start=(kb == 0), stop=(kb == 15)),
                                 reads=[mergedT, t], writes=[PB[b]])
                        k.op("act" if q == 0 else "dve",
                             (lambda e: e.activation(out=outxs[q].ap[:, g4 * 256:(g4 + 1) * 256], in_=PB[b].ap[:, 0:256], func=AF.Copy)) if q == 0 else
                             (lambda e: e.tensor_copy(out=outxs[q].ap[:, g4 * 256:(g4 + 1) * 256], in_=PB[b].ap[:, 0:256])),
                             reads=[PB[b]], writes=[outxs[q]])

                def tail_gen(s=s):
                    for q in range(NQ):
                        c = s * NQ + q
                        ox = outxs[q]
                        xt = xc[0]
                        x1t = x1buf
                        k.dma("sp", xt.ap[:], x_d[c * 128:(c + 1) * 128, :], xt, writes=[xt])
                        k.op("act", lambda e: e.activation(out=junk.ap[:], in_=ox.ap[:], func=AF.Square, accum_out=stat.ap[:, 0:1]), reads=[ox], writes=[junk, stat])
                        k.op("act", lambda e: e.activation(out=stat.ap[:, 1:2], in_=stat.ap[:, 0:1], func=AF.Sqrt, scale=1.0 / D, bias=EPS), reads=[stat], writes=[stat])
                        k.op("dve", lambda e: e.reciprocal(out=stat.ap[:, 2:3], in_=stat.ap[:, 1:2]), reads=[stat], writes=[stat])
                        k.op("dve", lambda e: e.tensor_tensor(out=ox.ap[:], in0=ox.ap[:], in1=G1.ap[:], op=ALU.mult), reads=[ox, G1], writes=[ox])
                        k.op("dve", lambda e: e.scalar_tensor_tensor(out=x1t.ap[:], in0=ox.ap[:], scalar=stat.ap[:, 2:3], in1=xt.ap[:], op0=ALU.mult, op1=ALU.add), reads=[ox, stat, xt], writes=[x1t])
                        k.dma("sp", x1_s[c * 128:(c + 1) * 128, :], x1t.ap[:], x1t, reads=[x1t])
                        k.op("act", lambda e: e.activation(out=junk.ap[:], in_=x1t.ap[:], func=AF.Square, accum_out=stat.ap[:, 0:1]), reads=[x1t], writes=[junk, stat])
                        k.op("act", lambda e: e.activation(out=stat.ap[:, 1:2], in_=stat.ap[:, 0:1], func=AF.Sqrt, scale=1.0 / D, bias=EPS), reads=[stat], writes=[stat])
                        k.op("dve", lambda e: e.reciprocal(out=stat.ap[:, 2:3], in_=stat.ap[:, 1:2]), reads=[stat], writes=[stat])
                        k.op("dve", lambda e: e.scalar_tensor_tensor(out=ox.ap[:], in0=x1t.ap[:], scalar=stat.ap[:, 2:3], in1=A2.ap[:], op0=ALU.mult, op1=ALU.mult), reads=[x1t, stat, A2], writes=[ox])
                        k.op("dve", lambda e: e.tensor_tensor(out=ox.ap[:], in0=ox.ap[:], in1=B2.ap[:], op=ALU.add), reads=[ox, B2], writes=[ox])
                        k.op("act", lambda e: e.activation(out=hx2.ap[:], in_=ox.ap[:], func=AF.Copy), reads=[ox], writes=[hx2])
                        k.dma("sp", hx2_s[c * 128:(c + 1) * 128, :], hx2.ap[:], hx2, reads=[hx2])
                        yield (8 if q == 0 else 6)
                        for kb in range(16):
                            bb = kb // 4
                            k.op("pe", lambda e: e.transpose(out=PB[bb].ap[:, (kb % 4) * 128:(kb % 4 + 1) * 128], in_=ox.ap[:, kb * 128:(kb + 1) * 128], identity=ident), reads=[ox, pk], writes=[PB[bb]])
                        k.op("act", lambda e: e.activation(out=x1t.ap[:], in_=pbf(0, 4), func=AF.Copy), reads=[PB[0], PB[1], PB[2], PB[3]], writes=[x1t])
                        yield (4 if q == 0 else 3)
                        for kb in range(16):
                            k.op("pe", lambda e: e.matmul(out=PB[4].ap[:, 0:16], lhsT=x1t.ap[:, kb * 128:(kb + 1) * 128], rhs=pk.ap[:, PK_WR + kb * 16:PK_WR + (kb + 1) * 16], start=(kb == 0), stop=(kb == 15)),
                                 reads=[x1t, pk], writes=[PB[4]])
                        k.op("dve", lambda e: e.tensor_reduce(out=lg.ap[:, 16:17], in_=PB[4].ap[:, 0:16], op=ALU.max, axis=mybir.AxisListType.X), reads=[PB[4]], writes=[lg])
                        k.op("dve", lambda e: e.tensor_scalar(out=lg.ap[:, 17:18], in0=lg.ap[:, 16:17], scalar1=-1.0, scalar2=None, op0=ALU.mult), reads=[lg], writes=[lg])
                        k.op("act", lambda e: e.activation(out=lg.ap[:, 0:16], in_=PB[4].ap[:, 0:16], func=AF.Exp, bias=lg.ap[:, 17:18], accum_out=lg.ap[:, 18:19]), reads=[PB[4], lg], writes=[lg])
                        k.op("dve", lambda e: e.reciprocal(out=lg.ap[:, 19:20], in_=lg.ap[:, 18:19]), reads=[lg], writes=[lg])
                        k.op("dve", lambda e: e.tensor_scalar(out=affAll.ap[:, c, :], in0=lg.ap[:, 0:16], scalar1=lg.ap[:, 19:20], scalar2=None, op0=ALU.mult), reads=[lg], writes=[affAll])
                pending_tail = tail_gen()
                if stage == 2 and s == 1:
                    break
            for _ in pending_tail:
                pass
            if dbg and stage in (2, 3):
                o = dbg_out("d_aff", [128, NCH * NE])
                k.dma("sp", o[:, :], affAll.ap[:].rearrange("p a b -> p (a b)"), affAll, reads=[affAll])
            k.barrier()
        if stage in (2, 3):
            with contextlib.ExitStack() as esd:
                Sd = mk_alloc(esd)
                tt = Sd("tt", [128, D])
                for c in range(NCH):
                    k.dma("sp", tt.ap[:], x1_s[c * 128:(c + 1) * 128, :], tt, writes=[tt])
                    k.dma("sp", out_d[c * 128:(c + 1) * 128, :], tt.ap[:], tt, reads=[tt])
                k.barrier()
            return nc, dbg_d

        RG = [list(range(8))]
        NCOL = NE
        aff_loc = nc.dram_tensor("aff_loc", [NE, L], F32, kind="Internal").ap()
        aff_all = nc.dram_tensor("aff_all", [8 * NE, L], F32, kind="Internal").ap()
        t_hx2all = T("hx2_s", hx2_s)
        t_ys = T("y_s", y_s)
        ccn = [0]

        def allgather(in_ap, out_ap, t_in, t_out):
            if one_core:
                k.dma("pool", out_ap[0:in_ap.shape[0], :], in_ap, t_out, reads=[t_in], writes=[t_out])
                return
            nm = "cc%d" % ccn[0]
            ccn[0] += 1
            k.sems[nm] = nc.alloc_semaphore(nm)
            k._wait("pool", k._deps([t_in], [t_out]))
            ins = nc.gpsimd.collective_compute("AllGather", ALU.bypass, replica_groups=RG, ins=[in_ap], outs=[out_ap])
            ins.then_inc(k.sems[nm], 1)
            k._mark((nm, 1), [t_in], [t_out])


        esX = contextlib.ExitStack()
        es_all.enter_context(esX)
        SX = mk_alloc(esX)
        gidx32 = SX("gidx32", [128, NCOL * 4], I32)
        gval = SX("gval", [128, NCOL * 4])
        osrc32 = SX("osrc32", [128, 64], I32)
        with contextlib.ExitStack() as esE:
            SE = mk_alloc(esE)
            affT = SE("affT", [16, L])
            selT = SE("selT", [NE, L])
            cumT = SE("cumT", [NE, L])
            onesb = SE("onesb", [NE, L], BF16)
            bs = SE("bs", [NE, 8])
            tokA = SE("tokA", [128, 3, NCH * NCOL])
            key = SE("key", [128, NCH * NCOL])
            tv = SE("tv", [128, NCH * NCOL, 5], BF16)
            r1 = SE("r1", [128, NCH * NCOL])
            oh = [SE("oh%d" % i, [128, CAP], BF16) for i in range(2)]
            idxv = SE("idxv", [128, NCOL * 4, 5])
            idxf = SE("idxf", [128, NCOL * 4])
            for c4 in range(NCH // 4):
                for j in range(4):
                    c = c4 * 4 + j
                    k.op("pe", lambda e: e.transpose(out=PB[c4 % 2].ap[0:16, j * 128:(j + 1) * 128], in_=affAll.ap[:, c, :], identity=ident), reads=[affAll, pk], writes=[PB[c4 % 2]])
                k.op("act", lambda e: e.activation(out=affT.ap[:, c4 * 512:(c4 + 1) * 512], in_=PB[c4 % 2].ap[0:16, :], func=AF.Copy), reads=[PB[c4 % 2]], writes=[affT])
            affG = affT
            k.op("dve", lambda e: e.memset(bs.ap[:, 0:1], 0.0), writes=[bs])
            k.op("dve", lambda e: e.memset(bs.ap[:, 1:2], 2.0), writes=[bs])
            k.op("dve", lambda e: e.memset(onesb.ap[:], 1.0), writes=[onesb])
            for it in range(NBIS):
                k.op("dve", lambda e: e.tensor_scalar(out=bs.ap[:, 2:3], in0=bs.ap[:, 0:1], scalar1=bs.ap[:, 1:2], scalar2=0.5, op0=ALU.add, op1=ALU.mult), reads=[bs], writes=[bs])
                k.op("dve", lambda e: e.tensor_scalar(out=selT.ap[:], in0=affG.ap[:], scalar1=bs.ap[:, 2:3], scalar2=None, op0=ALU.is_ge, op1=ALU.add, accum_out=bs.ap[:, 3:4]),
                     reads=[affG, bs], writes=[selT, bs])
                k.op("dve", lambda e: e.tensor_scalar(out=bs.ap[:, 4:5], in0=bs.ap[:, 3:4], scalar1=float(CAP), scalar2=None, op0=ALU.is_ge), reads=[bs], writes=[bs])
                k.op("dve", lambda e: e.scalar_tensor_tensor(out=bs.ap[:, 5:6], in0=bs.ap[:, 4:5], scalar=2.0, in1=bs.ap[:, 2:3], op0=ALU.mult, op1=ALU.add), reads=[bs], writes=[bs])
                k.op("dve", lambda e: e.scalar_tensor_tensor(out=bs.ap[:, 0:1], in0=bs.ap[:, 2:3], scalar=bs.ap[:, 4:5], in1=bs.ap[:, 0:1], op0=ALU.mult, op1=ALU.max), reads=[bs], writes=[bs])
                k.op("dve", lambda e: e.tensor_tensor(out=bs.ap[:, 1:2], in0=bs.ap[:, 1:2], in1=bs.ap[:, 5:6], op=ALU.min), reads=[bs], writes=[bs])
            k.op("dve", lambda e: e.tensor_scalar(out=selT.ap[:], in0=affG.ap[:], scalar1=bs.ap[:, 0:1], scalar2=None, op0=ALU.is_ge), reads=[affG, bs], writes=[selT])
            k.op("dve", lambda e: e.tensor_tensor_scan(out=cumT.ap[:], data0=onesb.ap[:], data1=selT.ap[:], initial=0.0, op0=ALU.mult, op1=ALU.add), reads=[onesb, selT], writes=[cumT])
            k.op("dve", lambda e: e.tensor_tensor(out=affG.ap[:], in0=affG.ap[:], in1=selT.ap[:], op=ALU.mult), reads=[affG, selT], writes=[affG])
            selm = pk.ap[0:NE, PK_SELM:PK_SELM + NCOL]
            for w, srcT in enumerate((cumT, selT, affG)):
                for c in range(NCH):
                    bb = 2 + 2 * w + c // 16
                    oc = (c % 16) * 32
                    k.op("pe", lambda e: e.matmul(out=PB[bb].ap[:, oc:oc + NCOL], lhsT=srcT.ap[:, c * 128:(c + 1) * 128], rhs=selm, start=True, stop=True), reads=[srcT, pk], writes=[PB[bb]])
                k.op("act", lambda e: e.activation(out=tokA.ap[:, w, :].rearrange("p (c x) -> p c x", x=NCOL), in_=pbf(2 + 2 * w, 2).rearrange("p (c x) -> p c x", x=32)[:, :, 0:NCOL], func=AF.Copy), reads=[PB[2 + 2 * w], PB[3 + 2 * w]], writes=[tokA])
            k.op("dve", lambda e: e.tensor_tensor(out=key.ap[:], in0=tokA.ap[:, 0, :], in1=tokA.ap[:, 1, :], op=ALU.mult), reads=[tokA], writes=[key])
            k.op("dve", lambda e: e.tensor_scalar(out=key.ap[:], in0=key.ap[:], scalar1=-1.0, scalar2=None, op0=ALU.add), reads=[key], writes=[key])
            k.op("pool", lambda e: e.iota(tv.ap[:, :, 0].rearrange("p (c e) -> p c e", e=NCOL), pattern=[[1, NCH], [0, NCOL]], base=0, channel_multiplier=0, allow_small_or_imprecise_dtypes=True), writes=[tv])
            k.op("pool", lambda e: e.iota(tv.ap[:, :, 1], pattern=[[0, NCH * NCOL]], base=0, channel_multiplier=1, allow_small_or_imprecise_dtypes=True), writes=[tv])
            k.op("dve", lambda e: e.tensor_copy(out=tv.ap[:, :, 2], in_=tokA.ap[:, 2, :]), reads=[tokA], writes=[tv])
            k.op("dve", lambda e: e.tensor_tensor(out=r1.ap[:], in0=tokA.ap[:, 2, :], in1=tv.ap[:, :, 2], op=ALU.subtract), reads=[tokA, tv], writes=[r1])
            k.op("dve", lambda e: e.tensor_copy(out=tv.ap[:, :, 3], in_=r1.ap[:]), reads=[r1], writes=[tv])
            k.op("dve", lambda e: e.tensor_tensor(out=r1.ap[:], in0=r1.ap[:], in1=tv.ap[:, :, 3], op=ALU.subtract), reads=[r1, tv], writes=[r1])
            k.op("dve", lambda e: e.tensor_copy(out=tv.ap[:, :, 4], in_=r1.ap[:]), reads=[r1], writes=[tv])
            first = True
            n_oh = 0
            for col in range(NCOL):
                for c in range(NCH):
                    o_t = oh[n_oh % 2]
                    n_oh += 1
                    kc = c * NCOL + col
                    k.op("dve", lambda e: e.tensor_scalar(out=o_t.ap[:], in0=pk.ap[:, PK_IOTA:PK_IOTA + CAP], scalar1=key.ap[:, kc:kc + 1], scalar2=None, op0=ALU.is_equal), reads=[pk, key], writes=[o_t])
                    for jb in range(4):
                        oc = (col * 4 + jb) * 5
                        k.op("pe", lambda e: e.matmul(out=PB[0].ap[:, oc:oc + 5], lhsT=o_t.ap[:, jb * 128:(jb + 1) * 128], rhs=tv.ap[:, kc, :], start=first, stop=(col == NCOL - 1 and c == NCH - 1 and jb == 3)),
                             reads=[o_t, tv], writes=[PB[0]])
                        first = False
            k.op("act", lambda e: e.activation(out=idxv.ap[:].rearrange("p a b -> p (a b)"), in_=PB[0].ap[:, 0:NCOL * 20], func=AF.Copy), reads=[PB[0]], writes=[idxv])
            k.op("dve", lambda e: e.scalar_tensor_tensor(out=idxf.ap[:], in0=idxv.ap[:, :, 0], scalar=128.0, in1=idxv.ap[:, :, 1], op0=ALU.mult, op1=ALU.add), reads=[idxv], writes=[idxf])
            k.op("dve", lambda e: e.tensor_copy(out=gidx32.ap[:], in_=idxf.ap[:]), reads=[idxf], writes=[gidx32])
            k.op("dve", lambda e: e.tensor_tensor(out=gval.ap[:], in0=idxv.ap[:, :, 2], in1=idxv.ap[:, :, 3], op=ALU.add), reads=[idxv], writes=[gval])
            k.op("dve", lambda e: e.tensor_tensor(out=gval.ap[:], in0=gval.ap[:], in1=idxv.ap[:, :, 4], op=ALU.add), reads=[gval, idxv], writes=[gval])
            if dbg and stage == 4:
                o = dbg_out("d_idx", [128, NCOL * 4], I32)
                k.dma("sp", o[:, :], gidx32.ap[:], gidx32, reads=[gidx32])
                o = dbg_out("d_gval", [128, NCOL * 4])
                k.dma("sp", o[:, :], gval.ap[:], gval, reads=[gval])
                o = dbg_out("d_bs", [NE, 8])
                k.dma("sp", o[:, :], bs.ap[:], bs, reads=[bs])
                k.barrier()
                return nc, dbg_d
            k.barrier()

        with contextlib.ExitStack() as esF:
            SF = mk_alloc(esF)
            xg = [SF("xg%d" % i, [128, D], BF16) for i in range(4)]
            xgT = SF("xgT", [128, 16, CAP], BF16)
            wq = [SF("wq%d" % i, [128, 16, 256], BF16) for i in range(6)]
            actT = SF("actT", [128, 16, CAP], BF16)
            sg = [SF("sg%d" % i, [128, CAP]) for i in range(2)]
            ost = [SF("ost%d" % i, [128, D], BF16) for i in range(4)]
            wqc = [0]

            def wload2(src_ap):
                t = wq[wqc[0] % 6]
                wqc[0] += 1
                k.dma("pool", t.ap[:], src_ap.rearrange("(kb p) c -> p kb c", p=128), t, writes=[t])
                return t

            pbc = [0]

            def npb():
                b = pbc[0] % 8
                pbc[0] += 1
                return b
            n_e = 2 if (dbg and stage == 5) else NE
            def gather(col):
                for jb in range(4):
                    ic = col * 4 + jb
                    k.dma("pool", None, None, xg[jb], reads=[gidx32, t_hx2all], writes=[xg[jb]],
                          fn=lambda e: e.indirect_dma_start(out=xg[jb].ap[:], out_offset=None, in_=hx2_s[:, :],
                                                            in_offset=bass.IndirectOffsetOnAxis(ap=gidx32.ap[:, ic:ic + 1], axis=0)))

            def scatter(col):
                for jb in range(4):
                    sc = col * 4 + jb
                    k.dma("pool", None, None, t_ys, reads=[ost[jb], gidx32], writes=([t_ys] if jb == 0 else []),
                          fn=lambda e: e.indirect_dma_start(out=y_s[:, :], out_offset=bass.IndirectOffsetOnAxis(ap=gidx32.ap[:, sc:sc + 1], axis=0), in_=ost[jb].ap[:], in_offset=None, compute_op=ALU.add))
                t_ys.root.w = (t_ys.root.dsem, 16 * t_ys.root.dcnt)
                t_ys.root.r = []

            wlist = []
            for e_ in range(n_e):
                for fg in range(8):
                    wlist.append(w_e1_d[e_, :, fg * 256:(fg + 1) * 256])
                    wlist.append(w_e3_d[e_, :, fg * 256:(fg + 1) * 256])
                for dg in range(8):
                    wlist.append(w_e2_d[e_, :, dg * 256:(dg + 1) * 256])
            wtile = {}

            def ensure(n):
                while wqc[0] < min(n, len(wlist)):
                    i = wqc[0]
                    wtile[i] = wload2(wlist[i])

            gather(0)
            ensure(6)
            for le in range(n_e):
                for b_ in range(1):
                    col = le
                    for kb in range(16):
                        bb = npb()
                        for jb in range(4):
                            k.op("pe", lambda e: e.transpose(out=pbh(bb)[:, jb * 128:(jb + 1) * 128], in_=xg[jb].ap[:, kb * 128:(kb + 1) * 128], identity=idb.ap[:]), reads=[xg[jb], idb], writes=[PB[bb]])
                        k.op("act" if kb % 2 == 0 else "dve",
                             (lambda e: e.activation(out=xgT.ap[:, kb, :], in_=pbh(bb)[:, 0:CAP], func=AF.Copy)) if kb % 2 == 0 else (lambda e: e.tensor_copy(out=xgT.ap[:, kb, :], in_=pbh(bb)[:, 0:CAP])),
                             reads=[PB[bb]], writes=[xgT])
                    if le + 1 < n_e:
                        gather(le + 1)
                    if le > 0:
                        scatter(le - 1)
                    for fg in range(8):
                        wi = le * 24 + fg * 2
                        w1t, w3t = wtile[wi], wtile[wi + 1]
                        for j in range(2):
                            fb = fg * 2 + j
                            bg, bu = npb(), npb()
                            for kb in range(16):
                                k.op("pe", lambda e: e.matmul(out=PB[bg].ap[:, :], lhsT=w1t.ap[:, kb, j * 128:(j + 1) * 128], rhs=xgT.ap[:, kb, :], start=(kb == 0), stop=(kb == 15)), reads=[w1t, xgT], writes=[PB[bg]])
                            for kb in range(16):
                                k.op("pe", lambda e: e.matmul(out=PB[bu].ap[:, :], lhsT=w3t.ap[:, kb, j * 128:(j + 1) * 128], rhs=xgT.ap[:, kb, :], start=(kb == 0), stop=(kb == 15)), reads=[w3t, xgT], writes=[PB[bu]])
                            sgt = sg[fb % 2]
                            k.op("act", lambda e: e.activation(out=sgt.ap[:], in_=PB[bg].ap[:, :], func=AF.Silu), reads=[PB[bg]], writes=[sgt])
                            k.op("dve", lambda e: e.tensor_tensor(out=actT.ap[:, fb, :], in0=PB[bu].ap[:, :], in1=sgt.ap[:], op=ALU.mult), reads=[PB[bu], sgt], writes=[actT])
                        ensure(wi + 2 + 6)
                    for dg in range(8):
                        wi = le * 24 + 16 + dg
                        w2t = wtile[wi]
                        for jb in range(4):
                            bo = npb()
                            for fb in range(16):
                                k.op("pe", lambda e: e.matmul(out=PB[bo].ap[:, 0:256], lhsT=actT.ap[:, fb, jb * 128:(jb + 1) * 128], rhs=w2t.ap[:, fb, :], start=(fb == 0), stop=(fb == 15)), reads=[actT, w2t], writes=[PB[bo]])
                            ic = col * 4 + jb
                            k.op("act", lambda e: e.activation(out=ost[jb].ap[:, dg * 256:(dg + 1) * 256], in_=PB[bo].ap[:, 0:256], func=AF.Copy, scale=gval.ap[:, ic:ic + 1]), reads=[PB[bo], gval], writes=[ost[jb]])
                        ensure(wi + 1 + 6)
            scatter(n_e - 1)
            k.barrier()

        with contextlib.ExitStack() as esG:
            SG = mk_alloc(esG)
            yt = [SG("yfin%d" % i, [128, D]) for i in range(2)]
            x1t = [SG("x1t%d" % i, [128, D]) for i in range(2)]
            G2 = SG("G2f", [128, D])
            st2 = SG("st2", [128, 8])
            jk = SG("jk", [128, D], BF16)
            k.dma("sp", G2.ap[:], g2_s[:, :], G2, writes=[G2])
            for c in range(NCH):
                y_t, x_t = yt[c % 2], x1t[c % 2]
                k.dma("sp", y_t.ap[:], y_s[c * 128:(c + 1) * 128, :], y_t, reads=[t_ys], writes=[y_t])
                k.dma("sp", x_t.ap[:], x1_s[c * 128:(c + 1) * 128, :], x_t, writes=[x_t])
                k.op("act", lambda e: e.activation(out=jk.ap[:], in_=y_t.ap[:], func=AF.Square, accum_out=st2.ap[:, 0:1]), reads=[y_t], writes=[jk, st2])
                k.op("act", lambda e: e.activation(out=st2.ap[:, 1:2], in_=st2.ap[:, 0:1], func=AF.Sqrt, scale=1.0 / D, bias=EPS), reads=[st2], writes=[st2])
                k.op("dve", lambda e: e.reciprocal(out=st2.ap[:, 2:3], in_=st2.ap[:, 1:2]), reads=[st2], writes=[st2])
                k.op("dve", lambda e: e.tensor_tensor(out=y_t.ap[:], in0=y_t.ap[:], in1=G2.ap[:], op=ALU.mult), reads=[y_t, G2], writes=[y_t])
                k.op("dve", lambda e: e.scalar_tensor_tensor(out=y_t.ap[:], in0=y_t.ap[:], scalar=st2.ap[:, 2:3], in1=x_t.ap[:], op0=ALU.mult, op1=ALU.add), reads=[y_t, st2, x_t], writes=[y_t])
                k.dma("sp", out_d[c * 128:(c + 1) * 128, :], y_t.ap[:], y_t, reads=[y_t])
            if dbg:
                o = dbg_out("d_ys", [L, D])
                tt2 = T("d_ys", o)
                k.dma("sp", o[:, :], y_s[:, :], tt2, reads=[t_ys], writes=[tt2])
            k.barrier()

    return nc, dbg_d


def _fm(v, nb):
    return np.ascontiguousarray(np.asarray(v, np.float32).reshape(nb, 128).T)


def _host_inputs(r, x, c, ctx, c_ctx, w_mod, b_mod, norm_pre_mix, norm_post_mix, w_in, ssd_conv_w,
                 ssd_conv_b, dt_bias, a_log, ssd_d, ssd_norm_w, w_ssd_out, sc_conv_w, w_sc_out,
                 b_gate, w_o, norm_pre_ffn, norm_post_ffn, w_router, w_e1, w_e3, w_e2):
    f = np.float32
    b = r % 4
    pk = np.zeros((128, NPK), f)
    ii = np.arange(128)
    pk[:, PK_ID:PK_ID + 128] = np.eye(128, dtype=f)
    pk[:, PK_SU:PK_SU + 128] = (ii[:, None] > ii[None, :])
    pk[:, PK_SL:PK_SL + 128] = (ii[:, None] < ii[None, :])
    pk[:, PK_TLE:PK_TLE + 128] = (ii[:, None] <= ii[None, :])
    pk[:, PK_TGE:PK_TGE + 128] = (ii[:, None] >= ii[None, :])
    pk[:, PK_ONE:PK_ONE + 128] = 1.0
    cv = np.stack([_fm(c[b], 16), _fm(c_ctx, 16)], axis=2).reshape(128, 32)
    pk[:, PK_C:PK_C + 32] = cv
    pk[:, PK_BMOD:PK_BMOD + 16] = _fm(b_mod[0, 0:D], 16)
    pk[:, PK_BMOD + 16:PK_BMOD + 32] = _fm(b_mod[0, D:2 * D], 16)
    pk[:, PK_NPM:PK_NPM + 16] = _fm(norm_pre_mix[0], 16)
    pk[:, PK_SNW:PK_SNW + 16] = _fm(ssd_norm_w[0], 16)
    for t in range(3):
        pk[:, PK_CW + t * 24:PK_CW + (t + 1) * 24] = _fm(ssd_conv_w[0, t], 24)
        pk[:, PK_SCW + t * 16:PK_SCW + (t + 1) * 16] = _fm(sc_conv_w[0, t], 16)
    pk[:, PK_CB:PK_CB + 24] = _fm(ssd_conv_b[0], 24)
    pk[:, PK_BG:PK_BG + 32] = _fm(b_gate[0], 32)
    pk[:, PK_DTB:PK_DTB + 64] = np.asarray(dt_bias[0], f).reshape(1, 64)
    pk[:, PK_ALOG:PK_ALOG + 64] = np.asarray(a_log[0], f).reshape(1, 64)
    pk[:, PK_SSD_D:PK_SSD_D + 32] = np.asarray(ssd_d[0], f).reshape(1, 32)
    pk[:, PK_WR:PK_WR + 256] = np.asarray(w_router[0], f).reshape(16, 128, 16).transpose(1, 0, 2).reshape(128, 256)
    pk[:, PK_IOTA:PK_IOTA + 512] = np.arange(512, dtype=f)[None, :]
    pk[:, PK_PIDX] = ii
    pk[0:NE, PK_SELM:PK_SELM + NE] = np.eye(NE, dtype=f)
    reps = np.empty((128, 7 * D), f)
    reps[:, 0:D] = np.asarray(norm_post_mix[0], f)[None, :]
    reps[:, D:2 * D] = np.asarray(norm_pre_ffn[0], f)[None, :]
    reps[:, 2 * D:3 * D] = np.asarray(norm_post_ffn[0], f)[None, :]
    reps[:, 3 * D:7 * D] = np.asarray(b_mod[0, 2 * D:6 * D], f)[None, :]
    return {
        "x": np.ascontiguousarray(x[b]), "ctx": np.ascontiguousarray(ctx[b]),
        "w_in": np.ascontiguousarray(w_in[0]), "w_mod": np.ascontiguousarray(w_mod[0]),
        "w_ssd_out": np.ascontiguousarray(w_ssd_out[0]), "w_sc_out": np.ascontiguousarray(w_sc_out[0]),
        "w_o": np.ascontiguousarray(w_o[0]),
        "w_e1": np.ascontiguousarray(w_e1[0]), "w_e3": np.ascontiguousarray(w_e3[0]), "w_e2": np.ascontiguousarray(w_e2[0]),
        "pk": pk, "reps": reps, "idb": np.eye(128, dtype=np.float32).astype(ml_dtypes.bfloat16),
    }


def kernel(**inputs):
    inputs = {k_: np.asarray(v) for k_, v in inputs.items()}
    nc, _ = build_program()
    in_maps = [_host_inputs(r, **inputs) for r in range(8)]
    res = run_bass_kernel_spmd(nc, in_maps, core_ids=list(range(8)))
    out = np.stack([np.asarray(res.results[b]["out"], np.float32) for b in range(4)], axis=0)
    return out
```
